# Optimizing a Trainium2 kernel written in Bass

```python
import jax, jax.numpy as jnp
from jax import lax
import numpy as np

D_MODEL = 1024
BATCH = 4
SEQ = 4096
DEPTH = 4

N_MIXERS = 2
N_A = (DEPTH + 1) // 2
N_B = DEPTH // 2
LRU_WIDTH = D_MODEL
LRU_HEADS = 4
LRU_BLOCK = LRU_WIDTH // LRU_HEADS
RG_C = 8.0
CONV_WIDTH = 4
CONV_LEFT = 2
FOURIER_GROUPS = 4
FOURIER_GROUP_DIM = D_MODEL // FOURIER_GROUPS
N_EXPERTS = 16
CAPACITY_FACTOR = 2
EXPERT_FF = 2 * D_MODEL
PLE_DIM = 256
EPS = 1e-6

kernel_name = "hybrid_rglru_fnet_expert_choice_encoder"


def rms_norm(x, g):
    xf = x.astype(jnp.float32)
    y = xf * lax.rsqrt(jnp.mean(xf * xf, axis=-1, keepdims=True) + EPS)
    return (y * g.astype(jnp.float32)).astype(x.dtype)


def _lin_rec(left, right):
    a1, b1 = left
    a2, b2 = right
    return a1 * a2, a2 * b1 + b2


def rglru_mixer(xn, w_in, b_in, conv_w, conv_b, gx_w, gx_b, ga_w, ga_b, lam, w_out, b_out):
    B, S, _ = xn.shape
    proj = xn @ w_in + b_in
    gate, u = jnp.split(proj, 2, axis=-1)
    up = jnp.pad(u, ((0, 0), (CONV_LEFT, CONV_WIDTH - 1 - CONV_LEFT), (0, 0)))
    conv = up[:, 0:S] * conv_w[0]
    for k in range(1, CONV_WIDTH):
        conv = conv + up[:, k:k + S] * conv_w[k]
    uf = (conv + conv_b).astype(jnp.float32)
    ub = uf.reshape(B, S, LRU_HEADS, LRU_BLOCK)
    h_sum = jnp.zeros_like(uf)
    for d in range(2):
        i_t = jax.nn.sigmoid(jnp.einsum('bshi,hij->bshj', ub, gx_w[d].astype(jnp.float32))
                             + gx_b[d].astype(jnp.float32)).reshape(B, S, LRU_WIDTH)
        r_t = jax.nn.sigmoid(jnp.einsum('bshi,hij->bshj', ub, ga_w[d].astype(jnp.float32))
                             + ga_b[d].astype(jnp.float32)).reshape(B, S, LRU_WIDTH)
        log_a = -RG_C * r_t * jax.nn.softplus(-lam[d].astype(jnp.float32))
        a_t = jnp.exp(log_a)
        b_t = jnp.sqrt(-jnp.expm1(2.0 * log_a)) * (i_t * uf)
        _, h = lax.associative_scan(_lin_rec, (a_t, b_t), axis=1, reverse=(d == 1))
        h_sum = h_sum + h
    y = h_sum.astype(xn.dtype) * jax.nn.gelu(gate, approximate=True)
    return y @ w_out + b_out


def fourier_mixer(xn, w_in, w_out):
    B, S, D = xn.shape
    u = (xn @ w_in).reshape(B, S, FOURIER_GROUPS, FOURIER_GROUP_DIM).astype(jnp.float32)
    f = jnp.fft.fft2(u, axes=(1, 3), norm="ortho").real
    return f.reshape(B, S, D).astype(xn.dtype) @ w_out


def expert_choice_ffn(xn, w_router, w_gate, w_up, w_down):
    B, S, D = xn.shape
    cap = max(1, CAPACITY_FACTOR * S // N_EXPERTS)
    aff = jax.nn.softmax((xn @ w_router).astype(jnp.float32), axis=-1)
    gates, idx = lax.top_k(jnp.swapaxes(aff, 1, 2), cap)
    idx_flat = idx.reshape(B, N_EXPERTS * cap)
    xg = jnp.take_along_axis(xn, idx_flat[..., None], axis=1).reshape(B, N_EXPERTS, cap, D)
    hg = jax.nn.silu(jnp.einsum('becd,edf->becf', xg, w_gate)) * jnp.einsum('becd,edf->becf', xg, w_up)
    yg = jnp.einsum('becf,efd->becd', hg, w_down) * gates[..., None].astype(xn.dtype)
    out = jnp.zeros_like(xn).at[jnp.arange(B)[:, None], idx_flat].add(
        yg.reshape(B, N_EXPERTS * cap, D))
    return out


def setup_inputs(seed: int = 0) -> dict:
    key = jax.random.key(seed)
    ks = jax.random.split(key, 32)
    f32 = jnp.float32
    nrm = lambda k, shape, scale: jax.random.normal(k, shape, f32) * scale
    D, W = D_MODEL, LRU_WIDTH
    u_rad = jax.random.uniform(ks[10], (N_A, 2, W), f32, minval=0.9, maxval=0.999)
    s = u_rad ** (1.0 / RG_C)
    lam = jnp.log(s) - jnp.log1p(-s)
    return {
        "x": nrm(ks[0], (BATCH, SEQ, D), 1.0),
        "p": nrm(ks[1], (DEPTH, BATCH, SEQ, PLE_DIM), 1.0),
        "g_mix": 1.0 + nrm(ks[2], (DEPTH, D), 0.02),
        "g_ffn": 1.0 + nrm(ks[3], (DEPTH, D), 0.02),
        "g_ple": 1.0 + nrm(ks[4], (DEPTH, D), 0.02),
        "g_final": 1.0 + nrm(ks[5], (D,), 0.02),
        "rg_w_in": nrm(ks[6], (N_A, D, 2 * W), D ** -0.5),
        "rg_b_in": nrm(ks[7], (N_A, 2 * W), 0.01),
        "rg_conv_w": nrm(ks[8], (N_A, CONV_WIDTH, W), CONV_WIDTH ** -0.5),
        "rg_conv_b": nrm(ks[9], (N_A, W), 0.01),
        "rg_gx_w": nrm(ks[11], (N_A, 2, LRU_HEADS, LRU_BLOCK, LRU_BLOCK), LRU_BLOCK ** -0.5),
        "rg_gx_b": nrm(ks[12], (N_A, 2, LRU_HEADS, LRU_BLOCK), 0.01),
        "rg_ga_w": nrm(ks[13], (N_A, 2, LRU_HEADS, LRU_BLOCK, LRU_BLOCK), LRU_BLOCK ** -0.5),
        "rg_ga_b": nrm(ks[14], (N_A, 2, LRU_HEADS, LRU_BLOCK), 0.01),
        "rg_lam": lam,
        "rg_w_out": nrm(ks[15], (N_A, W, D), W ** -0.5),
        "rg_b_out": nrm(ks[16], (N_A, D), 0.01),
        "ft_w_in": nrm(ks[17], (N_B, D, D), D ** -0.5),
        "ft_w_out": nrm(ks[18], (N_B, D, D), D ** -0.5),
        "w_router": nrm(ks[19], (DEPTH, D, N_EXPERTS), D ** -0.5),
        "w_gate": nrm(ks[20], (DEPTH, N_EXPERTS, D, EXPERT_FF), D ** -0.5),
        "w_up": nrm(ks[21], (DEPTH, N_EXPERTS, D, EXPERT_FF), D ** -0.5),
        "w_down": nrm(ks[22], (DEPTH, N_EXPERTS, EXPERT_FF, D), EXPERT_FF ** -0.5),
        "ple_w_proj": nrm(ks[23], (DEPTH, PLE_DIM, D), PLE_DIM ** -0.5),
        "ple_w_gate": nrm(ks[24], (DEPTH, D, D), D ** -0.5),
    }


def reference(x, p, g_mix, g_ffn, g_ple, g_final, rg_w_in, rg_b_in, rg_conv_w, rg_conv_b,
              rg_gx_w, rg_gx_b, rg_ga_w, rg_ga_b, rg_lam, rg_w_out, rg_b_out,
              ft_w_in, ft_w_out, w_router, w_gate, w_up, w_down, ple_w_proj, ple_w_gate):
    h = x
    for i in range(DEPTH):
        xn = rms_norm(h, g_mix[i])
        j = i // N_MIXERS
        if i % N_MIXERS == 0:
            mix = rglru_mixer(xn, rg_w_in[j], rg_b_in[j], rg_conv_w[j], rg_conv_b[j],
                              rg_gx_w[j], rg_gx_b[j], rg_ga_w[j], rg_ga_b[j], rg_lam[j],
                              rg_w_out[j], rg_b_out[j])
        else:
            mix = fourier_mixer(xn, ft_w_in[j], ft_w_out[j])
        h = h + mix
        h = h + expert_choice_ffn(rms_norm(h, g_ffn[i]), w_router[i], w_gate[i], w_up[i], w_down[i])
        gate = jax.nn.sigmoid(rms_norm(h, g_ple[i]) @ ple_w_gate[i])
        h = h + gate * (p[i] @ ple_w_proj[i])
    return rms_norm(h, g_final)
```

```python
import math
import contextlib
import numpy as np
import concourse.bass as bass
import concourse.mybir as mybir
from concourse.bass_utils import run_bass_kernel_spmd

F32 = mybir.dt.float32
BF16 = mybir.dt.bfloat16
I32 = mybir.dt.int32
ALU = mybir.AluOpType
AF = mybir.ActivationFunctionType
AX = mybir.AxisListType

D = 1024
S = 4096
NE = 16
CAP = 512
FF = 2048
PLE = 256
EPS = 1e-6
TG = 512
NG = S // TG


class Res:
    def __init__(self, name):
        self.name = name
        self.w = None
        self.r = []
        self.ent = None
        self.d = None


class Op:
    __slots__ = ("eng", "fn", "deps", "dma", "sem", "val", "epoch", "has_dep", "phase")


class Prog:
    ENGS = ["sync", "scalar", "gpsimd", "vector", "tensor"]

    def __init__(self, nc, stack):
        self.nc = nc
        self.stack = stack
        self.ops = []
        self.epoch = 0
        self.esem = {}
        self.ecnt = {}
        self.dpool = []
        self.dnext = 0
        self.barrier = []
        self.phase = 0
        self.pstack = None
        self.used_ent = []

    def new_sem(self, name):
        return self.stack.enter_context(self.nc.semaphore(name))

    def begin_phase(self):
        self.phase += 1
        self.ops = []
        self.dnext = 0
        self.used_ent = []
        self.pstack = contextlib.ExitStack()

    def end_phase(self):
        self.emit()
        self.pstack.close()
        self.pstack = None

    def op(self, eng, fn, reads=(), writes=(), dma=None, ndma=1):
        o = Op()
        o.eng = eng
        o.fn = fn
        o.dma = dma
        o.epoch = self.epoch
        o.has_dep = False
        o.phase = self.phase
        deps = []
        reads = list(reads)
        writes = list(writes)
        if dma is not None:
            if dma.d is None:
                dma.d = Res(dma.name + ".d")
            writes.append(dma.d)
        for r in reads:
            if r.w is not None and r.w.phase == self.phase:
                deps.append(r.w)
        for w in writes:
            if w.w is not None and w.w.phase == self.phase:
                deps.append(w.w)
            for x in w.r:
                if x.phase == self.phase:
                    deps.append(x)
        for r in reads:
            r.r.append(o)
        for w in writes:
            w.w = o
            w.r = []
        o.deps = [d for d in deps if d is not o]
        if dma is not None:
            if dma.ent is None or dma.ent[2] != self.phase:
                if self.dnext >= len(self.dpool):
                    self.dpool.append([self.new_sem("dp%d" % len(self.dpool)), 0])
                pe = self.dpool[self.dnext]
                self.dnext += 1
                dma.ent = [pe, None, self.phase]
                self.used_ent.append(pe)
            pe = dma.ent[0]
            pe[1] += 16 * ndma
            o.sem = pe[0]
            o.val = pe[1]
        else:
            o.sem = None
            o.val = None
        self.ops.append(o)
        return o

    def emit(self):
        nc = self.nc
        ops = self.ops
        for o in ops:
            for x in o.deps:
                if x.dma is None and x.eng == "tensor" and o.eng == "tensor" and o.dma is None:
                    continue
                x.has_dep = True
        per_eng = {e: [o for o in ops if o.eng == e] for e in self.ENGS}
        for e in self.ENGS:
            comp = [o for o in per_eng[e] if o.dma is None]
            if comp:
                comp[-1].has_dep = True
        for o in ops:
            if o.dma is None and o.has_dep:
                key = (o.eng, o.epoch)
                if key not in self.esem:
                    self.esem[key] = self.new_sem("e_%s_%d" % key)
                    self.ecnt[key] = 0
                self.ecnt[key] += 1
                o.sem = self.esem[key]
                o.val = self.ecnt[key]
        barrier_in = list(self.barrier)
        with nc.Block() as block:
            def run(eng_name):
                def body(eng):
                    waited = {}
                    for (sm, vl) in barrier_in:
                        eng.wait_ge(sm, vl)
                        waited[id(sm)] = vl
                    for o in per_eng[eng_name]:
                        for x in o.deps:
                            if x.sem is None:
                                continue
                            if x.dma is None and x.eng == "tensor" and eng_name == "tensor" and o.dma is None:
                                continue
                            k = id(x.sem)
                            if waited.get(k, 0) >= x.val:
                                continue
                            eng.wait_ge(x.sem, x.val)
                            waited[k] = x.val
                        if o.dma is not None:
                            o.fn(eng, o.sem)
                        else:
                            ins = o.fn(eng)
                            if o.has_dep:
                                ins.then_inc(o.sem, 1)
                return body
            block.sync(run("sync"))
            block.scalar(run("scalar"))
            block.gpsimd(run("gpsimd"))
            block.vector(run("vector"))
            block.tensor(run("tensor"))
        bar = {}
        for (sm, vl) in barrier_in:
            bar[id(sm)] = (sm, vl)
        for e in self.ENGS:
            comp = [o for o in per_eng[e] if o.dma is None]
            if comp:
                bar[id(comp[-1].sem)] = (comp[-1].sem, comp[-1].val)
        for pe in self.used_ent:
            bar[id(pe[0])] = (pe[0], pe[1])
        self.barrier = list(bar.values())


class T:
    def __init__(self, P, name, shape, dtype, psum=False, persist=False):
        nc = P.nc
        st = P.stack if (persist or psum) else P.pstack
        nm = name if (persist or psum) else "%s_p%d" % (name, P.phase)
        if psum:
            self.t = st.enter_context(nc.psum_tensor(nm, shape, dtype))
        else:
            self.t = st.enter_context(nc.sbuf_tensor(nm, shape, dtype))
        self.r = Res(nm)

    def __getitem__(self, k):
        return self.t[k]


def build(NB, layers, final=True, stop=None):
    nc = bass.Bass("TRN2", target_bir_lowering=False)
    NTOK = NB * S
    NL = 4

    def din(name, shape, dt=F32):
        return nc.dram_tensor(name, list(shape), dt, kind="ExternalInput").ap()

    x_in = din("x", [NTOK, D])
    p_in = din("p", [NL, NTOK, PLE])
    gvec = din("gvec", [13, 128, D])
    rg_w_in = din("rg_w_in", [2, D, 2 * D])
    rg_b_in = din("rg_b_in", [2, 128, 16])
    rg_conv = din("rg_conv", [2, 128, 5, 8])
    rg_gw = din("rg_gw", [2, 2, 2, 4, 256, 256])
    rg_gb = din("rg_gb", [2, 128, 2, 2, 8])
    rg_lam = din("rg_lam", [2, 128, 2, 8])
    rg_w_out = din("rg_w_out", [2, D, D])
    rg_b_out = din("rg_b_out", [2, 128, D])
    ft_w_in = din("ft_w_in", [2, D, D])
    ft_w_out = din("ft_w_out", [2, D, D])
    w_router = din("w_router", [NL, D, NE])
    NLW = len(layers)
    lidx = {li: i for i, li in enumerate(layers)}
    w_gate = din("w_gate", [NLW, NE, D, FF])
    w_up = din("w_up", [NLW, NE, D, FF])
    w_down = din("w_down", [NLW, NE, FF, D])
    ple_wp = din("ple_w_proj", [NL, PLE, D])
    ple_wg = din("ple_w_gate", [NL, D, D])
    cident = din("c_ident", [128, 128])
    ciota = din("c_iota", [128, 512])
    ctok = din("c_tok", [128, 32])
    cdft = din("c_dft", [2, S, S])
    ccd = din("c_cdft", [2, 256, 256])
    out_d = nc.dram_tensor("out", [NTOK, D], F32, kind="ExternalOutput").ap()

    HA = nc.dram_tensor("HA", [NB, S + 128, D], F32).ap()
    HB = nc.dram_tensor("HB", [NB, S + 128, D], F32).ap()
    XN2 = nc.dram_tensor("XN2", [NB, S, D], BF16).ap()
    UT = nc.dram_tensor("UT", [NB, D, S], F32).ap()
    GT = nc.dram_tensor("GT", [NB, D, S], BF16).ap()
    YT = nc.dram_tensor("YT", [NB, D, S], BF16).ap()
    UD = nc.dram_tensor("UD", [NB, S, D], BF16).ap()

    with contextlib.ExitStack() as stack:
        P = Prog(nc, stack)

        def sb(name, shape, dt=F32, persist=False):
            return T(P, name, shape, dt, persist=persist)

        def mm(out_ap, lhsT, rhs, start, stop, reads, writes):
            P.op("tensor", lambda e: e.matmul(out_ap, lhsT, rhs, start=start, stop=stop), reads=reads, writes=writes)

        def dma_in(tile, ap, eng="sync", reads=()):
            P.op(eng, lambda e, s: e.dma_start(out=tile[:], in_=ap).then_inc(s, 16), reads=list(reads), writes=[tile.r], dma=tile.r)

        ps = [T(P, "ps%d" % i, [128, 512], F32, psum=True) for i in range(7)]
        pbf = T(P, "pbf", [128, 1024], BF16, psum=True)

        ident_f = sb("ident_f", [128, 128], persist=True)
        ident_b = sb("ident_b", [128, 128], BF16, persist=True)
        iota = sb("iota", [128, 512], persist=True)
        tokid = sb("tokid", [128, 32], persist=True)
        ones16 = sb("ones16", [128, NE], persist=True)
        afftm = sb("afftm", [128, NB, 32, NE], persist=True)
        sinfo = sb("sinfo", [128, NB, NE, 4, 3], persist=True)
        sidx_g = sb("sidx_g", [128, NB, NE, 4], I32, persist=True)
        sidx_s = sb("sidx_s", [128, NB, NE, 4], I32, persist=True)

        hres = {}
        for nm in ("HA", "HB", "XN2", "UD"):
            for b in range(NB):
                for g in range(NG):
                    hres[(nm, b, g)] = Res("%s_%d_%d" % (nm, b, g))
        xres = Res("x_in")
        outres = Res("out")
        utres = [[Res("UT%d_%d" % (b, c)) for c in range(8)] for b in range(NB)]
        gtres = [[Res("GT%d_%d" % (b, c)) for c in range(8)] for b in range(NB)]
        ytres = [[Res("YT%d_%d" % (b, c)) for c in range(8)] for b in range(NB)]
        HD = {"HA": HA, "HB": HB}

        def H_ap(buf, b, g):
            return HD[buf][b, g * TG:(g + 1) * TG, :].rearrange("(j p) d -> p j d", p=128)

        P.begin_phase()
        dma_in(ident_f, cident)
        dma_in(iota, ciota)
        dma_in(tokid, ctok)
        P.op("vector", lambda e: e.tensor_copy(out=ident_b[:], in_=ident_f[:]), reads=[ident_f.r], writes=[ident_b.r])
        P.op("vector", lambda e: e.memset(ones16[:], 1.0), writes=[ones16.r])
        P.end_phase()

        class NormCtx:
            def __init__(self, gi, nh=2, want_o=True):
                self.hbuf = [sb("hbuf%d" % i, [128, 4, D]) for i in range(nh)]
                self.nh = nh
                if gi is not None:
                    self.xnb = sb("xnb", [128, 4, D], BF16)
                    self.xnT = [sb("xnT%d" % i, [128, 8, TG], BF16) for i in range(2)]
                    self.gv = sb("gv", [128, D])
                    dma_in(self.gv, gvec[gi])
                self.stat = [sb("stat%d" % i, [128, 8]) for i in range(nh)]
                self.sqj = sb("sqj", [128, D])
                if want_o:
                    self.obuf = sb("obuf", [128, 4, D])

            def load_h(self, src, b, g, k):
                hb = self.hbuf[k % self.nh]
                if src == "x":
                    ap = x_in[b * S + g * TG: b * S + (g + 1) * TG, :].rearrange("(j p) d -> p j d", p=128)
                    rd = [xres]
                else:
                    ap = H_ap(src, b, g)
                    rd = [hres[(src, b, g)]]
                dma_in(hb, ap, reads=rd)
                return hb

            def stats(self, k):
                hb, st, sqj = self.hbuf[k % self.nh], self.stat[k % self.nh], self.sqj
                for j in range(4):
                    P.op("scalar", lambda e, j=j: e.activation(out=sqj[:], in_=hb[:, j, :], func=AF.Square, accum_out=st[:, j:j + 1]),
                         reads=[hb.r], writes=[sqj.r, st.r])
                P.op("scalar", lambda e: e.activation(out=st[:, 4:8], in_=st[:, 0:4], func=AF.Sqrt, bias=EPS, scale=1.0 / D),
                     reads=[st.r], writes=[st.r])
                P.op("vector", lambda e: e.reciprocal(out=st[:, 4:8], in_=st[:, 4:8]), reads=[st.r], writes=[st.r])
                return st

            def norm(self, k):
                hb, xb, xt, gv = self.hbuf[k % self.nh], self.xnb, self.xnT[k % 2], self.gv
                st = self.stats(k)
                for j in range(4):
                    P.op("vector", lambda e, j=j: e.scalar_tensor_tensor(out=xb[:, j, :], in0=hb[:, j, :], scalar=st[:, 4 + j:5 + j],
                                                                        in1=gv[:], op0=ALU.mult, op1=ALU.mult),
                         reads=[hb.r, st.r, gv.r], writes=[xb.r])
                for j in range(4):
                    for c in range(8):
                        P.op("tensor", lambda e, j=j, c=c: e.transpose(out=pbf[:, c * 128:(c + 1) * 128], in_=xb[:, j, c * 128:(c + 1) * 128],
                                                                      identity=ident_b[:]),
                             reads=[xb.r, ident_b.r], writes=[pbf.r])
                    P.op("vector", lambda e, j=j: e.tensor_copy(out=xt[:, :, j * 128:(j + 1) * 128],
                                                               in_=pbf[:, :].rearrange("p (c t) -> p c t", c=8)),
                         reads=[pbf.r], writes=[xt.r])
                return xb, xt

            def store_o(self, dst, b, g, extra_reads=()):
                ob = self.obuf
                if dst == "out":
                    ap = out_d[b * S + g * TG: b * S + (g + 1) * TG, :].rearrange("(j p) d -> p j d", p=128)
                    wr = [outres]
                else:
                    ap = H_ap(dst, b, g)
                    wr = [hres[(dst, b, g)]]
                P.op("sync", lambda e, s: e.dma_start(out=ap, in_=ob[:]).then_inc(s, 16), reads=[ob.r], writes=wr, dma=ob.r)

        def load_w_bf16(tile, ap, parts=1):
            kc = tile.t.shape[1]
            step = max(1, kc // parts)
            rng = list(range(0, kc, step))

            def fn(e, s):
                for c0 in rng:
                    e.dma_start(out=tile[:, c0:c0 + step, :], in_=ap[c0 * 128:(c0 + step) * 128, :].rearrange("(c p) f -> p c f", p=128)).then_inc(s, 16)
            P.op("gpsimd", fn, writes=[tile.r], dma=tile.r, ndma=len(rng))

        def rg_layer(li, j, src, dst):
            P.begin_phase()
            N = NormCtx(li, want_o=False)
            wA = sb("wA", [128, 8, 2048], BF16)
            load_w_bf16(wA, rg_w_in[j], parts=4)
            bin_ = sb("rgbin", [128, 16])
            dma_in(bin_, rg_b_in[j])
            fm_f = [sb("rgfmf%d" % i, [128, TG]) for i in range(2)]
            fm_t = [sb("rgfmt%d" % i, [128, TG]) for i in range(2)]
            fm_g = [sb("rgfmg%d" % i, [128, TG], BF16) for i in range(2)]
            for b in range(NB):
                for g in range(NG):
                    N.load_h(src, b, g, g)
                    xb, xt = N.norm(g)
                    for fc in range(16):
                        pt = ps[fc % 4]
                        for kc in range(8):
                            mm(pt[:, :], wA[:, kc, fc * 128:(fc + 1) * 128], xt[:, kc, :], kc == 0, kc == 7, [wA.r, xt.r], [pt.r])
                        q = fc % 2
                        xg = fm_f[q]
                        P.op("scalar", lambda e, pt=pt, xg=xg, fc=fc: e.activation(out=xg[:], in_=pt[:, :], func=AF.Identity, bias=bin_[:, fc:fc + 1], scale=1.0),
                             reads=[pt.r, bin_.r], writes=[xg.r])
                        if fc < 8:
                            tt, gg = fm_t[q], fm_g[q]
                            P.op("vector", lambda e, xg=xg, tt=tt: e.tensor_tensor(out=tt[:], in0=xg[:], in1=xg[:], op=ALU.mult), reads=[xg.r], writes=[tt.r])
                            P.op("vector", lambda e, tt=tt: e.tensor_scalar(out=tt[:], in0=tt[:], scalar1=0.044715, scalar2=1.0, op0=ALU.mult, op1=ALU.add),
                                 reads=[tt.r], writes=[tt.r])
                            P.op("vector", lambda e, xg=xg, tt=tt: e.tensor_tensor(out=tt[:], in0=tt[:], in1=xg[:], op=ALU.mult), reads=[xg.r, tt.r], writes=[tt.r])
                            P.op("scalar", lambda e, tt=tt: e.activation(out=tt[:], in_=tt[:], func=AF.Sigmoid, scale=1.5957691216057308),
                                 reads=[tt.r], writes=[tt.r])
                            P.op("vector", lambda e, xg=xg, tt=tt, gg=gg: e.tensor_tensor(out=gg[:], in0=tt[:], in1=xg[:], op=ALU.mult),
                                 reads=[xg.r, tt.r], writes=[gg.r])
                            P.op("sync", lambda e, s, gg=gg, fc=fc, g=g, b=b: e.dma_start(out=GT[b, fc * 128:(fc + 1) * 128, g * TG:(g + 1) * TG], in_=gg[:]).then_inc(s, 16),
                                 reads=[gg.r], writes=[gtres[b][fc]], dma=gg.r)
                        else:
                            c = fc - 8
                            P.op("sync", lambda e, s, xg=xg, c=c, g=g, b=b: e.dma_start(out=UT[b, c * 128:(c + 1) * 128, g * TG:(g + 1) * TG], in_=xg[:]).then_inc(s, 16),
                                 reads=[xg.r], writes=[utres[b][c]], dma=xg.r)
            P.end_phase()

            P.begin_phase()
            gw = sb("rggw", [128, 2, 2, 4, 2, 256], BF16)

            def fn(e, s):
                for d_ in range(2):
                    for kd in range(2):
                        for h in range(4):
                            e.dma_start(out=gw[:, d_, kd, h, :, :],
                                        in_=rg_gw[j, d_, kd, h].rearrange("(c p) o -> p c o", p=128)).then_inc(s, 16)
            P.op("gpsimd", fn, writes=[gw.r], dma=gw.r, ndma=16)
            cw = sb("rgcw", [128, 5, 8])
            dma_in(cw, rg_conv[j])
            gb = sb("rggb", [128, 2, 2, 8])
            dma_in(gb, rg_gb[j])
            lam = sb("rglam", [128, 2, 8])
            dma_in(lam, rg_lam[j])
            c8 = sb("rgc8", [128, 2, 8])
            P.op("scalar", lambda e: e.activation(out=c8[:], in_=lam[:], func=AF.Exp, scale=-1.0), reads=[lam.r], writes=[c8.r])
            P.op("scalar", lambda e: e.activation(out=c8[:], in_=c8[:], func=AF.Ln, bias=1.0, scale=1.0), reads=[c8.r], writes=[c8.r])
            P.op("vector", lambda e: e.tensor_scalar(out=c8[:], in0=c8[:], scalar1=-8.0, scalar2=None, op0=ALU.mult), reads=[c8.r], writes=[c8.r])
            ue = sb("rgue", [128, 2, S + 4])
            uc = sb("rguc", [128, 2, S])
            ucb = sb("rgucb", [128, 2, S], BF16)
            Aa = sb("rgA", [128, S])
            Bb = sb("rgB", [128, S])
            hs = [sb("rghs%d" % i, [128, S]) for i in range(2)]
            gtl = sb("rggt", [128, S], BF16)
            yt = sb("rgyt", [128, S], BF16)
            ti = [sb("rgti%d" % i, [128, TG]) for i in range(2)]
            tr = [sb("rgtr%d" % i, [128, TG]) for i in range(2)]
            P.op("vector", lambda e: e.memset(ue[:, :, 0:2], 0.0), writes=[ue.r])
            P.op("vector", lambda e: e.memset(ue[:, :, S + 2:S + 4], 0.0), writes=[ue.r])
            for b in range(NB):
                for h in range(4):
                    for cc in range(2):
                        c = 2 * h + cc
                        P.op("sync", lambda e, s, c=c, cc=cc, b=b: e.dma_start(out=ue[:, cc, 2:S + 2], in_=UT[b, c * 128:(c + 1) * 128, :]).then_inc(s, 16),
                             reads=[utres[b][c]], writes=[ue.r], dma=ue.r)
                    for cc in range(2):
                        c = 2 * h + cc
                        P.op("vector", lambda e, c=c, cc=cc: e.tensor_scalar(out=uc[:, cc, :], in0=ue[:, cc, 0:S], scalar1=cw[:, 0, c:c + 1], scalar2=cw[:, 4, c:c + 1],
                                                                            op0=ALU.mult, op1=ALU.add), reads=[ue.r, cw.r], writes=[uc.r])
                        for kk in range(1, 4):
                            P.op("vector", lambda e, c=c, cc=cc, kk=kk: e.scalar_tensor_tensor(out=uc[:, cc, :], in0=ue[:, cc, kk:S + kk], scalar=cw[:, kk, c:c + 1],
                                                                                              in1=uc[:, cc, :], op0=ALU.mult, op1=ALU.add),
                                 reads=[ue.r, cw.r, uc.r], writes=[uc.r])
                        P.op("scalar", lambda e, cc=cc: e.copy(out=ucb[:, cc, :], in_=uc[:, cc, :]), reads=[uc.r], writes=[ucb.r])
                    for cc in range(2):
                        c = 2 * h + cc
                        P.op("sync", lambda e, s, c=c, b=b: e.dma_start(out=gtl[:], in_=GT[b, c * 128:(c + 1) * 128, :]).then_inc(s, 16),
                             reads=[gtres[b][c]], writes=[gtl.r], dma=gtl.r)
                        for d_ in range(2):
                            hd = hs[d_]
                            for tb in range(S // TG):
                                q = tb % 2
                                sl = slice(tb * TG, (tb + 1) * TG)
                                pi, pr = ps[4 + 0], ps[4 + 1]
                                for kc in range(2):
                                    mm(pi[:, :], gw[:, d_, 0, h, kc, cc * 128:(cc + 1) * 128], ucb[:, kc, sl], kc == 0, kc == 1, [gw.r, ucb.r], [pi.r])
                                for kc in range(2):
                                    mm(pr[:, :], gw[:, d_, 1, h, kc, cc * 128:(cc + 1) * 128], ucb[:, kc, sl], kc == 0, kc == 1, [gw.r, ucb.r], [pr.r])
                                tti, ttr = ti[q], tr[q]
                                P.op("scalar", lambda e, pi=pi, tti=tti, d_=d_, c=c: e.activation(out=tti[:], in_=pi[:, :], func=AF.Sigmoid, bias=gb[:, d_, 0, c:c + 1], scale=1.0),
                                     reads=[pi.r, gb.r], writes=[tti.r])
                                P.op("scalar", lambda e, pr=pr, ttr=ttr, d_=d_, c=c: e.activation(out=ttr[:], in_=pr[:, :], func=AF.Sigmoid, bias=gb[:, d_, 1, c:c + 1], scale=1.0),
                                     reads=[pr.r, gb.r], writes=[ttr.r])
                                P.op("scalar", lambda e, ttr=ttr, d_=d_, c=c, sl=sl: e.activation(out=Aa[:, sl], in_=ttr[:], func=AF.Exp, scale=c8[:, d_, c:c + 1]),
                                     reads=[ttr.r, c8.r], writes=[Aa.r])
                                P.op("vector", lambda e, ttr=ttr, sl=sl: e.tensor_tensor(out=ttr[:], in0=Aa[:, sl], in1=Aa[:, sl], op=ALU.mult), reads=[Aa.r], writes=[ttr.r])
                                P.op("scalar", lambda e, ttr=ttr: e.activation(out=ttr[:], in_=ttr[:], func=AF.Sqrt, bias=1.0, scale=-1.0), reads=[ttr.r], writes=[ttr.r])
                                P.op("vector", lambda e, tti=tti, ttr=ttr: e.tensor_tensor(out=tti[:], in0=tti[:], in1=ttr[:], op=ALU.mult), reads=[tti.r, ttr.r], writes=[tti.r])
                                P.op("vector", lambda e, tti=tti, cc=cc, sl=sl: e.tensor_tensor(out=Bb[:, sl], in0=tti[:], in1=uc[:, cc, sl], op=ALU.mult),
                                     reads=[tti.r, uc.r], writes=[Bb.r])
                            if d_ == 0:
                                P.op("vector", lambda e, hd=hd: e.tensor_tensor_scan(out=hd[:], data0=Aa[:], data1=Bb[:], initial=0.0, op0=ALU.mult, op1=ALU.add),
                                     reads=[Aa.r, Bb.r], writes=[hd.r])
                            else:
                                P.op("vector", lambda e, hd=hd: e.tensor_tensor_scan(out=hd[:, ::-1], data0=Aa[:, ::-1], data1=Bb[:, ::-1], initial=0.0,
                                                                                    op0=ALU.mult, op1=ALU.add),
                                     reads=[Aa.r, Bb.r], writes=[hd.r])
                        P.op("vector", lambda e: e.tensor_tensor(out=hs[0][:], in0=hs[0][:], in1=hs[1][:], op=ALU.add), reads=[hs[0].r, hs[1].r], writes=[hs[0].r])
                        P.op("vector", lambda e: e.tensor_tensor(out=yt[:], in0=hs[0][:], in1=gtl[:], op=ALU.mult), reads=[hs[0].r, gtl.r], writes=[yt.r])
                        P.op("sync", lambda e, s, c=c, b=b: e.dma_start(out=YT[b, c * 128:(c + 1) * 128, :], in_=yt[:]).then_inc(s, 16),
                             reads=[yt.r], writes=[ytres[b][c]], dma=yt.r)
            P.end_phase()

            P.begin_phase()
            N = NormCtx(None)
            wout = sb("rgwout", [128, 8, D], BF16)
            load_w_bf16(wout, rg_w_out[j], parts=2)
            bout = sb("rgbout", [128, D])
            dma_in(bout, rg_b_out[j])
            ylt = [sb("rgyl%d" % i, [128, 8, 128], BF16) for i in range(2)]
            n = 0
            for b in range(NB):
                for g in range(NG):
                    hb = N.load_h(src, b, g, g)
                    ob = N.obuf
                    for jt in range(4):
                        yl = ylt[n % 2]
                        n += 1
                        t0 = g * TG + jt * 128
                        P.op("sync", lambda e, s, yl=yl, t0=t0, b=b: e.dma_start(out=yl[:], in_=YT[b, :, t0:t0 + 128].rearrange("(c p) t -> p c t", p=128)).then_inc(s, 16),
                             reads=ytres[b], writes=[yl.r], dma=yl.r)
                        for nb in range(2):
                            pt = ps[nb]
                            for kc in range(8):
                                mm(pt[:, :], yl[:, kc, :], wout[:, kc, nb * 512:(nb + 1) * 512], kc == 0, kc == 7, [yl.r, wout.r], [pt.r])
                            P.op("vector", lambda e, pt=pt, jt=jt, nb=nb: e.tensor_tensor(out=ob[:, jt, nb * 512:(nb + 1) * 512], in0=pt[:, :],
                                                                                      in1=bout[:, nb * 512:(nb + 1) * 512], op=ALU.add),
                                 reads=[pt.r, bout.r], writes=[ob.r])
                        P.op("vector", lambda e, jt=jt, hb=hb: e.tensor_tensor(out=ob[:, jt, :], in0=ob[:, jt, :], in1=hb[:, jt, :], op=ALU.add),
                             reads=[ob.r, hb.r], writes=[ob.r])
                    N.store_o(dst, b, g)
            P.end_phase()

        def ft_layer(li, j, src, dst):
            P.begin_phase()
            N = NormCtx(li, want_o=False)
            win = sb("ftwin", [128, 8, D], BF16)
            load_w_bf16(win, ft_w_in[j], parts=2)
            ub = [sb("ftub%d" % i, [128, 4, D], BF16) for i in range(2)]
            for b in range(NB):
                for g in range(NG):
                    N.load_h(src, b, g, g)
                    xb, xt = N.norm(g)
                    u = ub[g % 2]
                    for jt in range(4):
                        for nb in range(2):
                            pt = ps[nb]
                            for kc in range(8):
                                mm(pt[:, :], xt[:, kc, jt * 128:(jt + 1) * 128], win[:, kc, nb * 512:(nb + 1) * 512], kc == 0, kc == 7, [xt.r, win.r], [pt.r])
                            P.op("scalar", lambda e, pt=pt, u=u, jt=jt, nb=nb: e.copy(out=u[:, jt, nb * 512:(nb + 1) * 512], in_=pt[:, :]),
                                 reads=[pt.r], writes=[u.r])
                    P.op("sync", lambda e, s, u=u, b=b, g=g: e.dma_start(out=UD[b, g * TG:(g + 1) * TG, :].rearrange("(j p) d -> p j d", p=128), in_=u[:]).then_inc(s, 16),
                         reads=[u.r], writes=[hres[("UD", b, g)]], dma=u.r)
            P.end_phase()

            P.begin_phase()
            N = NormCtx(None)
            wout = sb("ftwout", [128, 8, D], BF16)
            load_w_bf16(wout, ft_w_out[j], parts=2)
            cdc = sb("ftcd", [128, 2, 2, 256], BF16)

            def fn(e, s):
                for q in range(2):
                    e.dma_start(out=cdc[:, q, :, :], in_=ccd[q].rearrange("(c p) o -> p c o", p=128)).then_inc(s, 16)
            P.op("gpsimd", fn, writes=[cdc.r], dma=cdc.r, ndma=2)
            U = sb("ftU", [128, 32, D], BF16)
            tab = [sb("fttab%d" % i, [128, 16, TG], BF16) for i in range(2)]
            YZ = sb("ftYZ", [128, 2, 8, TG], BF16)
            fT = sb("ftfT", [128, 8, TG], BF16)
            tabn = 0
            for b in range(NB):
                P.op("sync", lambda e, s, b=b: e.dma_start(out=U[:], in_=UD[b].rearrange("(c p) d -> p c d", p=128)).then_inc(s, 16),
                     reads=[hres[("UD", b, g)] for g in range(NG)], writes=[U.r], dma=U.r)
                for pb in range(NG):
                    for q in range(2):
                        for half in range(2):
                            for th in range(2):
                                tb = tab[tabn % 2]
                                tabn += 1

                                def fn(e, s, tb=tb, q=q, th=th, pb=pb):
                                    for c4 in range(4):
                                        r0 = (th * 16 + c4 * 4) * 128
                                        e.dma_start(out=tb[:, c4 * 4:(c4 + 1) * 4, :],
                                                    in_=cdft[q, r0:r0 + 512, pb * TG:(pb + 1) * TG].rearrange("(c p) n -> p c n", p=128)).then_inc(s, 16)
                                P.op("gpsimd", fn, writes=[tb.r], dma=tb.r, ndma=4)
                                for tc_ in range(16):
                                    tcg = th * 16 + tc_
                                    for c4 in range(4):
                                        ch = half * 4 + c4
                                        pt = ps[c4]
                                        mm(pt[:, :], U[:, tcg, ch * 128:(ch + 1) * 128], tb[:, tc_, :], tcg == 0, tcg == 31, [U.r, tb.r], [pt.r])
                            for c4 in range(4):
                                ch = half * 4 + c4
                                pt = ps[c4]
                                if c4 % 2:
                                    P.op("vector", lambda e, pt=pt, q=q, ch=ch: e.tensor_copy(out=YZ[:, q, ch, :], in_=pt[:, :]), reads=[pt.r], writes=[YZ.r])
                                else:
                                    P.op("scalar", lambda e, pt=pt, q=q, ch=ch: e.copy(out=YZ[:, q, ch, :], in_=pt[:, :]), reads=[pt.r], writes=[YZ.r])
                    for ch in range(8):
                        grp = ch // 2
                        pt = ps[4 + ch % 2]
                        n = 0
                        for q in range(2):
                            for kc in range(2):
                                mm(pt[:, :], cdc[:, q, kc, (ch % 2) * 128:(ch % 2 + 1) * 128], YZ[:, q, grp * 2 + kc, :], n == 0, n == 3, [cdc.r, YZ.r], [pt.r])
                                n += 1
                        P.op("vector", lambda e, pt=pt, ch=ch: e.tensor_scalar(out=fT[:, ch, :], in0=pt[:, :], scalar1=1.0 / 1024.0, scalar2=None, op0=ALU.mult),
                             reads=[pt.r], writes=[fT.r])
                    hb = N.load_h(src, b, pb, pb)
                    ob = N.obuf
                    for jt in range(4):
                        for nb in range(2):
                            pt = ps[nb]
                            for kc in range(8):
                                mm(pt[:, :], fT[:, kc, jt * 128:(jt + 1) * 128], wout[:, kc, nb * 512:(nb + 1) * 512], kc == 0, kc == 7, [fT.r, wout.r], [pt.r])
                            P.op("vector", lambda e, pt=pt, jt=jt, nb=nb, hb=hb: e.tensor_tensor(out=ob[:, jt, nb * 512:(nb + 1) * 512], in0=pt[:, :],
                                                                                             in1=hb[:, jt, nb * 512:(nb + 1) * 512], op=ALU.add),
                                 reads=[pt.r, hb.r], writes=[ob.r])
                    N.store_o(dst, b, pb)
            P.end_phase()

        def moe_layer(li, src, dst):
            P.begin_phase()
            N = NormCtx(4 + li, want_o=False)
            wr = sb("wr", [128, 8, NE])
            dma_in(wr, w_router[li].rearrange("(c p) n -> p c n", p=128))
            xnf = sb("xnf", [128, 4, D])
            xnTf = sb("xnTf", [128, 8, TG])
            sm = sb("smx", [128, 4, NE])
            ssum = sb("ssum", [128, 12])
            xbs = sb("xbs", [128, 4, D], BF16)
            for b in range(NB):
                for g in range(NG):
                    hb = N.load_h(src, b, g, g)
                    P.op("sync", lambda e, s, hb=hb, b=b, g=g: e.dma_start(out=H_ap(dst, b, g), in_=hb[:]).then_inc(s, 16),
                         reads=[hb.r], writes=[hres[(dst, b, g)]], dma=hb.r)
                    st = N.stats(g)
                    for jq in range(4):
                        P.op("vector", lambda e, jq=jq, hb=hb, st=st: e.scalar_tensor_tensor(out=xnf[:, jq, :], in0=hb[:, jq, :], scalar=st[:, 4 + jq:5 + jq],
                                                                                            in1=N.gv[:], op0=ALU.mult, op1=ALU.mult),
                             reads=[hb.r, st.r, N.gv.r], writes=[xnf.r])
                    P.op("scalar", lambda e: e.copy(out=xbs[:], in_=xnf[:]), reads=[xnf.r], writes=[xbs.r])
                    for jq in range(4):
                        for c0 in range(2):
                            ptf = ps[4 + c0]
                            for c in range(4):
                                P.op("tensor", lambda e, jq=jq, c0=c0, c=c, ptf=ptf: e.transpose(out=ptf[:, c * 128:(c + 1) * 128], in_=xnf[:, jq, (c0 * 4 + c) * 128:(c0 * 4 + c + 1) * 128],
                                                                                             identity=ident_f[:]),
                                     reads=[xnf.r, ident_f.r], writes=[ptf.r])
                            P.op("vector" if c0 else "scalar",
                                 (lambda e, jq=jq, c0=c0, ptf=ptf: e.tensor_copy(out=xnTf[:, c0 * 4:(c0 + 1) * 4, jq * 128:(jq + 1) * 128], in_=ptf[:, :].rearrange("p (c t) -> p c t", c=4))) if c0 else
                                 (lambda e, jq=jq, c0=c0, ptf=ptf: e.copy(out=xnTf[:, c0 * 4:(c0 + 1) * 4, jq * 128:(jq + 1) * 128], in_=ptf[:, :].rearrange("p (c t) -> p c t", c=4))),
                                 reads=[ptf.r], writes=[xnTf.r])
                    xt = xnTf
                    P.op("sync", lambda e, s, b=b, g=g: e.dma_start(out=XN2[b, g * TG:(g + 1) * TG, :].rearrange("(j p) d -> p j d", p=128), in_=xbs[:]).then_inc(s, 16),
                         reads=[xbs.r], writes=[hres[("XN2", b, g)]], dma=xbs.r)
                    pt = ps[6]
                    for jt in range(4):
                        for kc in range(8):
                            mm(pt[:, jt * NE:(jt + 1) * NE], xt[:, kc, jt * 128:(jt + 1) * 128], wr[:, kc, :], kc == 0, kc == 7, [xt.r, wr.r], [pt.r])
                    P.op("vector", lambda e, pt=pt: e.tensor_reduce(out=ssum[:, 0:4], in_=pt[:, 0:4 * NE].rearrange("p (j n) -> p j n", n=NE), axis=AX.X, op=ALU.max),
                         reads=[pt.r], writes=[ssum.r])
                    P.op("vector", lambda e: e.tensor_scalar(out=ssum[:, 0:4], in0=ssum[:, 0:4], scalar1=-1.0, scalar2=None, op0=ALU.mult), reads=[ssum.r], writes=[ssum.r])
                    for jt in range(4):
                        P.op("scalar", lambda e, pt=pt, jt=jt: e.activation(out=sm[:, jt, :], in_=pt[:, jt * NE:(jt + 1) * NE], func=AF.Exp, bias=ssum[:, jt:jt + 1], scale=1.0,
                                                                           accum_out=ssum[:, 4 + jt:5 + jt]),
                             reads=[pt.r, ssum.r], writes=[sm.r, ssum.r])
                    P.op("vector", lambda e: e.reciprocal(out=ssum[:, 8:12], in_=ssum[:, 4:8]), reads=[ssum.r], writes=[ssum.r])
                    for jt in range(4):
                        P.op("vector", lambda e, b=b, g=g, jt=jt: e.tensor_scalar(out=afftm[:, b, g * 4 + jt, :], in0=sm[:, jt, :], scalar1=ssum[:, 8 + jt:9 + jt], scalar2=None, op0=ALU.mult),
                             reads=[sm.r, ssum.r], writes=[afftm.r])
            P.end_phase()

            P.begin_phase()
            affT = sb("affT", [NE, S])
            msk = sb("msk", [NE, S])
            rnk = sb("rnk", [NE, S])
            onesr = sb("onesr", [NE, S])
            P.op("vector", lambda e: e.memset(onesr[:], 1.0), writes=[onesr.r])
            bs = sb("bs", [NE, 8])
            mrtm = sb("mrtm", [128, 2, 32, NE])
            sel = [sb("sel%d" % i, [128, CAP]) for i in range(2)]
            rhs3 = sb("rhs3", [128, 32, NE, 3])
            for b in range(NB):
                for c in range(32):
                    pt = ps[4 + c % 2]
                    P.op("tensor", lambda e, pt=pt, b=b, c=c: e.transpose(out=pt[0:NE, 0:128], in_=afftm[:, b, c, :], identity=ident_f[:]),
                         reads=[afftm.r, ident_f.r], writes=[pt.r])
                    P.op("scalar", lambda e, pt=pt, c=c: e.copy(out=affT[:, c * 128:(c + 1) * 128], in_=pt[0:NE, 0:128]), reads=[pt.r], writes=[affT.r])
                P.op("vector", lambda e: e.memset(bs[:, 0:1], 0.0), writes=[bs.r])
                P.op("vector", lambda e: e.memset(bs[:, 1:2], 1.0), writes=[bs.r])
                for it in range(30):
                    P.op("vector", lambda e: e.tensor_tensor(out=bs[:, 2:3], in0=bs[:, 0:1], in1=bs[:, 1:2], op=ALU.add), reads=[bs.r], writes=[bs.r])
                    P.op("vector", lambda e: e.tensor_scalar(out=bs[:, 2:3], in0=bs[:, 2:3], scalar1=0.5, scalar2=None, op0=ALU.mult), reads=[bs.r], writes=[bs.r])
                    P.op("vector", lambda e: e.tensor_scalar(out=msk[:], in0=affT[:], scalar1=bs[:, 2:3], scalar2=0.0, op0=ALU.is_ge, op1=ALU.add, accum_out=bs[:, 3:4]),
                         reads=[affT.r, bs.r], writes=[msk.r, bs.r])
                    P.op("vector", lambda e: e.tensor_scalar(out=bs[:, 4:5], in0=bs[:, 3:4], scalar1=float(CAP), scalar2=None, op0=ALU.is_ge), reads=[bs.r], writes=[bs.r])
                    P.op("vector", lambda e: e.tensor_tensor(out=bs[:, 5:6], in0=bs[:, 2:3], in1=bs[:, 0:1], op=ALU.subtract), reads=[bs.r], writes=[bs.r])
                    P.op("vector", lambda e: e.tensor_tensor(out=bs[:, 6:7], in0=bs[:, 1:2], in1=bs[:, 2:3], op=ALU.subtract), reads=[bs.r], writes=[bs.r])
                    P.op("vector", lambda e: e.scalar_tensor_tensor(out=bs[:, 0:1], in0=bs[:, 5:6], scalar=bs[:, 4:5], in1=bs[:, 0:1], op0=ALU.mult, op1=ALU.add),
                         reads=[bs.r], writes=[bs.r])
                    P.op("vector", lambda e: e.scalar_tensor_tensor(out=bs[:, 1:2], in0=bs[:, 6:7], scalar=bs[:, 4:5], in1=bs[:, 2:3], op0=ALU.mult, op1=ALU.add),
                         reads=[bs.r], writes=[bs.r])
                P.op("vector", lambda e: e.tensor_scalar(out=msk[:], in0=affT[:], scalar1=bs[:, 0:1], scalar2=None, op0=ALU.is_ge), reads=[affT.r, bs.r], writes=[msk.r])
                P.op("vector", lambda e: e.tensor_tensor_scan(out=rnk[:], data0=onesr[:], data1=msk[:], initial=0.0, op0=ALU.mult, op1=ALU.add),
                     reads=[onesr.r, msk.r], writes=[rnk.r])
                for wi, src_t in enumerate((msk, rnk)):
                    pt = ps[4 + wi]
                    for c in range(32):
                        P.op("tensor", lambda e, pt=pt, c=c, src_t=src_t: e.transpose(out=pt[:, c * NE:(c + 1) * NE], in_=src_t[:, c * 128:(c + 1) * 128], identity=ident_f[0:NE, 0:NE]),
                             reads=[src_t.r, ident_f.r], writes=[pt.r])
                    P.op("vector", lambda e, pt=pt, wi=wi: e.tensor_copy(out=mrtm[:, wi, :, :], in_=pt[:, :].rearrange("p (c n) -> p c n", n=NE)),
                         reads=[pt.r], writes=[mrtm.r])
                for c in range(32):
                    P.op("vector", lambda e, c=c: e.tensor_scalar(out=rhs3[:, c, :, 0], in0=ones16[:], scalar1=tokid[:, c:c + 1], scalar2=None, op0=ALU.mult),
                         reads=[ones16.r, tokid.r], writes=[rhs3.r])
                P.op("vector", lambda e, b=b: e.tensor_copy(out=rhs3[:, :, :, 1], in_=afftm[:, b, :, :]), reads=[afftm.r], writes=[rhs3.r])
                P.op("vector", lambda e: e.memset(rhs3[:, :, :, 2], 1.0), writes=[rhs3.r])
                for ex in range(NE):
                    for c in range(32):
                        sl_ = sel[c % 2]
                        P.op("vector", lambda e, sl_=sl_, c=c, ex=ex: e.tensor_scalar(out=sl_[:], in0=iota[:], scalar1=mrtm[:, 1, c, ex:ex + 1],
                                                                                    scalar2=mrtm[:, 0, c, ex:ex + 1], op0=ALU.is_equal, op1=ALU.mult),
                             reads=[iota.r, mrtm.r], writes=[sl_.r])
                        for sbk in range(4):
                            pt = ps[sbk]
                            mm(pt[:, 0:3], sl_[:, sbk * 128:(sbk + 1) * 128], rhs3[:, c, ex, :], c == 0, c == 31, [sl_.r, rhs3.r], [pt.r])
                    for sbk in range(4):
                        pt = ps[sbk]
                        P.op("vector", lambda e, pt=pt, b=b, ex=ex, sbk=sbk: e.tensor_copy(out=sinfo[:, b, ex, sbk, :], in_=pt[:, 0:3]),
                             reads=[pt.r], writes=[sinfo.r])
                P.op("vector", lambda e, b=b: e.tensor_scalar(out=sinfo[:, b, :, :, 0], in0=sinfo[:, b, :, :, 0], scalar1=float(b * S), scalar2=None, op0=ALU.add),
                     reads=[sinfo.r], writes=[sinfo.r])
                P.op("vector", lambda e, b=b: e.tensor_copy(out=sidx_g[:, b, :, :], in_=sinfo[:, b, :, :, 0]), reads=[sinfo.r], writes=[sidx_g.r])
                P.op("vector", lambda e, b=b: e.tensor_scalar(out=sinfo[:, b, :, :, 2], in0=sinfo[:, b, :, :, 2], scalar1=-float(S), scalar2=float(S + b * 128), op0=ALU.mult, op1=ALU.add),
                     reads=[sinfo.r], writes=[sinfo.r])
                P.op("vector", lambda e, b=b: e.tensor_tensor(out=sinfo[:, b, :, :, 2], in0=sinfo[:, b, :, :, 2], in1=sinfo[:, b, :, :, 0], op=ALU.add),
                     reads=[sinfo.r], writes=[sinfo.r])
                P.op("vector", lambda e, b=b: e.tensor_copy(out=sidx_s[:, b, :, :], in_=sinfo[:, b, :, :, 2]), reads=[sinfo.r], writes=[sidx_s.r])
            P.end_phase()

            P.begin_phase()
            wA = sb("wA", [128, 8, FF], BF16)
            wB = sb("wB", [128, 8, FF], BF16)
            wC = sb("wC", [128, 16, D], BF16)
            xg = [sb("xg%d" % i, [128, 4, D], BF16) for i in range(2)]
            xgT = sb("xgT", [128, 8, CAP], BF16)
            hg = sb("hg", [128, 16, CAP], BF16)
            sg = [sb("sg%d" % i, [128, CAP]) for i in range(2)]
            yb = [sb("yb%d" % i, [128, D]) for i in range(4)]
            allh_dst = {b: [hres[(dst, b, g)] for g in range(NG)] for b in range(NB)}
            allx = {b: [hres[("XN2", b, g)] for g in range(NG)] for b in range(NB)}
            cnt = 0
            for ex in range(NE):
                load_w_bf16(wA, w_gate[lidx[li], ex], parts=4)
                load_w_bf16(wB, w_up[lidx[li], ex], parts=4)
                load_w_bf16(wC, w_down[lidx[li], ex], parts=4)
                for b in range(NB):
                    xgk = xg[cnt % 2]
                    cnt += 1

                    def fn(e, s, xgk=xgk, b=b, ex=ex):
                        for sbk in range(4):
                            e.indirect_dma_start(out=xgk[:, sbk, :], out_offset=None, in_=XN2.rearrange("b s d -> (b s) d"),
                                                 in_offset=bass.IndirectOffsetOnAxis(ap=sidx_g[:, b, ex, sbk:sbk + 1], axis=0)).then_inc(s, 16)
                    P.op("gpsimd", fn, reads=allx[b] + [sidx_g.r], writes=[xgk.r], dma=xgk.r, ndma=4)
                    for sbk in range(4):
                        for c in range(8):
                            P.op("tensor", lambda e, sbk=sbk, c=c, xgk=xgk: e.transpose(out=pbf[:, c * 128:(c + 1) * 128], in_=xgk[:, sbk, c * 128:(c + 1) * 128], identity=ident_b[:]),
                                 reads=[xgk.r, ident_b.r], writes=[pbf.r])
                        P.op("vector", lambda e, sbk=sbk: e.tensor_copy(out=xgT[:, :, sbk * 128:(sbk + 1) * 128], in_=pbf[:, :].rearrange("p (c t) -> p c t", c=8)),
                             reads=[pbf.r], writes=[xgT.r])
                    for fc in range(16):
                        pg, pu = ps[(fc % 2) * 2], ps[(fc % 2) * 2 + 1]
                        for kc in range(8):
                            mm(pg[:, :], wA[:, kc, fc * 128:(fc + 1) * 128], xgT[:, kc, :], kc == 0, kc == 7, [wA.r, xgT.r], [pg.r])
                        for kc in range(8):
                            mm(pu[:, :], wB[:, kc, fc * 128:(fc + 1) * 128], xgT[:, kc, :], kc == 0, kc == 7, [wB.r, xgT.r], [pu.r])
                        s_ = sg[fc % 2]
                        P.op("scalar", lambda e, pg=pg, s_=s_: e.activation(out=s_[:], in_=pg[:, :], func=AF.Silu), reads=[pg.r], writes=[s_.r])
                        P.op("vector", lambda e, pu=pu, s_=s_, fc=fc: e.tensor_tensor(out=hg[:, fc, :], in0=s_[:], in1=pu[:, :], op=ALU.mult),
                             reads=[pu.r, s_.r], writes=[hg.r])
                    for sbk in range(4):
                        y = yb[sbk]
                        for nb in range(2):
                            pt = ps[4 + nb]
                            for fc in range(16):
                                mm(pt[:, :], hg[:, fc, sbk * 128:(sbk + 1) * 128], wC[:, fc, nb * 512:(nb + 1) * 512], fc == 0, fc == 15, [hg.r, wC.r], [pt.r])
                            P.op("vector", lambda e, pt=pt, y=y, b=b, ex=ex, sbk=sbk, nb=nb: e.tensor_scalar(out=y[:, nb * 512:(nb + 1) * 512], in0=pt[:, :],
                                                                                                         scalar1=sinfo[:, b, ex, sbk, 1:2], scalar2=None, op0=ALU.mult),
                                 reads=[pt.r, sinfo.r], writes=[y.r])
                    hdst = HD[dst]
                    for sbk in range(4):
                        y = yb[sbk]
                        P.op("gpsimd", lambda e, s, y=y, b=b, ex=ex, sbk=sbk: e.indirect_dma_start(
                            out=hdst.rearrange("b s d -> (b s) d"), out_offset=bass.IndirectOffsetOnAxis(ap=sidx_s[:, b, ex, sbk:sbk + 1], axis=0),
                            in_=y[:], in_offset=None, compute_op=ALU.add).then_inc(s, 16),
                            reads=[y.r, sidx_s.r] + (allh_dst[b] if sbk > 0 else []), writes=allh_dst[b] if sbk == 0 else [], dma=y.r)
                    P.op("gpsimd", lambda e: e.nop(), reads=[yb[i].r.d for i in range(4)], writes=allh_dst[b])
            P.end_phase()

        def ple_layer(li, buf):
            P.begin_phase()
            N = NormCtx(8 + li)
            wg = sb("plewg", [128, 8, D], BF16)
            wp = sb("plewp", [128, 2, D], BF16)
            load_w_bf16(wg, ple_wg[li], parts=2)
            load_w_bf16(wp, ple_wp[li], parts=1)
            pl = [sb("plp%d" % i, [128, 4, PLE]) for i in range(2)]
            plb = sb("plpb", [128, 4, PLE], BF16)
            plT = sb("plpT", [128, 2, TG], BF16)
            gsb = [sb("plg%d" % i, [128, 512]) for i in range(2)]
            for b in range(NB):
                for g in range(NG):
                    hb = N.load_h(buf, b, g, g)
                    pk = pl[g % 2]
                    dma_in(pk, p_in[li, b * S + g * TG: b * S + (g + 1) * TG, :].rearrange("(j p) d -> p j d", p=128))
                    xb, xt = N.norm(g)
                    ob = N.obuf
                    P.op("vector", lambda e, pk=pk: e.tensor_copy(out=plb[:], in_=pk[:]), reads=[pk.r], writes=[plb.r])
                    for jt in range(4):
                        for c in range(2):
                            P.op("tensor", lambda e, jt=jt, c=c: e.transpose(out=pbf[:, c * 128:(c + 1) * 128], in_=plb[:, jt, c * 128:(c + 1) * 128], identity=ident_b[:]),
                                 reads=[plb.r, ident_b.r], writes=[pbf.r])
                        P.op("vector", lambda e, jt=jt: e.tensor_copy(out=plT[:, :, jt * 128:(jt + 1) * 128], in_=pbf[:, 0:256].rearrange("p (c t) -> p c t", c=2)),
                             reads=[pbf.r], writes=[plT.r])
                    for jt in range(4):
                        for nb in range(2):
                            pgt, ppt = ps[nb * 2], ps[nb * 2 + 1]
                            for kc in range(8):
                                mm(pgt[:, :], xt[:, kc, jt * 128:(jt + 1) * 128], wg[:, kc, nb * 512:(nb + 1) * 512], kc == 0, kc == 7, [xt.r, wg.r], [pgt.r])
                            for kc in range(2):
                                mm(ppt[:, :], plT[:, kc, jt * 128:(jt + 1) * 128], wp[:, kc, nb * 512:(nb + 1) * 512], kc == 0, kc == 1, [plT.r, wp.r], [ppt.r])
                            gs = gsb[nb]
                            P.op("scalar", lambda e, pgt=pgt, gs=gs: e.activation(out=gs[:], in_=pgt[:, :], func=AF.Sigmoid), reads=[pgt.r], writes=[gs.r])
                            P.op("vector", lambda e, ppt=ppt, gs=gs: e.tensor_tensor(out=gs[:], in0=gs[:], in1=ppt[:, :], op=ALU.mult), reads=[ppt.r, gs.r], writes=[gs.r])
                            P.op("vector", lambda e, gs=gs, jt=jt, nb=nb, hb=hb: e.tensor_tensor(out=ob[:, jt, nb * 512:(nb + 1) * 512], in0=gs[:],
                                                                                             in1=hb[:, jt, nb * 512:(nb + 1) * 512], op=ALU.add),
                                 reads=[gs.r, hb.r], writes=[ob.r])
                    N.store_o(buf, b, g)
            P.end_phase()

        def final_norm(buf):
            P.begin_phase()
            N = NormCtx(None)
            gv = sb("gvf", [128, D])
            dma_in(gv, gvec[12])
            for b in range(NB):
                for g in range(NG):
                    hb = N.load_h(buf, b, g, g)
                    st = N.stats(g)
                    ob = N.obuf
                    for j in range(4):
                        P.op("vector", lambda e, j=j, hb=hb, st=st: e.scalar_tensor_tensor(out=ob[:, j, :], in0=hb[:, j, :], scalar=st[:, 4 + j:5 + j], in1=gv[:],
                                                                                          op0=ALU.mult, op1=ALU.mult), reads=[hb.r, st.r, gv.r], writes=[ob.r])
                    N.store_o("out", b, g)
            P.end_phase()

        cur = "x"
        for li in layers:
            P.epoch = li
            j = li // 2
            if li % 2 == 0:
                rg_layer(li, j, cur, "HB")
            else:
                ft_layer(li, j, cur, "HB")
            cur = "HB"
            if stop == "mix" and li == layers[-1]:
                break
            moe_layer(li, "HB", "HA")
            cur = "HA"
            if stop == "moe" and li == layers[-1]:
                break
            ple_layer(li, "HA")
        P.epoch = 4
        if final:
            final_norm(cur)
        else:
            P.begin_phase()
            N = NormCtx(None)
            for b in range(NB):
                for g in range(NG):
                    hb = N.load_h(cur, b, g, g)
                    P.op("vector", lambda e, hb=hb: e.tensor_copy(out=N.obuf[:], in_=hb[:]), reads=[hb.r], writes=[N.obuf.r])
                    N.store_o("out", b, g)
            P.end_phase()
        P.begin_phase()
        P.op("sync", lambda e: e.nop(), reads=[], writes=[])
        P.end_phase()
    return nc


def _consts():
    ident = np.eye(128, dtype=np.float32)
    iota = np.tile(np.arange(1, 513, dtype=np.float32)[None, :], (128, 1))
    tok = (np.arange(32)[None, :] * 128 + np.arange(128)[:, None]).astype(np.float32)
    n = np.arange(S, dtype=np.int64)
    m = (n[:, None] * n[None, :]) % S
    ang = 2.0 * np.pi * m.astype(np.float64) / S
    dft = np.stack([np.cos(ang), -np.sin(ang)]).astype(np.float32)
    c = np.arange(256, dtype=np.int64)
    mc = (c[:, None] * c[None, :]) % 256
    angc = 2.0 * np.pi * mc.astype(np.float64) / 256
    cd = np.stack([np.cos(angc), np.sin(angc)]).astype(np.float32)
    return dict(c_ident=ident, c_iota=iota, c_tok=tok, c_dft=dft, c_cdft=cd)


def _prep(inputs, NB, layers=(0, 1, 2, 3)):
    f = lambda a: np.ascontiguousarray(np.asarray(a, dtype=np.float32))
    m = {}
    m["x"] = f(inputs["x"])[:NB].reshape(NB * S, D)
    m["p"] = f(inputs["p"])[:, :NB].reshape(4, NB * S, PLE)
    gvv = np.concatenate([f(inputs["g_mix"]), f(inputs["g_ffn"]), f(inputs["g_ple"]), f(inputs["g_final"])[None, :]], axis=0)
    m["gvec"] = np.ascontiguousarray(np.broadcast_to(gvv[:, None, :], (13, 128, D)))
    m["rg_w_in"] = f(inputs["rg_w_in"])
    m["rg_b_in"] = np.ascontiguousarray(f(inputs["rg_b_in"]).reshape(2, 16, 128).transpose(0, 2, 1))
    conv = np.concatenate([f(inputs["rg_conv_w"]), f(inputs["rg_conv_b"])[:, None, :]], axis=1)
    m["rg_conv"] = np.ascontiguousarray(conv.reshape(2, 5, 8, 128).transpose(0, 3, 1, 2))
    gw = np.stack([f(inputs["rg_gx_w"]), f(inputs["rg_ga_w"])], axis=2)
    m["rg_gw"] = np.ascontiguousarray(gw)
    gb = np.stack([f(inputs["rg_gx_b"]), f(inputs["rg_ga_b"])], axis=2)
    m["rg_gb"] = np.ascontiguousarray(gb.reshape(2, 2, 2, 8, 128).transpose(0, 4, 1, 2, 3))
    m["rg_lam"] = np.ascontiguousarray(f(inputs["rg_lam"]).reshape(2, 2, 8, 128).transpose(0, 3, 1, 2))
    m["rg_w_out"] = f(inputs["rg_w_out"])
    m["rg_b_out"] = np.ascontiguousarray(np.broadcast_to(f(inputs["rg_b_out"])[:, None, :], (2, 128, D)))
    for k in ("ft_w_in", "ft_w_out", "w_router", "ple_w_proj", "ple_w_gate"):
        m[k] = f(inputs[k])
    for k in ("w_gate", "w_up", "w_down"):
        m[k] = np.ascontiguousarray(np.asarray(inputs[k], dtype=np.float32)[list(layers)])
    m.update(_consts())
    return m


def kernel(**inputs):
    NB = 1
    layers = (0, 1, 2, 3)
    nc = build(NB, list(layers), final=True)
    base = _prep({**inputs, "x": np.asarray(inputs["x"])[0:1], "p": np.asarray(inputs["p"])[:, 0:1]}, NB, layers)
    in_maps = []
    for b in range(4):
        m = dict(base)
        m["x"] = np.ascontiguousarray(np.asarray(inputs["x"], dtype=np.float32)[b]).reshape(S, D)
        m["p"] = np.ascontiguousarray(np.asarray(inputs["p"], dtype=np.float32)[:, b]).reshape(4, S, PLE)
        in_maps.append(m)
    res = run_bass_kernel_spmd(nc, in_maps, core_ids=list(range(4)))
    out = np.stack([np.asarray(res.results[b]["out"], dtype=np.float32).reshape(S, D) for b in range(4)], axis=0)
    return out
```

```python
import math
import contextlib
import numpy as np
import concourse.bass as bass
import concourse.mybir as mybir
from concourse.bass_utils import run_bass_kernel_spmd

F32 = mybir.dt.float32
BF16 = mybir.dt.bfloat16
I32 = mybir.dt.int32
ALU = mybir.AluOpType
AF = mybir.ActivationFunctionType
AX = mybir.AxisListType

D = 1024
S = 4096
NE = 16
CAP = 512
FF = 2048
PLE = 256
EPS = 1e-6
TG = 512
NG = S // TG


class Res:
    def __init__(self, name):
        self.name = name
        self.w = None
        self.r = []
        self.ent = None
        self.d = None


class Op:
    __slots__ = ("eng", "fn", "deps", "dma", "sem", "val", "epoch", "has_dep", "phase")


class Prog:
    ENGS = ["sync", "scalar", "gpsimd", "vector", "tensor"]

    def __init__(self, nc, stack):
        self.nc = nc
        self.stack = stack
        self.ops = []
        self.epoch = 0
        self.esem = {}
        self.ecnt = {}
        self.dpool = []
        self.dnext = 0
        self.barrier = []
        self.phase = 0
        self.pstack = None
        self.used_ent = []

    def new_sem(self, name):
        return self.stack.enter_context(self.nc.semaphore(name))

    def begin_phase(self):
        self.phase += 1
        self.ops = []
        self.dnext = 0
        self.used_ent = []
        self.pstack = contextlib.ExitStack()

    def end_phase(self):
        self.emit()
        self.pstack.close()
        self.pstack = None

    def op(self, eng, fn, reads=(), writes=(), dma=None, ndma=1):
        o = Op()
        o.eng = eng
        o.fn = fn
        o.dma = dma
        o.epoch = self.epoch
        o.has_dep = False
        o.phase = self.phase
        deps = []
        reads = list(reads)
        writes = list(writes)
        if dma is not None:
            if dma.d is None:
                dma.d = Res(dma.name + ".d")
            writes.append(dma.d)
        for r in reads:
            if r.w is not None and r.w.phase == self.phase:
                deps.append(r.w)
        for w in writes:
            if w.w is not None and w.w.phase == self.phase:
                deps.append(w.w)
            for x in w.r:
                if x.phase == self.phase:
                    deps.append(x)
        for r in reads:
            r.r.append(o)
        for w in writes:
            w.w = o
            w.r = []
        o.deps = [d for d in deps if d is not o]
        if dma is not None:
            if dma.ent is None or dma.ent[2] != self.phase:
                if self.dnext >= len(self.dpool):
                    self.dpool.append([self.new_sem("dp%d" % len(self.dpool)), 0])
                pe = self.dpool[self.dnext]
                self.dnext += 1
                dma.ent = [pe, None, self.phase]
                self.used_ent.append(pe)
            pe = dma.ent[0]
            pe[1] += 16 * ndma
            o.sem = pe[0]
            o.val = pe[1]
        else:
            o.sem = None
            o.val = None
        self.ops.append(o)
        return o

    def emit(self):
        nc = self.nc
        ops = self.ops
        for o in ops:
            for x in o.deps:
                if x.dma is None and x.eng == "tensor" and o.eng == "tensor" and o.dma is None:
                    continue
                x.has_dep = True
        per_eng = {e: [o for o in ops if o.eng == e] for e in self.ENGS}
        for e in self.ENGS:
            comp = [o for o in per_eng[e] if o.dma is None]
            if comp:
                comp[-1].has_dep = True
        for o in ops:
            if o.dma is None and o.has_dep:
                key = (o.eng, o.epoch)
                if key not in self.esem:
                    self.esem[key] = self.new_sem("e_%s_%d" % key)
                    self.ecnt[key] = 0
                self.ecnt[key] += 1
                o.sem = self.esem[key]
                o.val = self.ecnt[key]
        barrier_in = list(self.barrier)
        with nc.Block() as block:
            def run(eng_name):
                def body(eng):
                    waited = {}
                    for (sm, vl) in barrier_in:
                        eng.wait_ge(sm, vl)
                        waited[id(sm)] = vl
                    for o in per_eng[eng_name]:
                        for x in o.deps:
                            if x.sem is None:
                                continue
                            if x.dma is None and x.eng == "tensor" and eng_name == "tensor" and o.dma is None:
                                continue
                            k = id(x.sem)
                            if waited.get(k, 0) >= x.val:
                                continue
                            eng.wait_ge(x.sem, x.val)
                            waited[k] = x.val
                        if o.dma is not None:
                            o.fn(eng, o.sem)
                        else:
                            ins = o.fn(eng)
                            if o.has_dep:
                                ins.then_inc(o.sem, 1)
                return body
            block.sync(run("sync"))
            block.scalar(run("scalar"))
            block.gpsimd(run("gpsimd"))
            block.vector(run("vector"))
            block.tensor(run("tensor"))
        bar = {}
        for (sm, vl) in barrier_in:
            bar[id(sm)] = (sm, vl)
        for e in self.ENGS:
            comp = [o for o in per_eng[e] if o.dma is None]
            if comp:
                bar[id(comp[-1].sem)] = (comp[-1].sem, comp[-1].val)
        for pe in self.used_ent:
            bar[id(pe[0])] = (pe[0], pe[1])
        self.barrier = list(bar.values())


class T:
    def __init__(self, P, name, shape, dtype, psum=False, persist=False):
        nc = P.nc
        st = P.stack if (persist or psum) else P.pstack
        nm = name if (persist or psum) else "%s_p%d" % (name, P.phase)
        if psum:
            self.t = st.enter_context(nc.psum_tensor(nm, shape, dtype))
        else:
            self.t = st.enter_context(nc.sbuf_tensor(nm, shape, dtype))
        self.r = Res(nm)

    def __getitem__(self, k):
        return self.t[k]


def build(NB, layers, final=True, stop=None):
    nc = bass.Bass("TRN2", target_bir_lowering=False)
    NTOK = NB * S
    NL = 4

    def din(name, shape, dt=F32):
        return nc.dram_tensor(name, list(shape), dt, kind="ExternalInput").ap()

    x_in = din("x", [NTOK, D])
    p_in = din("p", [NL, NTOK, PLE])
    gvec = din("gvec", [13, 128, D])
    rg_w_in = din("rg_w_in", [2, D, 2 * D])
    rg_b_in = din("rg_b_in", [2, 128, 16])
    rg_conv = din("rg_conv", [2, 128, 5, 8])
    rg_gw = din("rg_gw", [2, 2, 2, 4, 256, 256])
    rg_gb = din("rg_gb", [2, 128, 2, 2, 8])
    rg_lam = din("rg_lam", [2, 128, 2, 8])
    rg_w_out = din("rg_w_out", [2, D, D])
    rg_b_out = din("rg_b_out", [2, 128, D])
    ft_w_in = din("ft_w_in", [2, D, D])
    ft_w_out = din("ft_w_out", [2, D, D])
    w_router = din("w_router", [NL, D, NE])
    NLW = len(layers)
    lidx = {li: i for i, li in enumerate(layers)}
    w_gate = din("w_gate", [NLW, NE, D, FF])
    w_up = din("w_up", [NLW, NE, D, FF])
    w_down = din("w_down", [NLW, NE, FF, D])
    ple_wp = din("ple_w_proj", [NL, PLE, D])
    ple_wg = din("ple_w_gate", [NL, D, D])
    cident = din("c_ident", [128, 128])
    ciota = din("c_iota", [128, 512])
    ctok = din("c_tok", [128, 32])
    cdft = din("c_dft", [2, S, S])
    ccd = din("c_cdft", [2, 256, 256])
    out_d = nc.dram_tensor("out", [NTOK, D], F32, kind="ExternalOutput").ap()

    HA = nc.dram_tensor("HA", [NB, S + 128, D], F32).ap()
    HB = nc.dram_tensor("HB", [NB, S + 128, D], F32).ap()
    XN2 = nc.dram_tensor("XN2", [NB, S, D], BF16).ap()
    UT = nc.dram_tensor("UT", [NB, D, S], F32).ap()
    GT = nc.dram_tensor("GT", [NB, D, S], BF16).ap()
    YT = nc.dram_tensor("YT", [NB, D, S], BF16).ap()
    UD = nc.dram_tensor("UD", [NB, S, D], BF16).ap()

    with contextlib.ExitStack() as stack:
        P = Prog(nc, stack)

        def sb(name, shape, dt=F32, persist=False):
            return T(P, name, shape, dt, persist=persist)

        def mm(out_ap, lhsT, rhs, start, stop, reads, writes):
            P.op("tensor", lambda e: e.matmul(out_ap, lhsT, rhs, start=start, stop=stop), reads=reads, writes=writes)

        def dma_in(tile, ap, eng="sync", reads=()):
            P.op(eng, lambda e, s: e.dma_start(out=tile[:], in_=ap).then_inc(s, 16), reads=list(reads), writes=[tile.r], dma=tile.r)

        ps = [T(P, "ps%d" % i, [128, 512], F32, psum=True) for i in range(7)]
        pbf = T(P, "pbf", [128, 1024], BF16, psum=True)

        ident_f = sb("ident_f", [128, 128], persist=True)
        ident_b = sb("ident_b", [128, 128], BF16, persist=True)
        iota = sb("iota", [128, 512], persist=True)
        tokid = sb("tokid", [128, 32], persist=True)
        ones16 = sb("ones16", [128, NE], persist=True)
        afftm = sb("afftm", [128, NB, 32, NE], persist=True)
        sinfo = sb("sinfo", [128, NB, NE, 4, 3], persist=True)
        sidx_g = sb("sidx_g", [128, NB, NE, 4], I32, persist=True)
        sidx_s = sb("sidx_s", [128, NB, NE, 4], I32, persist=True)

        hres = {}
        for nm in ("HA", "HB", "XN2", "UD"):
            for b in range(NB):
                for g in range(NG):
                    hres[(nm, b, g)] = Res("%s_%d_%d" % (nm, b, g))
        xres = Res("x_in")
        outres = Res("out")
        utres = [[Res("UT%d_%d" % (b, c)) for c in range(8)] for b in range(NB)]
        gtres = [[Res("GT%d_%d" % (b, c)) for c in range(8)] for b in range(NB)]
        ytres = [[Res("YT%d_%d" % (b, c)) for c in range(8)] for b in range(NB)]
        HD = {"HA": HA, "HB": HB}

        def H_ap(buf, b, g):
            return HD[buf][b, g * TG:(g + 1) * TG, :].rearrange("(j p) d -> p j d", p=128)

        P.begin_phase()
        dma_in(ident_f, cident)
        dma_in(iota, ciota)
        dma_in(tokid, ctok)
        P.op("vector", lambda e: e.tensor_copy(out=ident_b[:], in_=ident_f[:]), reads=[ident_f.r], writes=[ident_b.r])
        P.op("vector", lambda e: e.memset(ones16[:], 1.0), writes=[ones16.r])
        P.end_phase()

        class NormCtx:
            def __init__(self, gi, nh=2, want_o=True):
                self.hbuf = [sb("hbuf%d" % i, [128, 4, D]) for i in range(nh)]
                self.nh = nh
                if gi is not None:
                    self.xnb = sb("xnb", [128, 4, D], BF16)
                    self.xnT = [sb("xnT%d" % i, [128, 8, TG], BF16) for i in range(2)]
                    self.gv = sb("gv", [128, D])
                    dma_in(self.gv, gvec[gi])
                self.stat = [sb("stat%d" % i, [128, 8]) for i in range(nh)]
                self.sqj = sb("sqj", [128, D])
                if want_o:
                    self.obuf = sb("obuf", [128, 4, D])

            def load_h(self, src, b, g, k):
                hb = self.hbuf[k % self.nh]
                if src == "x":
                    ap = x_in[b * S + g * TG: b * S + (g + 1) * TG, :].rearrange("(j p) d -> p j d", p=128)
                    rd = [xres]
                else:
                    ap = H_ap(src, b, g)
                    rd = [hres[(src, b, g)]]
                dma_in(hb, ap, reads=rd)
                return hb

            def stats(self, k):
                hb, st, sqj = self.hbuf[k % self.nh], self.stat[k % self.nh], self.sqj
                for j in range(4):
                    P.op("scalar", lambda e, j=j: e.activation(out=sqj[:], in_=hb[:, j, :], func=AF.Square, accum_out=st[:, j:j + 1]),
                         reads=[hb.r], writes=[sqj.r, st.r])
                P.op("scalar", lambda e: e.activation(out=st[:, 4:8], in_=st[:, 0:4], func=AF.Sqrt, bias=EPS, scale=1.0 / D),
                     reads=[st.r], writes=[st.r])
                P.op("vector", lambda e: e.reciprocal(out=st[:, 4:8], in_=st[:, 4:8]), reads=[st.r], writes=[st.r])
                return st

            def norm(self, k):
                hb, xb, xt, gv = self.hbuf[k % self.nh], self.xnb, self.xnT[k % 2], self.gv
                st = self.stats(k)
                for j in range(4):
                    P.op("vector", lambda e, j=j: e.scalar_tensor_tensor(out=xb[:, j, :], in0=hb[:, j, :], scalar=st[:, 4 + j:5 + j],
                                                                        in1=gv[:], op0=ALU.mult, op1=ALU.mult),
                         reads=[hb.r, st.r, gv.r], writes=[xb.r])
                for j in range(4):
                    for c in range(8):
                        P.op("tensor", lambda e, j=j, c=c: e.transpose(out=pbf[:, c * 128:(c + 1) * 128], in_=xb[:, j, c * 128:(c + 1) * 128],
                                                                      identity=ident_b[:]),
                             reads=[xb.r, ident_b.r], writes=[pbf.r])
                    P.op("vector", lambda e, j=j: e.tensor_copy(out=xt[:, :, j * 128:(j + 1) * 128],
                                                               in_=pbf[:, :].rearrange("p (c t) -> p c t", c=8)),
                         reads=[pbf.r], writes=[xt.r])
                return xb, xt

            def store_o(self, dst, b, g, extra_reads=()):
                ob = self.obuf
                if dst == "out":
                    ap = out_d[b * S + g * TG: b * S + (g + 1) * TG, :].rearrange("(j p) d -> p j d", p=128)
                    wr = [outres]
                else:
                    ap = H_ap(dst, b, g)
                    wr = [hres[(dst, b, g)]]
                P.op("sync", lambda e, s: e.dma_start(out=ap, in_=ob[:]).then_inc(s, 16), reads=[ob.r], writes=wr, dma=ob.r)

        def load_w_bf16(tile, ap, parts=1):
            kc = tile.t.shape[1]
            step = max(1, kc // parts)
            rng = list(range(0, kc, step))

            def fn(e, s):
                for c0 in rng:
                    e.dma_start(out=tile[:, c0:c0 + step, :], in_=ap[c0 * 128:(c0 + step) * 128, :].rearrange("(c p) f -> p c f", p=128)).then_inc(s, 16)
            P.op("gpsimd", fn, writes=[tile.r], dma=tile.r, ndma=len(rng))

        def rg_layer(li, j, src, dst):
            P.begin_phase()
            N = NormCtx(li, want_o=False)
            wA = sb("wA", [128, 8, 2048], BF16)
            load_w_bf16(wA, rg_w_in[j], parts=4)
            bin_ = sb("rgbin", [128, 16])
            dma_in(bin_, rg_b_in[j])
            fm_f = [sb("rgfmf%d" % i, [128, TG]) for i in range(2)]
            fm_t = [sb("rgfmt%d" % i, [128, TG]) for i in range(2)]
            fm_g = [sb("rgfmg%d" % i, [128, TG], BF16) for i in range(2)]
            for b in range(NB):
                for g in range(NG):
                    N.load_h(src, b, g, g)
                    xb, xt = N.norm(g)
                    for fc in range(16):
                        pt = ps[fc % 4]
                        for kc in range(8):
                            mm(pt[:, :], wA[:, kc, fc * 128:(fc + 1) * 128], xt[:, kc, :], kc == 0, kc == 7, [wA.r, xt.r], [pt.r])
                        q = fc % 2
                        xg = fm_f[q]
                        P.op("scalar", lambda e, pt=pt, xg=xg, fc=fc: e.activation(out=xg[:], in_=pt[:, :], func=AF.Identity, bias=bin_[:, fc:fc + 1], scale=1.0),
                             reads=[pt.r, bin_.r], writes=[xg.r])
                        if fc < 8:
                            tt, gg = fm_t[q], fm_g[q]
                            P.op("vector", lambda e, xg=xg, tt=tt: e.tensor_tensor(out=tt[:], in0=xg[:], in1=xg[:], op=ALU.mult), reads=[xg.r], writes=[tt.r])
                            P.op("vector", lambda e, tt=tt: e.tensor_scalar(out=tt[:], in0=tt[:], scalar1=0.044715, scalar2=1.0, op0=ALU.mult, op1=ALU.add),
                                 reads=[tt.r], writes=[tt.r])
                            P.op("vector", lambda e, xg=xg, tt=tt: e.tensor_tensor(out=tt[:], in0=tt[:], in1=xg[:], op=ALU.mult), reads=[xg.r, tt.r], writes=[tt.r])
                            P.op("scalar", lambda e, tt=tt: e.activation(out=tt[:], in_=tt[:], func=AF.Sigmoid, scale=1.5957691216057308),
                                 reads=[tt.r], writes=[tt.r])
                            P.op("vector", lambda e, xg=xg, tt=tt, gg=gg: e.tensor_tensor(out=gg[:], in0=tt[:], in1=xg[:], op=ALU.mult),
                                 reads=[xg.r, tt.r], writes=[gg.r])
                            P.op("sync", lambda e, s, gg=gg, fc=fc, g=g, b=b: e.dma_start(out=GT[b, fc * 128:(fc + 1) * 128, g * TG:(g + 1) * TG], in_=gg[:]).then_inc(s, 16),
                                 reads=[gg.r], writes=[gtres[b][fc]], dma=gg.r)
                        else:
                            c = fc - 8
                            P.op("sync", lambda e, s, xg=xg, c=c, g=g, b=b: e.dma_start(out=UT[b, c * 128:(c + 1) * 128, g * TG:(g + 1) * TG], in_=xg[:]).then_inc(s, 16),
                                 reads=[xg.r], writes=[utres[b][c]], dma=xg.r)
            P.end_phase()

            P.begin_phase()
            gw = sb("rggw", [128, 2, 2, 4, 2, 256], BF16)

            def fn(e, s):
                for d_ in range(2):
                    for kd in range(2):
                        for h in range(4):
                            e.dma_start(out=gw[:, d_, kd, h, :, :],
                                        in_=rg_gw[j, d_, kd, h].rearrange("(c p) o -> p c o", p=128)).then_inc(s, 16)
            P.op("gpsimd", fn, writes=[gw.r], dma=gw.r, ndma=16)
            cw = sb("rgcw", [128, 5, 8])
            dma_in(cw, rg_conv[j])
            gb = sb("rggb", [128, 2, 2, 8])
            dma_in(gb, rg_gb[j])
            lam = sb("rglam", [128, 2, 8])
            dma_in(lam, rg_lam[j])
            c8 = sb("rgc8", [128, 2, 8])
            P.op("scalar", lambda e: e.activation(out=c8[:], in_=lam[:], func=AF.Exp, scale=-1.0), reads=[lam.r], writes=[c8.r])
            P.op("scalar", lambda e: e.activation(out=c8[:], in_=c8[:], func=AF.Ln, bias=1.0, scale=1.0), reads=[c8.r], writes=[c8.r])
            P.op("vector", lambda e: e.tensor_scalar(out=c8[:], in0=c8[:], scalar1=-8.0, scalar2=None, op0=ALU.mult), reads=[c8.r], writes=[c8.r])
            ue = sb("rgue", [128, 2, S + 4])
            uc = sb("rguc", [128, 2, S])
            ucb = sb("rgucb", [128, 2, S], BF16)
            Aa = sb("rgA", [128, S])
            Bb = sb("rgB", [128, S])
            hs = [sb("rghs%d" % i, [128, S]) for i in range(2)]
            gtl = sb("rggt", [128, S], BF16)
            yt = sb("rgyt", [128, S], BF16)
            ti = [sb("rgti%d" % i, [128, TG]) for i in range(2)]
            tr = [sb("rgtr%d" % i, [128, TG]) for i in range(2)]
            P.op("vector", lambda e: e.memset(ue[:, :, 0:2], 0.0), writes=[ue.r])
            P.op("vector", lambda e: e.memset(ue[:, :, S + 2:S + 4], 0.0), writes=[ue.r])
            for b in range(NB):
                for h in range(4):
                    for cc in range(2):
                        c = 2 * h + cc
                        P.op("sync", lambda e, s, c=c, cc=cc, b=b: e.dma_start(out=ue[:, cc, 2:S + 2], in_=UT[b, c * 128:(c + 1) * 128, :]).then_inc(s, 16),
                             reads=[utres[b][c]], writes=[ue.r], dma=ue.r)
                    for cc in range(2):
                        c = 2 * h + cc
                        P.op("vector", lambda e, c=c, cc=cc: e.tensor_scalar(out=uc[:, cc, :], in0=ue[:, cc, 0:S], scalar1=cw[:, 0, c:c + 1], scalar2=cw[:, 4, c:c + 1],
                                                                            op0=ALU.mult, op1=ALU.add), reads=[ue.r, cw.r], writes=[uc.r])
                        for kk in range(1, 4):
                            P.op("vector", lambda e, c=c, cc=cc, kk=kk: e.scalar_tensor_tensor(out=uc[:, cc, :], in0=ue[:, cc, kk:S + kk], scalar=cw[:, kk, c:c + 1],
                                                                                              in1=uc[:, cc, :], op0=ALU.mult, op1=ALU.add),
                                 reads=[ue.r, cw.r, uc.r], writes=[uc.r])
                        P.op("scalar", lambda e, cc=cc: e.copy(out=ucb[:, cc, :], in_=uc[:, cc, :]), reads=[uc.r], writes=[ucb.r])
                    for cc in range(2):
                        c = 2 * h + cc
                        P.op("sync", lambda e, s, c=c, b=b: e.dma_start(out=gtl[:], in_=GT[b, c * 128:(c + 1) * 128, :]).then_inc(s, 16),
                             reads=[gtres[b][c]], writes=[gtl.r], dma=gtl.r)
                        for d_ in range(2):
                            hd = hs[d_]
                            for tb in range(S // TG):
                                q = tb % 2
                                sl = slice(tb * TG, (tb + 1) * TG)
                                pi, pr = ps[4 + 0], ps[4 + 1]
                                for kc in range(2):
                                    mm(pi[:, :], gw[:, d_, 0, h, kc, cc * 128:(cc + 1) * 128], ucb[:, kc, sl], kc == 0, kc == 1, [gw.r, ucb.r], [pi.r])
                                for kc in range(2):
                                    mm(pr[:, :], gw[:, d_, 1, h, kc, cc * 128:(cc + 1) * 128], ucb[:, kc, sl], kc == 0, kc == 1, [gw.r, ucb.r], [pr.r])
                                tti, ttr = ti[q], tr[q]
                                P.op("scalar", lambda e, pi=pi, tti=tti, d_=d_, c=c: e.activation(out=tti[:], in_=pi[:, :], func=AF.Sigmoid, bias=gb[:, d_, 0, c:c + 1], scale=1.0),
                                     reads=[pi.r, gb.r], writes=[tti.r])
                                P.op("scalar", lambda e, pr=pr, ttr=ttr, d_=d_, c=c: e.activation(out=ttr[:], in_=pr[:, :], func=AF.Sigmoid, bias=gb[:, d_, 1, c:c + 1], scale=1.0),
                                     reads=[pr.r, gb.r], writes=[ttr.r])
                                P.op("scalar", lambda e, ttr=ttr, d_=d_, c=c, sl=sl: e.activation(out=Aa[:, sl], in_=ttr[:], func=AF.Exp, scale=c8[:, d_, c:c + 1]),
                                     reads=[ttr.r, c8.r], writes=[Aa.r])
                                P.op("vector", lambda e, ttr=ttr, sl=sl: e.tensor_tensor(out=ttr[:], in0=Aa[:, sl], in1=Aa[:, sl], op=ALU.mult), reads=[Aa.r], writes=[ttr.r])
                                P.op("scalar", lambda e, ttr=ttr: e.activation(out=ttr[:], in_=ttr[:], func=AF.Sqrt, bias=1.0, scale=-1.0), reads=[ttr.r], writes=[ttr.r])
                                P.op("vector", lambda e, tti=tti, ttr=ttr: e.tensor_tensor(out=tti[:], in0=tti[:], in1=ttr[:], op=ALU.mult), reads=[tti.r, ttr.r], writes=[tti.r])
                                P.op("vector", lambda e, tti=tti, cc=cc, sl=sl: e.tensor_tensor(out=Bb[:, sl], in0=tti[:], in1=uc[:, cc, sl], op=ALU.mult),
                                     reads=[tti.r, uc.r], writes=[Bb.r])
                            if d_ == 0:
                                P.op("vector", lambda e, hd=hd: e.tensor_tensor_scan(out=hd[:], data0=Aa[:], data1=Bb[:], initial=0.0, op0=ALU.mult, op1=ALU.add),
                                     reads=[Aa.r, Bb.r], writes=[hd.r])
                            else:
                                P.op("vector", lambda e, hd=hd: e.tensor_tensor_scan(out=hd[:, ::-1], data0=Aa[:, ::-1], data1=Bb[:, ::-1], initial=0.0,
                                                                                    op0=ALU.mult, op1=ALU.add),
                                     reads=[Aa.r, Bb.r], writes=[hd.r])
                        P.op("vector", lambda e: e.tensor_tensor(out=hs[0][:], in0=hs[0][:], in1=hs[1][:], op=ALU.add), reads=[hs[0].r, hs[1].r], writes=[hs[0].r])
                        P.op("vector", lambda e: e.tensor_tensor(out=yt[:], in0=hs[0][:], in1=gtl[:], op=ALU.mult), reads=[hs[0].r, gtl.r], writes=[yt.r])
                        P.op("sync", lambda e, s, c=c, b=b: e.dma_start(out=YT[b, c * 128:(c + 1) * 128, :], in_=yt[:]).then_inc(s, 16),
                             reads=[yt.r], writes=[ytres[b][c]], dma=yt.r)
            P.end_phase()

            P.begin_phase()
            N = NormCtx(None)
            wout = sb("rgwout", [128, 8, D], BF16)
            load_w_bf16(wout, rg_w_out[j], parts=2)
            bout = sb("rgbout", [128, D])
            dma_in(bout, rg_b_out[j])
            ylt = [sb("rgyl%d" % i, [128, 8, 128], BF16) for i in range(2)]
            n = 0
            for b in range(NB):
                for g in range(NG):
                    hb = N.load_h(src, b, g, g)
                    ob = N.obuf
                    for jt in range(4):
                        yl = ylt[n % 2]
                        n += 1
                        t0 = g * TG + jt * 128
                        P.op("sync", lambda e, s, yl=yl, t0=t0, b=b: e.dma_start(out=yl[:], in_=YT[b, :, t0:t0 + 128].rearrange("(c p) t -> p c t", p=128)).then_inc(s, 16),
                             reads=ytres[b], writes=[yl.r], dma=yl.r)
                        for nb in range(2):
                            pt = ps[nb]
                            for kc in range(8):
                                mm(pt[:, :], yl[:, kc, :], wout[:, kc, nb * 512:(nb + 1) * 512], kc == 0, kc == 7, [yl.r, wout.r], [pt.r])
                            P.op("vector", lambda e, pt=pt, jt=jt, nb=nb: e.tensor_tensor(out=ob[:, jt, nb * 512:(nb + 1) * 512], in0=pt[:, :],
                                                                                      in1=bout[:, nb * 512:(nb + 1) * 512], op=ALU.add),
                                 reads=[pt.r, bout.r], writes=[ob.r])
                        P.op("vector", lambda e, jt=jt, hb=hb: e.tensor_tensor(out=ob[:, jt, :], in0=ob[:, jt, :], in1=hb[:, jt, :], op=ALU.add),
                             reads=[ob.r, hb.r], writes=[ob.r])
                    N.store_o(dst, b, g)
            P.end_phase()

        def ft_layer(li, j, src, dst):
            P.begin_phase()
            N = NormCtx(li, want_o=False)
            win = sb("ftwin", [128, 8, D], BF16)
            load_w_bf16(win, ft_w_in[j], parts=2)
            ub = [sb("ftub%d" % i, [128, 4, D], BF16) for i in range(2)]
            for b in range(NB):
                for g in range(NG):
                    N.load_h(src, b, g, g)
                    xb, xt = N.norm(g)
                    u = ub[g % 2]
                    for jt in range(4):
                        for nb in range(2):
                            pt = ps[nb]
                            for kc in range(8):
                                mm(pt[:, :], xt[:, kc, jt * 128:(jt + 1) * 128], win[:, kc, nb * 512:(nb + 1) * 512], kc == 0, kc == 7, [xt.r, win.r], [pt.r])
                            P.op("scalar", lambda e, pt=pt, u=u, jt=jt, nb=nb: e.copy(out=u[:, jt, nb * 512:(nb + 1) * 512], in_=pt[:, :]),
                                 reads=[pt.r], writes=[u.r])
                    P.op("sync", lambda e, s, u=u, b=b, g=g: e.dma_start(out=UD[b, g * TG:(g + 1) * TG, :].rearrange("(j p) d -> p j d", p=128), in_=u[:]).then_inc(s, 16),
                         reads=[u.r], writes=[hres[("UD", b, g)]], dma=u.r)
            P.end_phase()

            P.begin_phase()
            N = NormCtx(None)
            wout = sb("ftwout", [128, 8, D], BF16)
            load_w_bf16(wout, ft_w_out[j], parts=2)
            cdc = sb("ftcd", [128, 2, 2, 256], BF16)

            def fn(e, s):
                for q in range(2):
                    e.dma_start(out=cdc[:, q, :, :], in_=ccd[q].rearrange("(c p) o -> p c o", p=128)).then_inc(s, 16)
            P.op("gpsimd", fn, writes=[cdc.r], dma=cdc.r, ndma=2)
            U = sb("ftU", [128, 32, D], BF16)
            tab = [sb("fttab%d" % i, [128, 16, TG], BF16) for i in range(2)]
            YZ = sb("ftYZ", [128, 2, 8, TG], BF16)
            fT = sb("ftfT", [128, 8, TG], BF16)
            tabn = 0
            for b in range(NB):
                P.op("sync", lambda e, s, b=b: e.dma_start(out=U[:], in_=UD[b].rearrange("(c p) d -> p c d", p=128)).then_inc(s, 16),
                     reads=[hres[("UD", b, g)] for g in range(NG)], writes=[U.r], dma=U.r)
                for pb in range(NG):
                    for q in range(2):
                        for half in range(2):
                            for th in range(2):
                                tb = tab[tabn % 2]
                                tabn += 1

                                def fn(e, s, tb=tb, q=q, th=th, pb=pb):
                                    for c4 in range(4):
                                        r0 = (th * 16 + c4 * 4) * 128
                                        e.dma_start(out=tb[:, c4 * 4:(c4 + 1) * 4, :],
                                                    in_=cdft[q, r0:r0 + 512, pb * TG:(pb + 1) * TG].rearrange("(c p) n -> p c n", p=128)).then_inc(s, 16)
                                P.op("gpsimd", fn, writes=[tb.r], dma=tb.r, ndma=4)
                                for tc_ in range(16):
                                    tcg = th * 16 + tc_
                                    for c4 in range(4):
                                        ch = half * 4 + c4
                                        pt = ps[c4]
                                        mm(pt[:, :], U[:, tcg, ch * 128:(ch + 1) * 128], tb[:, tc_, :], tcg == 0, tcg == 31, [U.r, tb.r], [pt.r])
                            for c4 in range(4):
                                ch = half * 4 + c4
                                pt = ps[c4]
                                if c4 % 2:
                                    P.op("vector", lambda e, pt=pt, q=q, ch=ch: e.tensor_copy(out=YZ[:, q, ch, :], in_=pt[:, :]), reads=[pt.r], writes=[YZ.r])
                                else:
                                    P.op("scalar", lambda e, pt=pt, q=q, ch=ch: e.copy(out=YZ[:, q, ch, :], in_=pt[:, :]), reads=[pt.r], writes=[YZ.r])
                    for ch in range(8):
                        grp = ch // 2
                        pt = ps[4 + ch % 2]
                        n = 0
                        for q in range(2):
                            for kc in range(2):
                                mm(pt[:, :], cdc[:, q, kc, (ch % 2) * 128:(ch % 2 + 1) * 128], YZ[:, q, grp * 2 + kc, :], n == 0, n == 3, [cdc.r, YZ.r], [pt.r])
                                n += 1
                        P.op("vector", lambda e, pt=pt, ch=ch: e.tensor_scalar(out=fT[:, ch, :], in0=pt[:, :], scalar1=1.0 / 1024.0, scalar2=None, op0=ALU.mult),
                             reads=[pt.r], writes=[fT.r])
                    hb = N.load_h(src, b, pb, pb)
                    ob = N.obuf
                    for jt in range(4):
                        for nb in range(2):
                            pt = ps[nb]
                            for kc in range(8):
                                mm(pt[:, :], fT[:, kc, jt * 128:(jt + 1) * 128], wout[:, kc, nb * 512:(nb + 1) * 512], kc == 0, kc == 7, [fT.r, wout.r], [pt.r])
                            P.op("vector", lambda e, pt=pt, jt=jt, nb=nb, hb=hb: e.tensor_tensor(out=ob[:, jt, nb * 512:(nb + 1) * 512], in0=pt[:, :],
                                                                                             in1=hb[:, jt, nb * 512:(nb + 1) * 512], op=ALU.add),
                                 reads=[pt.r, hb.r], writes=[ob.r])
                    N.store_o(dst, b, pb)
            P.end_phase()

        def moe_layer(li, src, dst):
            P.begin_phase()
            N = NormCtx(4 + li, want_o=False)
            wr = sb("wr", [128, 8, NE])
            dma_in(wr, w_router[li].rearrange("(c p) n -> p c n", p=128))
            xnf = sb("xnf", [128, 4, D])
            xnTf = sb("xnTf", [128, 8, TG])
            sm = sb("smx", [128, 4, NE])
            ssum = sb("ssum", [128, 12])
            xbs = sb("xbs", [128, 4, D], BF16)
            for b in range(NB):
                for g in range(NG):
                    hb = N.load_h(src, b, g, g)
                    P.op("sync", lambda e, s, hb=hb, b=b, g=g: e.dma_start(out=H_ap(dst, b, g), in_=hb[:]).then_inc(s, 16),
                         reads=[hb.r], writes=[hres[(dst, b, g)]], dma=hb.r)
                    st = N.stats(g)
                    for jq in range(4):
                        P.op("vector", lambda e, jq=jq, hb=hb, st=st: e.scalar_tensor_tensor(out=xnf[:, jq, :], in0=hb[:, jq, :], scalar=st[:, 4 + jq:5 + jq],
                                                                                            in1=N.gv[:], op0=ALU.mult, op1=ALU.mult),
                             reads=[hb.r, st.r, N.gv.r], writes=[xnf.r])
                    P.op("scalar", lambda e: e.copy(out=xbs[:], in_=xnf[:]), reads=[xnf.r], writes=[xbs.r])
                    for jq in range(4):
                        for c0 in range(2):
                            ptf = ps[4 + c0]
                            for c in range(4):
                                P.op("tensor", lambda e, jq=jq, c0=c0, c=c, ptf=ptf: e.transpose(out=ptf[:, c * 128:(c + 1) * 128], in_=xnf[:, jq, (c0 * 4 + c) * 128:(c0 * 4 + c + 1) * 128],
                                                                                             identity=ident_f[:]),
                                     reads=[xnf.r, ident_f.r], writes=[ptf.r])
                            P.op("vector" if c0 else "scalar",
                                 (lambda e, jq=jq, c0=c0, ptf=ptf: e.tensor_copy(out=xnTf[:, c0 * 4:(c0 + 1) * 4, jq * 128:(jq + 1) * 128], in_=ptf[:, :].rearrange("p (c t) -> p c t", c=4))) if c0 else
                                 (lambda e, jq=jq, c0=c0, ptf=ptf: e.copy(out=xnTf[:, c0 * 4:(c0 + 1) * 4, jq * 128:(jq + 1) * 128], in_=ptf[:, :].rearrange("p (c t) -> p c t", c=4))),
                                 reads=[ptf.r], writes=[xnTf.r])
                    xt = xnTf
                    P.op("sync", lambda e, s, b=b, g=g: e.dma_start(out=XN2[b, g * TG:(g + 1) * TG, :].rearrange("(j p) d -> p j d", p=128), in_=xbs[:]).then_inc(s, 16),
                         reads=[xbs.r], writes=[hres[("XN2", b, g)]], dma=xbs.r)
                    pt = ps[6]
                    for jt in range(4):
                        for kc in range(8):
                            mm(pt[:, jt * NE:(jt + 1) * NE], xt[:, kc, jt * 128:(jt + 1) * 128], wr[:, kc, :], kc == 0, kc == 7, [xt.r, wr.r], [pt.r])
                    P.op("vector", lambda e, pt=pt: e.tensor_reduce(out=ssum[:, 0:4], in_=pt[:, 0:4 * NE].rearrange("p (j n) -> p j n", n=NE), axis=AX.X, op=ALU.max),
                         reads=[pt.r], writes=[ssum.r])
                    P.op("vector", lambda e: e.tensor_scalar(out=ssum[:, 0:4], in0=ssum[:, 0:4], scalar1=-1.0, scalar2=None, op0=ALU.mult), reads=[ssum.r], writes=[ssum.r])
                    for jt in range(4):
                        P.op("scalar", lambda e, pt=pt, jt=jt: e.activation(out=sm[:, jt, :], in_=pt[:, jt * NE:(jt + 1) * NE], func=AF.Exp, bias=ssum[:, jt:jt + 1], scale=1.0,
                                                                           accum_out=ssum[:, 4 + jt:5 + jt]),
                             reads=[pt.r, ssum.r], writes=[sm.r, ssum.r])
                    P.op("vector", lambda e: e.reciprocal(out=ssum[:, 8:12], in_=ssum[:, 4:8]), reads=[ssum.r], writes=[ssum.r])
                    for jt in range(4):
                        P.op("vector", lambda e, b=b, g=g, jt=jt: e.tensor_scalar(out=afftm[:, b, g * 4 + jt, :], in0=sm[:, jt, :], scalar1=ssum[:, 8 + jt:9 + jt], scalar2=None, op0=ALU.mult),
                             reads=[sm.r, ssum.r], writes=[afftm.r])
            P.end_phase()

            P.begin_phase()
            affT = sb("affT", [NE, S])
            msk = sb("msk", [NE, S])
            rnk = sb("rnk", [NE, S])
            onesr = sb("onesr", [NE, S])
            P.op("vector", lambda e: e.memset(onesr[:], 1.0), writes=[onesr.r])
            bs = sb("bs", [NE, 8])
            mrtm = sb("mrtm", [128, 2, 32, NE])
            sel = [sb("sel%d" % i, [128, CAP]) for i in range(2)]
            rhs3 = sb("rhs3", [128, 32, NE, 3])
            for b in range(NB):
                for c in range(32):
                    pt = ps[4 + c % 2]
                    P.op("tensor", lambda e, pt=pt, b=b, c=c: e.transpose(out=pt[0:NE, 0:128], in_=afftm[:, b, c, :], identity=ident_f[:]),
                         reads=[afftm.r, ident_f.r], writes=[pt.r])
                    P.op("scalar", lambda e, pt=pt, c=c: e.copy(out=affT[:, c * 128:(c + 1) * 128], in_=pt[0:NE, 0:128]), reads=[pt.r], writes=[affT.r])
                P.op("vector", lambda e: e.memset(bs[:, 0:1], 0.0), writes=[bs.r])
                P.op("vector", lambda e: e.memset(bs[:, 1:2], 1.0), writes=[bs.r])
                for it in range(30):
                    P.op("vector", lambda e: e.tensor_tensor(out=bs[:, 2:3], in0=bs[:, 0:1], in1=bs[:, 1:2], op=ALU.add), reads=[bs.r], writes=[bs.r])
                    P.op("vector", lambda e: e.tensor_scalar(out=bs[:, 2:3], in0=bs[:, 2:3], scalar1=0.5, scalar2=None, op0=ALU.mult), reads=[bs.r], writes=[bs.r])
                    P.op("vector", lambda e: e.tensor_scalar(out=msk[:], in0=affT[:], scalar1=bs[:, 2:3], scalar2=0.0, op0=ALU.is_ge, op1=ALU.add, accum_out=bs[:, 3:4]),
                         reads=[affT.r, bs.r], writes=[msk.r, bs.r])
                    P.op("vector", lambda e: e.tensor_scalar(out=bs[:, 4:5], in0=bs[:, 3:4], scalar1=float(CAP), scalar2=None, op0=ALU.is_ge), reads=[bs.r], writes=[bs.r])
                    P.op("vector", lambda e: e.tensor_tensor(out=bs[:, 5:6], in0=bs[:, 2:3], in1=bs[:, 0:1], op=ALU.subtract), reads=[bs.r], writes=[bs.r])
                    P.op("vector", lambda e: e.tensor_tensor(out=bs[:, 6:7], in0=bs[:, 1:2], in1=bs[:, 2:3], op=ALU.subtract), reads=[bs.r], writes=[bs.r])
                    P.op("vector", lambda e: e.scalar_tensor_tensor(out=bs[:, 0:1], in0=bs[:, 5:6], scalar=bs[:, 4:5], in1=bs[:, 0:1], op0=ALU.mult, op1=ALU.add),
                         reads=[bs.r], writes=[bs.r])
                    P.op("vector", lambda e: e.scalar_tensor_tensor(out=bs[:, 1:2], in0=bs[:, 6:7], scalar=bs[:, 4:5], in1=bs[:, 2:3], op0=ALU.mult, op1=ALU.add),
                         reads=[bs.r], writes=[bs.r])
                P.op("vector", lambda e: e.tensor_scalar(out=msk[:], in0=affT[:], scalar1=bs[:, 0:1], scalar2=None, op0=ALU.is_ge), reads=[affT.r, bs.r], writes=[msk.r])
                P.op("vector", lambda e: e.tensor_tensor_scan(out=rnk[:], data0=onesr[:], data1=msk[:], initial=0.0, op0=ALU.mult, op1=ALU.add),
                     reads=[onesr.r, msk.r], writes=[rnk.r])
                for wi, src_t in enumerate((msk, rnk)):
                    pt = ps[4 + wi]
                    for c in range(32):
                        P.op("tensor", lambda e, pt=pt, c=c, src_t=src_t: e.transpose(out=pt[:, c * NE:(c + 1) * NE], in_=src_t[:, c * 128:(c + 1) * 128], identity=ident_f[0:NE, 0:NE]),
                             reads=[src_t.r, ident_f.r], writes=[pt.r])
                    P.op("vector", lambda e, pt=pt, wi=wi: e.tensor_copy(out=mrtm[:, wi, :, :], in_=pt[:, :].rearrange("p (c n) -> p c n", n=NE)),
                         reads=[pt.r], writes=[mrtm.r])
                for c in range(32):
                    P.op("vector", lambda e, c=c: e.tensor_scalar(out=rhs3[:, c, :, 0], in0=ones16[:], scalar1=tokid[:, c:c + 1], scalar2=None, op0=ALU.mult),
                         reads=[ones16.r, tokid.r], writes=[rhs3.r])
                P.op("vector", lambda e, b=b: e.tensor_copy(out=rhs3[:, :, :, 1], in_=afftm[:, b, :, :]), reads=[afftm.r], writes=[rhs3.r])
                P.op("vector", lambda e: e.memset(rhs3[:, :, :, 2], 1.0), writes=[rhs3.r])
                for ex in range(NE):
                    for c in range(32):
                        sl_ = sel[c % 2]
                        P.op("vector", lambda e, sl_=sl_, c=c, ex=ex: e.tensor_scalar(out=sl_[:], in0=iota[:], scalar1=mrtm[:, 1, c, ex:ex + 1],
                                                                                    scalar2=mrtm[:, 0, c, ex:ex + 1], op0=ALU.is_equal, op1=ALU.mult),
                             reads=[iota.r, mrtm.r], writes=[sl_.r])
                        for sbk in range(4):
                            pt = ps[sbk]
                            mm(pt[:, 0:3], sl_[:, sbk * 128:(sbk + 1) * 128], rhs3[:, c, ex, :], c == 0, c == 31, [sl_.r, rhs3.r], [pt.r])
                    for sbk in range(4):
                        pt = ps[sbk]
                        P.op("vector", lambda e, pt=pt, b=b, ex=ex, sbk=sbk: e.tensor_copy(out=sinfo[:, b, ex, sbk, :], in_=pt[:, 0:3]),
                             reads=[pt.r], writes=[sinfo.r])
                P.op("vector", lambda e, b=b: e.tensor_scalar(out=sinfo[:, b, :, :, 0], in0=sinfo[:, b, :, :, 0], scalar1=float(b * S), scalar2=None, op0=ALU.add),
                     reads=[sinfo.r], writes=[sinfo.r])
                P.op("vector", lambda e, b=b: e.tensor_copy(out=sidx_g[:, b, :, :], in_=sinfo[:, b, :, :, 0]), reads=[sinfo.r], writes=[sidx_g.r])
                P.op("vector", lambda e, b=b: e.tensor_scalar(out=sinfo[:, b, :, :, 2], in0=sinfo[:, b, :, :, 2], scalar1=-float(S), scalar2=float(S + b * 128), op0=ALU.mult, op1=ALU.add),
                     reads=[sinfo.r], writes=[sinfo.r])
                P.op("vector", lambda e, b=b: e.tensor_tensor(out=sinfo[:, b, :, :, 2], in0=sinfo[:, b, :, :, 2], in1=sinfo[:, b, :, :, 0], op=ALU.add),
                     reads=[sinfo.r], writes=[sinfo.r])
                P.op("vector", lambda e, b=b: e.tensor_copy(out=sidx_s[:, b, :, :], in_=sinfo[:, b, :, :, 2]), reads=[sinfo.r], writes=[sidx_s.r])
            P.end_phase()

            P.begin_phase()
            wA = [sb("wA%d" % h, [128, 8, FF // 2], BF16) for h in range(2)]
            wB = [sb("wB%d" % h, [128, 8, FF // 2], BF16) for h in range(2)]
            wC = [sb("wC%d" % h, [128, 8, D], BF16) for h in range(2)]
            xg = [sb("xg%d" % i, [128, 4, D], BF16) for i in range(2)]
            xgT = sb("xgT", [128, 8, CAP], BF16)
            hg = sb("hg", [128, 16, CAP], BF16)
            sg = [sb("sg%d" % i, [128, CAP]) for i in range(2)]
            yb = [sb("yb%d" % i, [128, D]) for i in range(4)]
            allh_dst = {b: [hres[(dst, b, g)] for g in range(NG)] for b in range(NB)}
            allx = {b: [hres[("XN2", b, g)] for g in range(NG)] for b in range(NB)}
            hdst = HD[dst]
            b = 0

            def load_half(tile, ap):
                def fn(e, s):
                    for c0 in (0, 4):
                        e.dma_start(out=tile[:, c0:c0 + 4, :], in_=ap[c0 * 128:(c0 + 4) * 128, :].rearrange("(c p) f -> p c f", p=128)).then_inc(s, 16)
                P.op("gpsimd", fn, writes=[tile.r], dma=tile.r, ndma=2)

            def load_gu(ex, h):
                load_half(wA[h], w_gate[lidx[li], ex][:, h * 1024:(h + 1) * 1024])
                load_half(wB[h], w_up[lidx[li], ex][:, h * 1024:(h + 1) * 1024])

            def load_dn(ex):
                for h in range(2):
                    load_half(wC[h], w_down[lidx[li], ex][h * 1024:(h + 1) * 1024, :])

            def gather(ex):
                xgk = xg[ex % 2]

                def fn(e, s):
                    for sbk in range(4):
                        e.indirect_dma_start(out=xgk[:, sbk, :], out_offset=None, in_=XN2.rearrange("b s d -> (b s) d"),
                                             in_offset=bass.IndirectOffsetOnAxis(ap=sidx_g[:, b, ex, sbk:sbk + 1], axis=0)).then_inc(s, 16)
                P.op("gpsimd", fn, reads=allx[b] + [sidx_g.r], writes=[xgk.r], dma=xgk.r, ndma=4)

            gather(0)
            load_gu(0, 0)
            load_gu(0, 1)
            load_dn(0)
            for ex in range(NE):
                xgk = xg[ex % 2]
                for sbk in range(4):
                    for c in range(8):
                        P.op("tensor", lambda e, sbk=sbk, c=c, xgk=xgk: e.transpose(out=pbf[:, c * 128:(c + 1) * 128], in_=xgk[:, sbk, c * 128:(c + 1) * 128], identity=ident_b[:]),
                             reads=[xgk.r, ident_b.r], writes=[pbf.r])
                    P.op("vector", lambda e, sbk=sbk: e.tensor_copy(out=xgT[:, :, sbk * 128:(sbk + 1) * 128], in_=pbf[:, :].rearrange("p (c t) -> p c t", c=8)),
                         reads=[pbf.r], writes=[xgT.r])
                if ex + 1 < NE:
                    gather(ex + 1)
                for fc in range(16):
                    h, f8 = fc // 8, fc % 8
                    pg, pu = ps[(fc % 2) * 2], ps[(fc % 2) * 2 + 1]
                    for kc in range(8):
                        mm(pg[:, :], wA[h][:, kc, f8 * 128:(f8 + 1) * 128], xgT[:, kc, :], kc == 0, kc == 7, [wA[h].r, xgT.r], [pg.r])
                    for kc in range(8):
                        mm(pu[:, :], wB[h][:, kc, f8 * 128:(f8 + 1) * 128], xgT[:, kc, :], kc == 0, kc == 7, [wB[h].r, xgT.r], [pu.r])
                    s_ = sg[fc % 2]
                    P.op("scalar", lambda e, pg=pg, s_=s_: e.activation(out=s_[:], in_=pg[:, :], func=AF.Silu), reads=[pg.r], writes=[s_.r])
                    P.op("vector", lambda e, pu=pu, s_=s_, fc=fc: e.tensor_tensor(out=hg[:, fc, :], in0=s_[:], in1=pu[:, :], op=ALU.mult),
                         reads=[pu.r, s_.r], writes=[hg.r])
                    if f8 == 7 and ex + 1 < NE:
                        load_gu(ex + 1, h)
                for sbk in range(4):
                    y = yb[sbk]
                    for nb in range(2):
                        pt = ps[4 + nb]
                        for fc in range(16):
                            mm(pt[:, :], hg[:, fc, sbk * 128:(sbk + 1) * 128], wC[fc // 8][:, fc % 8, nb * 512:(nb + 1) * 512], fc == 0, fc == 15,
                               [hg.r, wC[fc // 8].r], [pt.r])
                        P.op("vector", lambda e, pt=pt, y=y, ex=ex, sbk=sbk, nb=nb: e.tensor_scalar(out=y[:, nb * 512:(nb + 1) * 512], in0=pt[:, :],
                                                                                           scalar1=sinfo[:, b, ex, sbk, 1:2], scalar2=None, op0=ALU.mult),
                             reads=[pt.r, sinfo.r], writes=[y.r])
                if ex + 1 < NE:
                    load_dn(ex + 1)
                for sbk in range(4):
                    y = yb[sbk]
                    P.op("gpsimd", lambda e, s, y=y, ex=ex, sbk=sbk: e.indirect_dma_start(
                        out=hdst.rearrange("b s d -> (b s) d"), out_offset=bass.IndirectOffsetOnAxis(ap=sidx_s[:, b, ex, sbk:sbk + 1], axis=0),
                        in_=y[:], in_offset=None, compute_op=ALU.add).then_inc(s, 16),
                        reads=[y.r, sidx_s.r] + (allh_dst[b] if sbk > 0 else []), writes=allh_dst[b] if sbk == 0 else [], dma=y.r)
                P.op("gpsimd", lambda e: e.nop(), reads=[yb[i].r.d for i in range(4)], writes=allh_dst[b])
            P.end_phase()

        def ple_layer(li, buf):
            P.begin_phase()
            N = NormCtx(8 + li)
            wg = sb("plewg", [128, 8, D], BF16)
            wp = sb("plewp", [128, 2, D], BF16)
            load_w_bf16(wg, ple_wg[li], parts=2)
            load_w_bf16(wp, ple_wp[li], parts=1)
            pl = [sb("plp%d" % i, [128, 4, PLE]) for i in range(2)]
            plb = sb("plpb", [128, 4, PLE], BF16)
            plT = sb("plpT", [128, 2, TG], BF16)
            gsb = [sb("plg%d" % i, [128, 512]) for i in range(2)]
            for b in range(NB):
                for g in range(NG):
                    hb = N.load_h(buf, b, g, g)
                    pk = pl[g % 2]
                    dma_in(pk, p_in[li, b * S + g * TG: b * S + (g + 1) * TG, :].rearrange("(j p) d -> p j d", p=128))
                    xb, xt = N.norm(g)
                    ob = N.obuf
                    P.op("vector", lambda e, pk=pk: e.tensor_copy(out=plb[:], in_=pk[:]), reads=[pk.r], writes=[plb.r])
                    for jt in range(4):
                        for c in range(2):
                            P.op("tensor", lambda e, jt=jt, c=c: e.transpose(out=pbf[:, c * 128:(c + 1) * 128], in_=plb[:, jt, c * 128:(c + 1) * 128], identity=ident_b[:]),
                                 reads=[plb.r, ident_b.r], writes=[pbf.r])
                        P.op("vector", lambda e, jt=jt: e.tensor_copy(out=plT[:, :, jt * 128:(jt + 1) * 128], in_=pbf[:, 0:256].rearrange("p (c t) -> p c t", c=2)),
                             reads=[pbf.r], writes=[plT.r])
                    for jt in range(4):
                        for nb in range(2):
                            pgt, ppt = ps[nb * 2], ps[nb * 2 + 1]
                            for kc in range(8):
                                mm(pgt[:, :], xt[:, kc, jt * 128:(jt + 1) * 128], wg[:, kc, nb * 512:(nb + 1) * 512], kc == 0, kc == 7, [xt.r, wg.r], [pgt.r])
                            for kc in range(2):
                                mm(ppt[:, :], plT[:, kc, jt * 128:(jt + 1) * 128], wp[:, kc, nb * 512:(nb + 1) * 512], kc == 0, kc == 1, [plT.r, wp.r], [ppt.r])
                            gs = gsb[nb]
                            P.op("scalar", lambda e, pgt=pgt, gs=gs: e.activation(out=gs[:], in_=pgt[:, :], func=AF.Sigmoid), reads=[pgt.r], writes=[gs.r])
                            P.op("vector", lambda e, ppt=ppt, gs=gs: e.tensor_tensor(out=gs[:], in0=gs[:], in1=ppt[:, :], op=ALU.mult), reads=[ppt.r, gs.r], writes=[gs.r])
                            P.op("vector", lambda e, gs=gs, jt=jt, nb=nb, hb=hb: e.tensor_tensor(out=ob[:, jt, nb * 512:(nb + 1) * 512], in0=gs[:],
                                                                                             in1=hb[:, jt, nb * 512:(nb + 1) * 512], op=ALU.add),
                                 reads=[gs.r, hb.r], writes=[ob.r])
                    N.store_o(buf, b, g)
            P.end_phase()

        def final_norm(buf):
            P.begin_phase()
            N = NormCtx(None)
            gv = sb("gvf", [128, D])
            dma_in(gv, gvec[12])
            for b in range(NB):
                for g in range(NG):
                    hb = N.load_h(buf, b, g, g)
                    st = N.stats(g)
                    ob = N.obuf
                    for j in range(4):
                        P.op("vector", lambda e, j=j, hb=hb, st=st: e.scalar_tensor_tensor(out=ob[:, j, :], in0=hb[:, j, :], scalar=st[:, 4 + j:5 + j], in1=gv[:],
                                                                                          op0=ALU.mult, op1=ALU.mult), reads=[hb.r, st.r, gv.r], writes=[ob.r])
                    N.store_o("out", b, g)
            P.end_phase()

        cur = "x"
        for li in layers:
            P.epoch = li
            j = li // 2
            if li % 2 == 0:
                rg_layer(li, j, cur, "HB")
            else:
                ft_layer(li, j, cur, "HB")
            cur = "HB"
            if stop == "mix" and li == layers[-1]:
                break
            moe_layer(li, "HB", "HA")
            cur = "HA"
            if stop == "moe" and li == layers[-1]:
                break
            ple_layer(li, "HA")
        P.epoch = 4
        if final:
            final_norm(cur)
        else:
            P.begin_phase()
            N = NormCtx(None)
            for b in range(NB):
                for g in range(NG):
                    hb = N.load_h(cur, b, g, g)
                    P.op("vector", lambda e, hb=hb: e.tensor_copy(out=N.obuf[:], in_=hb[:]), reads=[hb.r], writes=[N.obuf.r])
                    N.store_o("out", b, g)
            P.end_phase()
        P.begin_phase()
        P.op("sync", lambda e: e.nop(), reads=[], writes=[])
        P.end_phase()
    return nc


def _consts():
    ident = np.eye(128, dtype=np.float32)
    iota = np.tile(np.arange(1, 513, dtype=np.float32)[None, :], (128, 1))
    tok = (np.arange(32)[None, :] * 128 + np.arange(128)[:, None]).astype(np.float32)
    n = np.arange(S, dtype=np.int64)
    m = (n[:, None] * n[None, :]) % S
    ang = 2.0 * np.pi * m.astype(np.float64) / S
    dft = np.stack([np.cos(ang), -np.sin(ang)]).astype(np.float32)
    c = np.arange(256, dtype=np.int64)
    mc = (c[:, None] * c[None, :]) % 256
    angc = 2.0 * np.pi * mc.astype(np.float64) / 256
    cd = np.stack([np.cos(angc), np.sin(angc)]).astype(np.float32)
    return dict(c_ident=ident, c_iota=iota, c_tok=tok, c_dft=dft, c_cdft=cd)


def _prep(inputs, NB, layers=(0, 1, 2, 3)):
    f = lambda a: np.ascontiguousarray(np.asarray(a, dtype=np.float32))
    m = {}
    m["x"] = f(inputs["x"])[:NB].reshape(NB * S, D)
    m["p"] = f(inputs["p"])[:, :NB].reshape(4, NB * S, PLE)
    gvv = np.concatenate([f(inputs["g_mix"]), f(inputs["g_ffn"]), f(inputs["g_ple"]), f(inputs["g_final"])[None, :]], axis=0)
    m["gvec"] = np.ascontiguousarray(np.broadcast_to(gvv[:, None, :], (13, 128, D)))
    m["rg_w_in"] = f(inputs["rg_w_in"])
    m["rg_b_in"] = np.ascontiguousarray(f(inputs["rg_b_in"]).reshape(2, 16, 128).transpose(0, 2, 1))
    conv = np.concatenate([f(inputs["rg_conv_w"]), f(inputs["rg_conv_b"])[:, None, :]], axis=1)
    m["rg_conv"] = np.ascontiguousarray(conv.reshape(2, 5, 8, 128).transpose(0, 3, 1, 2))
    gw = np.stack([f(inputs["rg_gx_w"]), f(inputs["rg_ga_w"])], axis=2)
    m["rg_gw"] = np.ascontiguousarray(gw)
    gb = np.stack([f(inputs["rg_gx_b"]), f(inputs["rg_ga_b"])], axis=2)
    m["rg_gb"] = np.ascontiguousarray(gb.reshape(2, 2, 2, 8, 128).transpose(0, 4, 1, 2, 3))
    m["rg_lam"] = np.ascontiguousarray(f(inputs["rg_lam"]).reshape(2, 2, 8, 128).transpose(0, 3, 1, 2))
    m["rg_w_out"] = f(inputs["rg_w_out"])
    m["rg_b_out"] = np.ascontiguousarray(np.broadcast_to(f(inputs["rg_b_out"])[:, None, :], (2, 128, D)))
    for k in ("ft_w_in", "ft_w_out", "w_router", "ple_w_proj", "ple_w_gate"):
        m[k] = f(inputs[k])
    for k in ("w_gate", "w_up", "w_down"):
        m[k] = np.ascontiguousarray(np.asarray(inputs[k], dtype=np.float32)[list(layers)])
    m.update(_consts())
    return m


def kernel(**inputs):
    NB = 1
    layers = (0, 1, 2, 3)
    nc = build(NB, list(layers), final=True)
    base = _prep({**inputs, "x": np.asarray(inputs["x"])[0:1], "p": np.asarray(inputs["p"])[:, 0:1]}, NB, layers)
    in_maps = []
    for b in range(4):
        m = dict(base)
        m["x"] = np.ascontiguousarray(np.asarray(inputs["x"], dtype=np.float32)[b]).reshape(S, D)
        m["p"] = np.ascontiguousarray(np.asarray(inputs["p"], dtype=np.float32)[:, b]).reshape(4, S, PLE)
        in_maps.append(m)
    res = run_bass_kernel_spmd(nc, in_maps, core_ids=list(range(4)))
    out = np.stack([np.asarray(res.results[b]["out"], dtype=np.float32).reshape(S, D) for b in range(4)], axis=0)
    return out
```

```python
import math
import contextlib
import numpy as np
import concourse.bass as bass
import concourse.mybir as mybir
from concourse.bass_utils import run_bass_kernel_spmd

F32 = mybir.dt.float32
BF16 = mybir.dt.bfloat16
I32 = mybir.dt.int32
ALU = mybir.AluOpType
AF = mybir.ActivationFunctionType
AX = mybir.AxisListType

D = 1024
S = 4096
NE = 16
CAP = 512
FF = 2048
PLE = 256
EPS = 1e-6
TG = 512
NG = S // TG


class Res:
    def __init__(self, name):
        self.name = name
        self.w = None
        self.r = []
        self.ent = None
        self.d = None


class Op:
    __slots__ = ("eng", "fn", "deps", "dma", "sem", "val", "epoch", "has_dep", "phase")


class Prog:
    ENGS = ["sync", "scalar", "gpsimd", "vector", "tensor"]

    def __init__(self, nc, stack):
        self.nc = nc
        self.stack = stack
        self.ops = []
        self.epoch = 0
        self.esem = {}
        self.ecnt = {}
        self.dpool = []
        self.dnext = 0
        self.barrier = []
        self.phase = 0
        self.pstack = None
        self.used_ent = []

    def new_sem(self, name):
        return self.stack.enter_context(self.nc.semaphore(name))

    def begin_phase(self):
        self.phase += 1
        self.ops = []
        self.dnext = 0
        self.used_ent = []
        self.pstack = contextlib.ExitStack()

    def end_phase(self):
        self.emit()
        self.pstack.close()
        self.pstack = None

    def op(self, eng, fn, reads=(), writes=(), dma=None, ndma=1):
        o = Op()
        o.eng = eng
        o.fn = fn
        o.dma = dma
        o.epoch = self.epoch
        o.has_dep = False
        o.phase = self.phase
        deps = []
        reads = list(reads)
        writes = list(writes)
        if dma is not None:
            if dma.d is None:
                dma.d = Res(dma.name + ".d")
            writes.append(dma.d)
        for r in reads:
            if r.w is not None and r.w.phase == self.phase:
                deps.append(r.w)
        for w in writes:
            if w.w is not None and w.w.phase == self.phase:
                deps.append(w.w)
            for x in w.r:
                if x.phase == self.phase:
                    deps.append(x)
        for r in reads:
            r.r.append(o)
        for w in writes:
            w.w = o
            w.r = []
        o.deps = [d for d in deps if d is not o]
        if dma is not None:
            if dma.ent is None or dma.ent[2] != self.phase:
                if self.dnext >= len(self.dpool):
                    self.dpool.append([self.new_sem("dp%d" % len(self.dpool)), 0])
                pe = self.dpool[self.dnext]
                self.dnext += 1
                dma.ent = [pe, None, self.phase]
                self.used_ent.append(pe)
            pe = dma.ent[0]
            pe[1] += 16 * ndma
            o.sem = pe[0]
            o.val = pe[1]
        else:
            o.sem = None
            o.val = None
        self.ops.append(o)
        return o

    def emit(self):
        nc = self.nc
        ops = self.ops
        for o in ops:
            for x in o.deps:
                if x.dma is None and x.eng == "tensor" and o.eng == "tensor" and o.dma is None:
                    continue
                x.has_dep = True
        per_eng = {e: [o for o in ops if o.eng == e] for e in self.ENGS}
        for e in self.ENGS:
            comp = [o for o in per_eng[e] if o.dma is None]
            if comp:
                comp[-1].has_dep = True
        for o in ops:
            if o.dma is None and o.has_dep:
                key = (o.eng, o.epoch)
                if key not in self.esem:
                    self.esem[key] = self.new_sem("e_%s_%d" % key)
                    self.ecnt[key] = 0
                self.ecnt[key] += 1
                o.sem = self.esem[key]
                o.val = self.ecnt[key]
        barrier_in = list(self.barrier)
        with nc.Block() as block:
            def run(eng_name):
                def body(eng):
                    waited = {}
                    for (sm, vl) in barrier_in:
                        eng.wait_ge(sm, vl)
                        waited[id(sm)] = vl
                    for o in per_eng[eng_name]:
                        for x in o.deps:
                            if x.sem is None:
                                continue
                            if x.dma is None and x.eng == "tensor" and eng_name == "tensor" and o.dma is None:
                                continue
                            k = id(x.sem)
                            if waited.get(k, 0) >= x.val:
                                continue
                            eng.wait_ge(x.sem, x.val)
                            waited[k] = x.val
                        if o.dma is not None:
                            o.fn(eng, o.sem)
                        else:
                            ins = o.fn(eng)
                            if o.has_dep:
                                ins.then_inc(o.sem, 1)
                return body
            block.sync(run("sync"))
            block.scalar(run("scalar"))
            block.gpsimd(run("gpsimd"))
            block.vector(run("vector"))
            block.tensor(run("tensor"))
        bar = {}
        for (sm, vl) in barrier_in:
            bar[id(sm)] = (sm, vl)
        for e in self.ENGS:
            comp = [o for o in per_eng[e] if o.dma is None]
            if comp:
                bar[id(comp[-1].sem)] = (comp[-1].sem, comp[-1].val)
        for pe in self.used_ent:
            bar[id(pe[0])] = (pe[0], pe[1])
        self.barrier = list(bar.values())


class T:
    def __init__(self, P, name, shape, dtype, psum=False, persist=False):
        nc = P.nc
        st = P.stack if (persist or psum) else P.pstack
        nm = name if (persist or psum) else "%s_p%d" % (name, P.phase)
        if psum:
            self.t = st.enter_context(nc.psum_tensor(nm, shape, dtype))
        else:
            self.t = st.enter_context(nc.sbuf_tensor(nm, shape, dtype))
        self.r = Res(nm)

    def __getitem__(self, k):
        return self.t[k]


def build(NB, layers, final=True, stop=None):
    nc = bass.Bass("TRN2", target_bir_lowering=False)
    NTOK = NB * S
    NL = 4

    def din(name, shape, dt=F32):
        return nc.dram_tensor(name, list(shape), dt, kind="ExternalInput").ap()

    x_in = din("x", [NTOK, D])
    p_in = din("p", [NL, NTOK, PLE])
    gvec = din("gvec", [13, 128, D])
    rg_w_in = din("rg_w_in", [2, D, 2 * D])
    rg_b_in = din("rg_b_in", [2, 128, 16])
    rg_conv = din("rg_conv", [2, 128, 5, 8])
    rg_gw = din("rg_gw", [2, 2, 2, 4, 256, 256])
    rg_gb = din("rg_gb", [2, 128, 2, 2, 8])
    rg_lam = din("rg_lam", [2, 128, 2, 8])
    rg_w_out = din("rg_w_out", [2, D, D])
    rg_b_out = din("rg_b_out", [2, 128, D])
    ft_w_in = din("ft_w_in", [2, D, D])
    ft_w_out = din("ft_w_out", [2, D, D])
    w_router = din("w_router", [NL, D, NE])
    NLW = len(layers)
    lidx = {li: i for i, li in enumerate(layers)}
    w_gate = din("w_gate", [NLW, NE, D, FF])
    w_up = din("w_up", [NLW, NE, D, FF])
    w_down = din("w_down", [NLW, NE, FF, D])
    ple_wp = din("ple_w_proj", [NL, PLE, D])
    ple_wg = din("ple_w_gate", [NL, D, D])
    cident = din("c_ident", [128, 128])
    ciota = din("c_iota", [128, 512])
    ctok = din("c_tok", [128, 32])
    ctok2 = din("c_tok2", [128, 32, NE, 2])
    cdft = din("c_dft", [2, S, S])
    ccd = din("c_cdft", [2, 256, 256])
    out_d = nc.dram_tensor("out", [NTOK, D], F32, kind="ExternalOutput").ap()

    HA = nc.dram_tensor("HA", [NB, S + 128, D], F32).ap()
    HB = nc.dram_tensor("HB", [NB, S + 128, D], F32).ap()
    XN2 = nc.dram_tensor("XN2", [NB, S, D], BF16).ap()
    UT = nc.dram_tensor("UT", [NB, D, S], F32).ap()
    GT = nc.dram_tensor("GT", [NB, D, S], BF16).ap()
    YT = nc.dram_tensor("YT", [NB, D, S], BF16).ap()
    UD = nc.dram_tensor("UD", [NB, S, D], BF16).ap()

    with contextlib.ExitStack() as stack:
        P = Prog(nc, stack)

        def sb(name, shape, dt=F32, persist=False):
            return T(P, name, shape, dt, persist=persist)

        def mm(out_ap, lhsT, rhs, start, stop, reads, writes):
            P.op("tensor", lambda e: e.matmul(out_ap, lhsT, rhs, start=start, stop=stop), reads=reads, writes=writes)

        def dma_in(tile, ap, eng="sync", reads=()):
            P.op(eng, lambda e, s: e.dma_start(out=tile[:], in_=ap).then_inc(s, 16), reads=list(reads), writes=[tile.r], dma=tile.r)

        ps = [T(P, "ps%d" % i, [128, 512], F32, psum=True) for i in range(7)]
        pbf = T(P, "pbf", [128, 1024], BF16, psum=True)

        ident_f = sb("ident_f", [128, 128], persist=True)
        ident_b = sb("ident_b", [128, 128], BF16, persist=True)
        iota = sb("iota", [128, 512], persist=True)
        tokid = sb("tokid", [128, 32], persist=True)
        ones16 = sb("ones16", [128, NE], persist=True)
        afftm = sb("afftm", [128, NB, 32, NE], persist=True)
        sinfo = sb("sinfo", [128, NB, NE, 4, 3], persist=True)
        sidx_g = sb("sidx_g", [128, NB, NE, 4], I32, persist=True)
        sidx_s = sb("sidx_s", [128, NB, NE, 4], I32, persist=True)

        hres = {}
        for nm in ("HA", "HB", "XN2", "UD"):
            for b in range(NB):
                for g in range(NG):
                    hres[(nm, b, g)] = Res("%s_%d_%d" % (nm, b, g))
        xres = Res("x_in")
        outres = Res("out")
        utres = [[Res("UT%d_%d" % (b, c)) for c in range(8)] for b in range(NB)]
        gtres = [[Res("GT%d_%d" % (b, c)) for c in range(8)] for b in range(NB)]
        ytres = [[Res("YT%d_%d" % (b, c)) for c in range(8)] for b in range(NB)]
        HD = {"HA": HA, "HB": HB}

        def H_ap(buf, b, g):
            return HD[buf][b, g * TG:(g + 1) * TG, :].rearrange("(j p) d -> p j d", p=128)

        P.begin_phase()
        dma_in(ident_f, cident)
        dma_in(iota, ciota)
        dma_in(tokid, ctok)
        P.op("vector", lambda e: e.tensor_copy(out=ident_b[:], in_=ident_f[:]), reads=[ident_f.r], writes=[ident_b.r])
        P.op("vector", lambda e: e.memset(ones16[:], 1.0), writes=[ones16.r])
        P.end_phase()

        class NormCtx:
            def __init__(self, gi, nh=2, want_o=True):
                self.hbuf = [sb("hbuf%d" % i, [128, 4, D]) for i in range(nh)]
                self.nh = nh
                if gi is not None:
                    self.xnb = sb("xnb", [128, 4, D], BF16)
                    self.xnT = [sb("xnT%d" % i, [128, 8, TG], BF16) for i in range(2)]
                    self.gv = sb("gv", [128, D])
                    dma_in(self.gv, gvec[gi])
                self.stat = [sb("stat%d" % i, [128, 8]) for i in range(nh)]
                self.sqj = sb("sqj", [128, D])
                if want_o:
                    self.obuf = sb("obuf", [128, 4, D])

            def load_h(self, src, b, g, k):
                hb = self.hbuf[k % self.nh]
                if src == "x":
                    ap = x_in[b * S + g * TG: b * S + (g + 1) * TG, :].rearrange("(j p) d -> p j d", p=128)
                    rd = [xres]
                else:
                    ap = H_ap(src, b, g)
                    rd = [hres[(src, b, g)]]
                dma_in(hb, ap, reads=rd)
                return hb

            def stats(self, k):
                hb, st, sqj = self.hbuf[k % self.nh], self.stat[k % self.nh], self.sqj
                for j in range(4):
                    P.op("scalar", lambda e, j=j: e.activation(out=sqj[:], in_=hb[:, j, :], func=AF.Square, accum_out=st[:, j:j + 1]),
                         reads=[hb.r], writes=[sqj.r, st.r])
                P.op("scalar", lambda e: e.activation(out=st[:, 4:8], in_=st[:, 0:4], func=AF.Sqrt, bias=EPS, scale=1.0 / D),
                     reads=[st.r], writes=[st.r])
                P.op("vector", lambda e: e.reciprocal(out=st[:, 4:8], in_=st[:, 4:8]), reads=[st.r], writes=[st.r])
                return st

            def norm(self, k):
                hb, xb, xt, gv = self.hbuf[k % self.nh], self.xnb, self.xnT[k % 2], self.gv
                st = self.stats(k)
                for j in range(4):
                    P.op("vector", lambda e, j=j: e.scalar_tensor_tensor(out=xb[:, j, :], in0=hb[:, j, :], scalar=st[:, 4 + j:5 + j],
                                                                        in1=gv[:], op0=ALU.mult, op1=ALU.mult),
                         reads=[hb.r, st.r, gv.r], writes=[xb.r])
                for j in range(4):
                    for c in range(8):
                        P.op("tensor", lambda e, j=j, c=c: e.transpose(out=pbf[:, c * 128:(c + 1) * 128], in_=xb[:, j, c * 128:(c + 1) * 128],
                                                                      identity=ident_b[:]),
                             reads=[xb.r, ident_b.r], writes=[pbf.r])
                    P.op("vector", lambda e, j=j: e.tensor_copy(out=xt[:, :, j * 128:(j + 1) * 128],
                                                               in_=pbf[:, :].rearrange("p (c t) -> p c t", c=8)),
                         reads=[pbf.r], writes=[xt.r])
                return xb, xt

            def store_o(self, dst, b, g, extra_reads=()):
                ob = self.obuf
                if dst == "out":
                    ap = out_d[b * S + g * TG: b * S + (g + 1) * TG, :].rearrange("(j p) d -> p j d", p=128)
                    wr = [outres]
                else:
                    ap = H_ap(dst, b, g)
                    wr = [hres[(dst, b, g)]]
                P.op("sync", lambda e, s: e.dma_start(out=ap, in_=ob[:]).then_inc(s, 16), reads=[ob.r], writes=wr, dma=ob.r)

        def load_w_bf16(tile, ap, parts=1):
            kc = tile.t.shape[1]
            step = max(1, kc // parts)
            rng = list(range(0, kc, step))

            def fn(e, s):
                for c0 in rng:
                    e.dma_start(out=tile[:, c0:c0 + step, :], in_=ap[c0 * 128:(c0 + step) * 128, :].rearrange("(c p) f -> p c f", p=128)).then_inc(s, 16)
            P.op("gpsimd", fn, writes=[tile.r], dma=tile.r, ndma=len(rng))

        def rg_layer(li, j, src, dst):
            P.begin_phase()
            N = NormCtx(li, want_o=False)
            wA = sb("wA", [128, 8, 2048], BF16)
            load_w_bf16(wA, rg_w_in[j], parts=4)
            bin_ = sb("rgbin", [128, 16])
            dma_in(bin_, rg_b_in[j])
            fm_f = [sb("rgfmf%d" % i, [128, TG]) for i in range(2)]
            fm_t = [sb("rgfmt%d" % i, [128, TG]) for i in range(2)]
            fm_g = [sb("rgfmg%d" % i, [128, TG], BF16) for i in range(2)]
            for b in range(NB):
                for g in range(NG):
                    N.load_h(src, b, g, g)
                    xb, xt = N.norm(g)
                    for fc in range(16):
                        pt = ps[fc % 4]
                        for kc in range(8):
                            mm(pt[:, :], wA[:, kc, fc * 128:(fc + 1) * 128], xt[:, kc, :], kc == 0, kc == 7, [wA.r, xt.r], [pt.r])
                        q = fc % 2
                        xg = fm_f[q]
                        P.op("scalar", lambda e, pt=pt, xg=xg, fc=fc: e.activation(out=xg[:], in_=pt[:, :], func=AF.Identity, bias=bin_[:, fc:fc + 1], scale=1.0),
                             reads=[pt.r, bin_.r], writes=[xg.r])
                        if fc < 8:
                            tt, gg = fm_t[q], fm_g[q]
                            P.op("vector", lambda e, xg=xg, tt=tt: e.tensor_tensor(out=tt[:], in0=xg[:], in1=xg[:], op=ALU.mult), reads=[xg.r], writes=[tt.r])
                            P.op("vector", lambda e, tt=tt: e.tensor_scalar(out=tt[:], in0=tt[:], scalar1=0.044715, scalar2=1.0, op0=ALU.mult, op1=ALU.add),
                                 reads=[tt.r], writes=[tt.r])
                            P.op("vector", lambda e, xg=xg, tt=tt: e.tensor_tensor(out=tt[:], in0=tt[:], in1=xg[:], op=ALU.mult), reads=[xg.r, tt.r], writes=[tt.r])
                            P.op("scalar", lambda e, tt=tt: e.activation(out=tt[:], in_=tt[:], func=AF.Sigmoid, scale=1.5957691216057308),
                                 reads=[tt.r], writes=[tt.r])
                            P.op("vector", lambda e, xg=xg, tt=tt, gg=gg: e.tensor_tensor(out=gg[:], in0=tt[:], in1=xg[:], op=ALU.mult),
                                 reads=[xg.r, tt.r], writes=[gg.r])
                            P.op("sync", lambda e, s, gg=gg, fc=fc, g=g, b=b: e.dma_start(out=GT[b, fc * 128:(fc + 1) * 128, g * TG:(g + 1) * TG], in_=gg[:]).then_inc(s, 16),
                                 reads=[gg.r], writes=[gtres[b][fc]], dma=gg.r)
                        else:
                            c = fc - 8
                            P.op("sync", lambda e, s, xg=xg, c=c, g=g, b=b: e.dma_start(out=UT[b, c * 128:(c + 1) * 128, g * TG:(g + 1) * TG], in_=xg[:]).then_inc(s, 16),
                                 reads=[xg.r], writes=[utres[b][c]], dma=xg.r)
            P.end_phase()

            P.begin_phase()
            gw = sb("rggw", [128, 2, 2, 4, 2, 256], BF16)

            def fn(e, s):
                for d_ in range(2):
                    for kd in range(2):
                        for h in range(4):
                            e.dma_start(out=gw[:, d_, kd, h, :, :],
                                        in_=rg_gw[j, d_, kd, h].rearrange("(c p) o -> p c o", p=128)).then_inc(s, 16)
            P.op("gpsimd", fn, writes=[gw.r], dma=gw.r, ndma=16)
            cw = sb("rgcw", [128, 5, 8])
            dma_in(cw, rg_conv[j])
            gb = sb("rggb", [128, 2, 2, 8])
            dma_in(gb, rg_gb[j])
            lam = sb("rglam", [128, 2, 8])
            dma_in(lam, rg_lam[j])
            c8 = sb("rgc8", [128, 2, 8])
            P.op("scalar", lambda e: e.activation(out=c8[:], in_=lam[:], func=AF.Exp, scale=-1.0), reads=[lam.r], writes=[c8.r])
            P.op("scalar", lambda e: e.activation(out=c8[:], in_=c8[:], func=AF.Ln, bias=1.0, scale=1.0), reads=[c8.r], writes=[c8.r])
            P.op("vector", lambda e: e.tensor_scalar(out=c8[:], in0=c8[:], scalar1=-8.0, scalar2=None, op0=ALU.mult), reads=[c8.r], writes=[c8.r])
            ue = sb("rgue", [128, 2, S + 4])
            uc = sb("rguc", [128, 2, S])
            ucb = sb("rgucb", [128, 2, S], BF16)
            Aa = sb("rgA", [128, S])
            Bb = sb("rgB", [128, S])
            hs = [sb("rghs%d" % i, [128, S]) for i in range(2)]
            gtl = sb("rggt", [128, S], BF16)
            yt = sb("rgyt", [128, S], BF16)
            ti = [sb("rgti%d" % i, [128, TG]) for i in range(2)]
            tr = [sb("rgtr%d" % i, [128, TG]) for i in range(2)]
            P.op("vector", lambda e: e.memset(ue[:, :, 0:2], 0.0), writes=[ue.r])
            P.op("vector", lambda e: e.memset(ue[:, :, S + 2:S + 4], 0.0), writes=[ue.r])
            for b in range(NB):
                for h in range(4):
                    for cc in range(2):
                        c = 2 * h + cc
                        P.op("sync", lambda e, s, c=c, cc=cc, b=b: e.dma_start(out=ue[:, cc, 2:S + 2], in_=UT[b, c * 128:(c + 1) * 128, :]).then_inc(s, 16),
                             reads=[utres[b][c]], writes=[ue.r], dma=ue.r)
                    for cc in range(2):
                        c = 2 * h + cc
                        P.op("vector", lambda e, c=c, cc=cc: e.tensor_scalar(out=uc[:, cc, :], in0=ue[:, cc, 0:S], scalar1=cw[:, 0, c:c + 1], scalar2=cw[:, 4, c:c + 1],
                                                                            op0=ALU.mult, op1=ALU.add), reads=[ue.r, cw.r], writes=[uc.r])
                        for kk in range(1, 4):
                            P.op("vector", lambda e, c=c, cc=cc, kk=kk: e.scalar_tensor_tensor(out=uc[:, cc, :], in0=ue[:, cc, kk:S + kk], scalar=cw[:, kk, c:c + 1],
                                                                                              in1=uc[:, cc, :], op0=ALU.mult, op1=ALU.add),
                                 reads=[ue.r, cw.r, uc.r], writes=[uc.r])
                        P.op("scalar", lambda e, cc=cc: e.copy(out=ucb[:, cc, :], in_=uc[:, cc, :]), reads=[uc.r], writes=[ucb.r])
                    for cc in range(2):
                        c = 2 * h + cc
                        P.op("sync", lambda e, s, c=c, b=b: e.dma_start(out=gtl[:], in_=GT[b, c * 128:(c + 1) * 128, :]).then_inc(s, 16),
                             reads=[gtres[b][c]], writes=[gtl.r], dma=gtl.r)
                        for d_ in range(2):
                            hd = hs[d_]
                            for tb in range(S // TG):
                                q = tb % 2
                                sl = slice(tb * TG, (tb + 1) * TG)
                                pi, pr = ps[4 + 0], ps[4 + 1]
                                for kc in range(2):
                                    mm(pi[:, :], gw[:, d_, 0, h, kc, cc * 128:(cc + 1) * 128], ucb[:, kc, sl], kc == 0, kc == 1, [gw.r, ucb.r], [pi.r])
                                for kc in range(2):
                                    mm(pr[:, :], gw[:, d_, 1, h, kc, cc * 128:(cc + 1) * 128], ucb[:, kc, sl], kc == 0, kc == 1, [gw.r, ucb.r], [pr.r])
                                tti, ttr = ti[q], tr[q]
                                P.op("scalar", lambda e, pi=pi, tti=tti, d_=d_, c=c: e.activation(out=tti[:], in_=pi[:, :], func=AF.Sigmoid, bias=gb[:, d_, 0, c:c + 1], scale=1.0),
                                     reads=[pi.r, gb.r], writes=[tti.r])
                                P.op("scalar", lambda e, pr=pr, ttr=ttr, d_=d_, c=c: e.activation(out=ttr[:], in_=pr[:, :], func=AF.Sigmoid, bias=gb[:, d_, 1, c:c + 1], scale=1.0),
                                     reads=[pr.r, gb.r], writes=[ttr.r])
                                P.op("scalar", lambda e, ttr=ttr, d_=d_, c=c, sl=sl: e.activation(out=Aa[:, sl], in_=ttr[:], func=AF.Exp, scale=c8[:, d_, c:c + 1]),
                                     reads=[ttr.r, c8.r], writes=[Aa.r])
                                P.op("vector", lambda e, ttr=ttr, sl=sl: e.tensor_tensor(out=ttr[:], in0=Aa[:, sl], in1=Aa[:, sl], op=ALU.mult), reads=[Aa.r], writes=[ttr.r])
                                P.op("scalar", lambda e, ttr=ttr: e.activation(out=ttr[:], in_=ttr[:], func=AF.Sqrt, bias=1.0, scale=-1.0), reads=[ttr.r], writes=[ttr.r])
                                P.op("vector", lambda e, tti=tti, ttr=ttr: e.tensor_tensor(out=tti[:], in0=tti[:], in1=ttr[:], op=ALU.mult), reads=[tti.r, ttr.r], writes=[tti.r])
                                P.op("vector", lambda e, tti=tti, cc=cc, sl=sl: e.tensor_tensor(out=Bb[:, sl], in0=tti[:], in1=uc[:, cc, sl], op=ALU.mult),
                                     reads=[tti.r, uc.r], writes=[Bb.r])
                            if d_ == 0:
                                P.op("vector", lambda e, hd=hd: e.tensor_tensor_scan(out=hd[:], data0=Aa[:], data1=Bb[:], initial=0.0, op0=ALU.mult, op1=ALU.add),
                                     reads=[Aa.r, Bb.r], writes=[hd.r])
                            else:
                                P.op("vector", lambda e, hd=hd: e.tensor_tensor_scan(out=hd[:, ::-1], data0=Aa[:, ::-1], data1=Bb[:, ::-1], initial=0.0,
                                                                                    op0=ALU.mult, op1=ALU.add),
                                     reads=[Aa.r, Bb.r], writes=[hd.r])
                        P.op("vector", lambda e: e.tensor_tensor(out=hs[0][:], in0=hs[0][:], in1=hs[1][:], op=ALU.add), reads=[hs[0].r, hs[1].r], writes=[hs[0].r])
                        P.op("vector", lambda e: e.tensor_tensor(out=yt[:], in0=hs[0][:], in1=gtl[:], op=ALU.mult), reads=[hs[0].r, gtl.r], writes=[yt.r])
                        P.op("sync", lambda e, s, c=c, b=b: e.dma_start(out=YT[b, c * 128:(c + 1) * 128, :], in_=yt[:]).then_inc(s, 16),
                             reads=[yt.r], writes=[ytres[b][c]], dma=yt.r)
            P.end_phase()

            P.begin_phase()
            N = NormCtx(None)
            wout = sb("rgwout", [128, 8, D], BF16)
            load_w_bf16(wout, rg_w_out[j], parts=2)
            bout = sb("rgbout", [128, D])
            dma_in(bout, rg_b_out[j])
            ylt = [sb("rgyl%d" % i, [128, 8, 128], BF16) for i in range(2)]
            n = 0
            for b in range(NB):
                for g in range(NG):
                    hb = N.load_h(src, b, g, g)
                    ob = N.obuf
                    for jt in range(4):
                        yl = ylt[n % 2]
                        n += 1
                        t0 = g * TG + jt * 128
                        P.op("sync", lambda e, s, yl=yl, t0=t0, b=b: e.dma_start(out=yl[:], in_=YT[b, :, t0:t0 + 128].rearrange("(c p) t -> p c t", p=128)).then_inc(s, 16),
                             reads=ytres[b], writes=[yl.r], dma=yl.r)
                        for nb in range(2):
                            pt = ps[nb]
                            for kc in range(8):
                                mm(pt[:, :], yl[:, kc, :], wout[:, kc, nb * 512:(nb + 1) * 512], kc == 0, kc == 7, [yl.r, wout.r], [pt.r])
                            P.op("vector", lambda e, pt=pt, jt=jt, nb=nb: e.tensor_tensor(out=ob[:, jt, nb * 512:(nb + 1) * 512], in0=pt[:, :],
                                                                                      in1=bout[:, nb * 512:(nb + 1) * 512], op=ALU.add),
                                 reads=[pt.r, bout.r], writes=[ob.r])
                        P.op("vector", lambda e, jt=jt, hb=hb: e.tensor_tensor(out=ob[:, jt, :], in0=ob[:, jt, :], in1=hb[:, jt, :], op=ALU.add),
                             reads=[ob.r, hb.r], writes=[ob.r])
                    N.store_o(dst, b, g)
            P.end_phase()

        def ft_layer(li, j, src, dst):
            P.begin_phase()
            N = NormCtx(li, want_o=False)
            win = sb("ftwin", [128, 8, D], BF16)
            load_w_bf16(win, ft_w_in[j], parts=2)
            ub = [sb("ftub%d" % i, [128, 4, D], BF16) for i in range(2)]
            for b in range(NB):
                for g in range(NG):
                    N.load_h(src, b, g, g)
                    xb, xt = N.norm(g)
                    u = ub[g % 2]
                    for jt in range(4):
                        for nb in range(2):
                            pt = ps[nb]
                            for kc in range(8):
                                mm(pt[:, :], xt[:, kc, jt * 128:(jt + 1) * 128], win[:, kc, nb * 512:(nb + 1) * 512], kc == 0, kc == 7, [xt.r, win.r], [pt.r])
                            P.op("scalar", lambda e, pt=pt, u=u, jt=jt, nb=nb: e.copy(out=u[:, jt, nb * 512:(nb + 1) * 512], in_=pt[:, :]),
                                 reads=[pt.r], writes=[u.r])
                    P.op("sync", lambda e, s, u=u, b=b, g=g: e.dma_start(out=UD[b, g * TG:(g + 1) * TG, :].rearrange("(j p) d -> p j d", p=128), in_=u[:]).then_inc(s, 16),
                         reads=[u.r], writes=[hres[("UD", b, g)]], dma=u.r)
            P.end_phase()

            P.begin_phase()
            N = NormCtx(None)
            wout = sb("ftwout", [128, 8, D], BF16)
            load_w_bf16(wout, ft_w_out[j], parts=2)
            cdc = sb("ftcd", [128, 2, 2, 256], BF16)

            def fn(e, s):
                for q in range(2):
                    e.dma_start(out=cdc[:, q, :, :], in_=ccd[q].rearrange("(c p) o -> p c o", p=128)).then_inc(s, 16)
            P.op("gpsimd", fn, writes=[cdc.r], dma=cdc.r, ndma=2)
            U = sb("ftU", [128, 32, D], BF16)
            tab = [sb("fttab%d" % i, [128, 16, TG], BF16) for i in range(2)]
            YZ = sb("ftYZ", [128, 2, 8, TG], BF16)
            fT = sb("ftfT", [128, 8, TG], BF16)
            tabn = 0
            for b in range(NB):
                P.op("sync", lambda e, s, b=b: e.dma_start(out=U[:], in_=UD[b].rearrange("(c p) d -> p c d", p=128)).then_inc(s, 16),
                     reads=[hres[("UD", b, g)] for g in range(NG)], writes=[U.r], dma=U.r)
                for pb in range(NG):
                    for q in range(2):
                        for half in range(2):
                            for th in range(2):
                                tb = tab[tabn % 2]
                                tabn += 1

                                def fn(e, s, tb=tb, q=q, th=th, pb=pb):
                                    for c4 in range(4):
                                        r0 = (th * 16 + c4 * 4) * 128
                                        e.dma_start(out=tb[:, c4 * 4:(c4 + 1) * 4, :],
                                                    in_=cdft[q, r0:r0 + 512, pb * TG:(pb + 1) * TG].rearrange("(c p) n -> p c n", p=128)).then_inc(s, 16)
                                P.op("gpsimd", fn, writes=[tb.r], dma=tb.r, ndma=4)
                                for tc_ in range(16):
                                    tcg = th * 16 + tc_
                                    for c4 in range(4):
                                        ch = half * 4 + c4
                                        pt = ps[c4]
                                        mm(pt[:, :], U[:, tcg, ch * 128:(ch + 1) * 128], tb[:, tc_, :], tcg == 0, tcg == 31, [U.r, tb.r], [pt.r])
                            for c4 in range(4):
                                ch = half * 4 + c4
                                pt = ps[c4]
                                if c4 % 2:
                                    P.op("vector", lambda e, pt=pt, q=q, ch=ch: e.tensor_copy(out=YZ[:, q, ch, :], in_=pt[:, :]), reads=[pt.r], writes=[YZ.r])
                                else:
                                    P.op("scalar", lambda e, pt=pt, q=q, ch=ch: e.copy(out=YZ[:, q, ch, :], in_=pt[:, :]), reads=[pt.r], writes=[YZ.r])
                    for ch in range(8):
                        grp = ch // 2
                        pt = ps[4 + ch % 2]
                        n = 0
                        for q in range(2):
                            for kc in range(2):
                                mm(pt[:, :], cdc[:, q, kc, (ch % 2) * 128:(ch % 2 + 1) * 128], YZ[:, q, grp * 2 + kc, :], n == 0, n == 3, [cdc.r, YZ.r], [pt.r])
                                n += 1
                        P.op("vector", lambda e, pt=pt, ch=ch: e.tensor_scalar(out=fT[:, ch, :], in0=pt[:, :], scalar1=1.0 / 1024.0, scalar2=None, op0=ALU.mult),
                             reads=[pt.r], writes=[fT.r])
                    hb = N.load_h(src, b, pb, pb)
                    ob = N.obuf
                    for jt in range(4):
                        for nb in range(2):
                            pt = ps[nb]
                            for kc in range(8):
                                mm(pt[:, :], fT[:, kc, jt * 128:(jt + 1) * 128], wout[:, kc, nb * 512:(nb + 1) * 512], kc == 0, kc == 7, [fT.r, wout.r], [pt.r])
                            P.op("vector", lambda e, pt=pt, jt=jt, nb=nb, hb=hb: e.tensor_tensor(out=ob[:, jt, nb * 512:(nb + 1) * 512], in0=pt[:, :],
                                                                                             in1=hb[:, jt, nb * 512:(nb + 1) * 512], op=ALU.add),
                                 reads=[pt.r, hb.r], writes=[ob.r])
                    N.store_o(dst, b, pb)
            P.end_phase()

        def moe_layer(li, src, dst):
            P.begin_phase()
            N = NormCtx(4 + li, want_o=False)
            wr = sb("wr", [128, 8, NE])
            dma_in(wr, w_router[li].rearrange("(c p) n -> p c n", p=128))
            xnf = sb("xnf", [128, 4, D])
            xnTf = sb("xnTf", [128, 8, TG])
            sm = sb("smx", [128, 4, NE])
            ssum = sb("ssum", [128, 12])
            xbs = sb("xbs", [128, 4, D], BF16)
            for b in range(NB):
                for g in range(NG):
                    hb = N.load_h(src, b, g, g)
                    P.op("sync", lambda e, s, hb=hb, b=b, g=g: e.dma_start(out=H_ap(dst, b, g), in_=hb[:]).then_inc(s, 16),
                         reads=[hb.r], writes=[hres[(dst, b, g)]], dma=hb.r)
                    st = N.stats(g)
                    for jq in range(4):
                        P.op("vector", lambda e, jq=jq, hb=hb, st=st: e.scalar_tensor_tensor(out=xnf[:, jq, :], in0=hb[:, jq, :], scalar=st[:, 4 + jq:5 + jq],
                                                                                            in1=N.gv[:], op0=ALU.mult, op1=ALU.mult),
                             reads=[hb.r, st.r, N.gv.r], writes=[xnf.r])
                    P.op("scalar", lambda e: e.copy(out=xbs[:], in_=xnf[:]), reads=[xnf.r], writes=[xbs.r])
                    for jq in range(4):
                        for c0 in range(2):
                            ptf = ps[4 + c0]
                            for c in range(4):
                                P.op("tensor", lambda e, jq=jq, c0=c0, c=c, ptf=ptf: e.transpose(out=ptf[:, c * 128:(c + 1) * 128], in_=xnf[:, jq, (c0 * 4 + c) * 128:(c0 * 4 + c + 1) * 128],
                                                                                             identity=ident_f[:]),
                                     reads=[xnf.r, ident_f.r], writes=[ptf.r])
                            P.op("vector" if c0 else "scalar",
                                 (lambda e, jq=jq, c0=c0, ptf=ptf: e.tensor_copy(out=xnTf[:, c0 * 4:(c0 + 1) * 4, jq * 128:(jq + 1) * 128], in_=ptf[:, :].rearrange("p (c t) -> p c t", c=4))) if c0 else
                                 (lambda e, jq=jq, c0=c0, ptf=ptf: e.copy(out=xnTf[:, c0 * 4:(c0 + 1) * 4, jq * 128:(jq + 1) * 128], in_=ptf[:, :].rearrange("p (c t) -> p c t", c=4))),
                                 reads=[ptf.r], writes=[xnTf.r])
                    xt = xnTf
                    P.op("sync", lambda e, s, b=b, g=g: e.dma_start(out=XN2[b, g * TG:(g + 1) * TG, :].rearrange("(j p) d -> p j d", p=128), in_=xbs[:]).then_inc(s, 16),
                         reads=[xbs.r], writes=[hres[("XN2", b, g)]], dma=xbs.r)
                    pt = ps[6]
                    for jt in range(4):
                        for kc in range(8):
                            mm(pt[:, jt * NE:(jt + 1) * NE], xt[:, kc, jt * 128:(jt + 1) * 128], wr[:, kc, :], kc == 0, kc == 7, [xt.r, wr.r], [pt.r])
                    P.op("vector", lambda e, pt=pt: e.tensor_reduce(out=ssum[:, 0:4], in_=pt[:, 0:4 * NE].rearrange("p (j n) -> p j n", n=NE), axis=AX.X, op=ALU.max),
                         reads=[pt.r], writes=[ssum.r])
                    P.op("vector", lambda e: e.tensor_scalar(out=ssum[:, 0:4], in0=ssum[:, 0:4], scalar1=-1.0, scalar2=None, op0=ALU.mult), reads=[ssum.r], writes=[ssum.r])
                    for jt in range(4):
                        P.op("scalar", lambda e, pt=pt, jt=jt: e.activation(out=sm[:, jt, :], in_=pt[:, jt * NE:(jt + 1) * NE], func=AF.Exp, bias=ssum[:, jt:jt + 1], scale=1.0,
                                                                           accum_out=ssum[:, 4 + jt:5 + jt]),
                             reads=[pt.r, ssum.r], writes=[sm.r, ssum.r])
                    P.op("vector", lambda e: e.reciprocal(out=ssum[:, 8:12], in_=ssum[:, 4:8]), reads=[ssum.r], writes=[ssum.r])
                    for jt in range(4):
                        P.op("vector", lambda e, b=b, g=g, jt=jt: e.tensor_scalar(out=afftm[:, b, g * 4 + jt, :], in0=sm[:, jt, :], scalar1=ssum[:, 8 + jt:9 + jt], scalar2=None, op0=ALU.mult),
                             reads=[sm.r, ssum.r], writes=[afftm.r])
            P.end_phase()

            P.begin_phase()
            affT = sb("affT", [NE, S])
            msk = sb("msk", [NE, S])
            rnk = sb("rnk", [NE, S])
            onesr = sb("onesr", [NE, S])
            P.op("vector", lambda e: e.memset(onesr[:], 1.0), writes=[onesr.r])
            bs = sb("bs", [NE, 8])
            mrtm = sb("mrtm", [128, 2, 32, NE])
            sel = [sb("sel%d" % i, [128, CAP], BF16) for i in range(2)]
            rhs6 = sb("rhs6", [128, 32, NE, 6], BF16)
            tk2 = sb("tk2", [128, 32, NE, 2])
            dma_in(tk2, ctok2)
            rr = sb("rr", [128, 32, NE])
            s6 = [sb("s6_%d" % i, [6, CAP]) for i in range(2)]
            t6 = [sb("t6_%d" % i, [128, 4, 6]) for i in range(2)]
            for b in range(NB):
                for c in range(32):
                    pt = ps[4 + c % 2]
                    P.op("tensor", lambda e, pt=pt, b=b, c=c: e.transpose(out=pt[0:NE, 0:128], in_=afftm[:, b, c, :], identity=ident_f[:]),
                         reads=[afftm.r, ident_f.r], writes=[pt.r])
                    P.op("scalar", lambda e, pt=pt, c=c: e.copy(out=affT[:, c * 128:(c + 1) * 128], in_=pt[0:NE, 0:128]), reads=[pt.r], writes=[affT.r])
                P.op("vector", lambda e: e.memset(bs[:, 0:1], 0.0), writes=[bs.r])
                P.op("vector", lambda e: e.memset(bs[:, 1:2], 1.0), writes=[bs.r])
                for it in range(30):
                    P.op("vector", lambda e: e.tensor_tensor(out=bs[:, 2:3], in0=bs[:, 0:1], in1=bs[:, 1:2], op=ALU.add), reads=[bs.r], writes=[bs.r])
                    P.op("vector", lambda e: e.tensor_scalar(out=bs[:, 2:3], in0=bs[:, 2:3], scalar1=0.5, scalar2=None, op0=ALU.mult), reads=[bs.r], writes=[bs.r])
                    P.op("vector", lambda e: e.tensor_scalar(out=msk[:], in0=affT[:], scalar1=bs[:, 2:3], scalar2=0.0, op0=ALU.is_ge, op1=ALU.add, accum_out=bs[:, 3:4]),
                         reads=[affT.r, bs.r], writes=[msk.r, bs.r])
                    P.op("vector", lambda e: e.tensor_scalar(out=bs[:, 4:5], in0=bs[:, 3:4], scalar1=float(CAP), scalar2=None, op0=ALU.is_ge), reads=[bs.r], writes=[bs.r])
                    P.op("vector", lambda e: e.tensor_tensor(out=bs[:, 5:6], in0=bs[:, 2:3], in1=bs[:, 0:1], op=ALU.subtract), reads=[bs.r], writes=[bs.r])
                    P.op("vector", lambda e: e.tensor_tensor(out=bs[:, 6:7], in0=bs[:, 1:2], in1=bs[:, 2:3], op=ALU.subtract), reads=[bs.r], writes=[bs.r])
                    P.op("vector", lambda e: e.scalar_tensor_tensor(out=bs[:, 0:1], in0=bs[:, 5:6], scalar=bs[:, 4:5], in1=bs[:, 0:1], op0=ALU.mult, op1=ALU.add),
                         reads=[bs.r], writes=[bs.r])
                    P.op("vector", lambda e: e.scalar_tensor_tensor(out=bs[:, 1:2], in0=bs[:, 6:7], scalar=bs[:, 4:5], in1=bs[:, 2:3], op0=ALU.mult, op1=ALU.add),
                         reads=[bs.r], writes=[bs.r])
                P.op("vector", lambda e: e.tensor_scalar(out=msk[:], in0=affT[:], scalar1=bs[:, 0:1], scalar2=None, op0=ALU.is_ge), reads=[affT.r, bs.r], writes=[msk.r])
                P.op("vector", lambda e: e.tensor_tensor_scan(out=rnk[:], data0=onesr[:], data1=msk[:], initial=0.0, op0=ALU.mult, op1=ALU.add),
                     reads=[onesr.r, msk.r], writes=[rnk.r])
                for wi, src_t in enumerate((msk, rnk)):
                    pt = ps[4 + wi]
                    for c in range(32):
                        P.op("tensor", lambda e, pt=pt, c=c, src_t=src_t: e.transpose(out=pt[:, c * NE:(c + 1) * NE], in_=src_t[:, c * 128:(c + 1) * 128], identity=ident_f[0:NE, 0:NE]),
                             reads=[src_t.r, ident_f.r], writes=[pt.r])
                    P.op("vector", lambda e, pt=pt, wi=wi: e.tensor_copy(out=mrtm[:, wi, :, :], in_=pt[:, :].rearrange("p (c n) -> p c n", n=NE)),
                         reads=[pt.r], writes=[mrtm.r])
                P.op("vector", lambda e: e.tensor_copy(out=rhs6[:, :, :, 0:2], in_=tk2[:]), reads=[tk2.r], writes=[rhs6.r])
                P.op("vector", lambda e: e.memset(rhs6[:, :, :, 5], 1.0), writes=[rhs6.r])
                P.op("vector", lambda e, b=b: e.tensor_copy(out=rhs6[:, :, :, 2], in_=afftm[:, b, :, :]), reads=[afftm.r], writes=[rhs6.r])
                P.op("vector", lambda e, b=b: e.tensor_tensor(out=rr[:], in0=afftm[:, b, :, :], in1=rhs6[:, :, :, 2], op=ALU.subtract), reads=[afftm.r, rhs6.r], writes=[rr.r])
                P.op("vector", lambda e: e.tensor_copy(out=rhs6[:, :, :, 3], in_=rr[:]), reads=[rr.r], writes=[rhs6.r])
                P.op("vector", lambda e: e.tensor_tensor(out=rr[:], in0=rr[:], in1=rhs6[:, :, :, 3], op=ALU.subtract), reads=[rr.r, rhs6.r], writes=[rr.r])
                P.op("vector", lambda e: e.tensor_copy(out=rhs6[:, :, :, 4], in_=rr[:]), reads=[rr.r], writes=[rhs6.r])
                for ex in range(NE):
                    pt = ps[ex % 2]
                    for c in range(32):
                        sl_ = sel[c % 2]
                        P.op("vector", lambda e, sl_=sl_, c=c, ex=ex: e.tensor_scalar(out=sl_[:], in0=iota[:], scalar1=mrtm[:, 1, c, ex:ex + 1],
                                                                                    scalar2=mrtm[:, 0, c, ex:ex + 1], op0=ALU.is_equal, op1=ALU.mult),
                             reads=[iota.r, mrtm.r], writes=[sl_.r])
                        mm(pt[0:6, :], rhs6[:, c, ex, :], sl_[:], c == 0, c == 31, [sl_.r, rhs6.r], [pt.r])
                    s6k, t6k = s6[ex % 2], t6[ex % 2]
                    P.op("scalar", lambda e, pt=pt, s6k=s6k: e.copy(out=s6k[:], in_=pt[0:6, :]), reads=[pt.r], writes=[s6k.r])
                    ptT = ps[2 + ex % 2]
                    for sbk in range(4):
                        P.op("tensor", lambda e, ptT=ptT, s6k=s6k, sbk=sbk: e.transpose(out=ptT[:, sbk * 6:(sbk + 1) * 6], in_=s6k[:, sbk * 128:(sbk + 1) * 128], identity=ident_f[0:6, 0:6]),
                             reads=[s6k.r, ident_f.r], writes=[ptT.r])
                    P.op("vector", lambda e, ptT=ptT, t6k=t6k: e.tensor_copy(out=t6k[:], in_=ptT[:, 0:24].rearrange("p (s k) -> p s k", k=6)), reads=[ptT.r], writes=[t6k.r])
                    P.op("vector", lambda e, t6k=t6k, b=b, ex=ex: e.scalar_tensor_tensor(out=sinfo[:, b, ex, :, 0], in0=t6k[:, :, 0], scalar=64.0, in1=t6k[:, :, 1], op0=ALU.mult, op1=ALU.add),
                         reads=[t6k.r], writes=[sinfo.r])
                    P.op("vector", lambda e, t6k=t6k, b=b, ex=ex: e.tensor_tensor(out=sinfo[:, b, ex, :, 1], in0=t6k[:, :, 2], in1=t6k[:, :, 3], op=ALU.add), reads=[t6k.r], writes=[sinfo.r])
                    P.op("vector", lambda e, t6k=t6k, b=b, ex=ex: e.tensor_tensor(out=sinfo[:, b, ex, :, 1], in0=sinfo[:, b, ex, :, 1], in1=t6k[:, :, 4], op=ALU.add), reads=[t6k.r, sinfo.r], writes=[sinfo.r])
                    P.op("vector", lambda e, t6k=t6k, b=b, ex=ex: e.tensor_copy(out=sinfo[:, b, ex, :, 2], in_=t6k[:, :, 5]), reads=[t6k.r], writes=[sinfo.r])
                P.op("vector", lambda e, b=b: e.tensor_scalar(out=sinfo[:, b, :, :, 0], in0=sinfo[:, b, :, :, 0], scalar1=float(b * S), scalar2=None, op0=ALU.add),
                     reads=[sinfo.r], writes=[sinfo.r])
                P.op("vector", lambda e, b=b: e.tensor_copy(out=sidx_g[:, b, :, :], in_=sinfo[:, b, :, :, 0]), reads=[sinfo.r], writes=[sidx_g.r])
                P.op("vector", lambda e, b=b: e.tensor_scalar(out=sinfo[:, b, :, :, 2], in0=sinfo[:, b, :, :, 2], scalar1=-float(S), scalar2=float(S + b * 128), op0=ALU.mult, op1=ALU.add),
                     reads=[sinfo.r], writes=[sinfo.r])
                P.op("vector", lambda e, b=b: e.tensor_tensor(out=sinfo[:, b, :, :, 2], in0=sinfo[:, b, :, :, 2], in1=sinfo[:, b, :, :, 0], op=ALU.add),
                     reads=[sinfo.r], writes=[sinfo.r])
                P.op("vector", lambda e, b=b: e.tensor_copy(out=sidx_s[:, b, :, :], in_=sinfo[:, b, :, :, 2]), reads=[sinfo.r], writes=[sidx_s.r])
            P.end_phase()

            P.begin_phase()
            wA = [sb("wA%d" % h, [128, 8, FF // 2], BF16) for h in range(2)]
            wB = [sb("wB%d" % h, [128, 8, FF // 2], BF16) for h in range(2)]
            wC = [sb("wC%d" % h, [128, 8, D], BF16) for h in range(2)]
            xg = [sb("xg%d" % i, [128, 4, D], BF16) for i in range(2)]
            xgT = sb("xgT", [128, 8, CAP], BF16)
            hg = sb("hg", [128, 16, CAP], BF16)
            sg = [sb("sg%d" % i, [128, CAP]) for i in range(2)]
            yb = [sb("yb%d" % i, [128, D]) for i in range(4)]
            allh_dst = {b: [hres[(dst, b, g)] for g in range(NG)] for b in range(NB)}
            allx = {b: [hres[("XN2", b, g)] for g in range(NG)] for b in range(NB)}
            hdst = HD[dst]
            b = 0

            def load_half(tile, ap):
                def fn(e, s):
                    for c0 in (0, 4):
                        e.dma_start(out=tile[:, c0:c0 + 4, :], in_=ap[c0 * 128:(c0 + 4) * 128, :].rearrange("(c p) f -> p c f", p=128)).then_inc(s, 16)
                P.op("gpsimd", fn, writes=[tile.r], dma=tile.r, ndma=2)

            def load_gu(ex, h):
                load_half(wA[h], w_gate[lidx[li], ex][:, h * 1024:(h + 1) * 1024])
                load_half(wB[h], w_up[lidx[li], ex][:, h * 1024:(h + 1) * 1024])

            def load_dn(ex):
                for h in range(2):
                    load_half(wC[h], w_down[lidx[li], ex][h * 1024:(h + 1) * 1024, :])

            def gather(ex):
                xgk = xg[ex % 2]

                def fn(e, s):
                    for sbk in range(4):
                        e.indirect_dma_start(out=xgk[:, sbk, :], out_offset=None, in_=XN2.rearrange("b s d -> (b s) d"),
                                             in_offset=bass.IndirectOffsetOnAxis(ap=sidx_g[:, b, ex, sbk:sbk + 1], axis=0)).then_inc(s, 16)
                P.op("gpsimd", fn, reads=allx[b] + [sidx_g.r], writes=[xgk.r], dma=xgk.r, ndma=4)

            gather(0)
            load_gu(0, 0)
            load_gu(0, 1)
            load_dn(0)
            for ex in range(NE):
                xgk = xg[ex % 2]
                for sbk in range(4):
                    for c in range(8):
                        P.op("tensor", lambda e, sbk=sbk, c=c, xgk=xgk: e.transpose(out=pbf[:, c * 128:(c + 1) * 128], in_=xgk[:, sbk, c * 128:(c + 1) * 128], identity=ident_b[:]),
                             reads=[xgk.r, ident_b.r], writes=[pbf.r])
                    P.op("vector", lambda e, sbk=sbk: e.tensor_copy(out=xgT[:, :, sbk * 128:(sbk + 1) * 128], in_=pbf[:, :].rearrange("p (c t) -> p c t", c=8)),
                         reads=[pbf.r], writes=[xgT.r])
                if ex + 1 < NE:
                    gather(ex + 1)
                for fc in range(16):
                    h, f8 = fc // 8, fc % 8
                    pg, pu = ps[(fc % 2) * 2], ps[(fc % 2) * 2 + 1]
                    for kc in range(8):
                        mm(pg[:, :], wA[h][:, kc, f8 * 128:(f8 + 1) * 128], xgT[:, kc, :], kc == 0, kc == 7, [wA[h].r, xgT.r], [pg.r])
                    for kc in range(8):
                        mm(pu[:, :], wB[h][:, kc, f8 * 128:(f8 + 1) * 128], xgT[:, kc, :], kc == 0, kc == 7, [wB[h].r, xgT.r], [pu.r])
                    s_ = sg[fc % 2]
                    P.op("scalar", lambda e, pg=pg, s_=s_: e.activation(out=s_[:], in_=pg[:, :], func=AF.Silu), reads=[pg.r], writes=[s_.r])
                    P.op("vector", lambda e, pu=pu, s_=s_, fc=fc: e.tensor_tensor(out=hg[:, fc, :], in0=s_[:], in1=pu[:, :], op=ALU.mult),
                         reads=[pu.r, s_.r], writes=[hg.r])
                    if f8 == 7 and ex + 1 < NE:
                        load_gu(ex + 1, h)
                for sbk in range(4):
                    y = yb[sbk]
                    for nb in range(2):
                        pt = ps[4 + nb]
                        for fc in range(16):
                            mm(pt[:, :], hg[:, fc, sbk * 128:(sbk + 1) * 128], wC[fc // 8][:, fc % 8, nb * 512:(nb + 1) * 512], fc == 0, fc == 15,
                               [hg.r, wC[fc // 8].r], [pt.r])
                        P.op("vector", lambda e, pt=pt, y=y, ex=ex, sbk=sbk, nb=nb: e.tensor_scalar(out=y[:, nb * 512:(nb + 1) * 512], in0=pt[:, :],
                                                                                           scalar1=sinfo[:, b, ex, sbk, 1:2], scalar2=None, op0=ALU.mult),
                             reads=[pt.r, sinfo.r], writes=[y.r])
                if ex + 1 < NE:
                    load_dn(ex + 1)
                for sbk in range(4):
                    y = yb[sbk]
                    P.op("gpsimd", lambda e, s, y=y, ex=ex, sbk=sbk: e.indirect_dma_start(
                        out=hdst.rearrange("b s d -> (b s) d"), out_offset=bass.IndirectOffsetOnAxis(ap=sidx_s[:, b, ex, sbk:sbk + 1], axis=0),
                        in_=y[:], in_offset=None, compute_op=ALU.add).then_inc(s, 16),
                        reads=[y.r, sidx_s.r] + (allh_dst[b] if sbk > 0 else []), writes=allh_dst[b] if sbk == 0 else [], dma=y.r)
                P.op("gpsimd", lambda e: e.nop(), reads=[yb[i].r.d for i in range(4)], writes=allh_dst[b])
            P.end_phase()

        def ple_layer(li, buf):
            P.begin_phase()
            N = NormCtx(8 + li)
            wg = sb("plewg", [128, 8, D], BF16)
            wp = sb("plewp", [128, 2, D], BF16)
            load_w_bf16(wg, ple_wg[li], parts=2)
            load_w_bf16(wp, ple_wp[li], parts=1)
            pl = [sb("plp%d" % i, [128, 4, PLE]) for i in range(2)]
            plb = sb("plpb", [128, 4, PLE], BF16)
            plT = sb("plpT", [128, 2, TG], BF16)
            gsb = [sb("plg%d" % i, [128, 512]) for i in range(2)]
            for b in range(NB):
                for g in range(NG):
                    hb = N.load_h(buf, b, g, g)
                    pk = pl[g % 2]
                    dma_in(pk, p_in[li, b * S + g * TG: b * S + (g + 1) * TG, :].rearrange("(j p) d -> p j d", p=128))
                    xb, xt = N.norm(g)
                    ob = N.obuf
                    P.op("vector", lambda e, pk=pk: e.tensor_copy(out=plb[:], in_=pk[:]), reads=[pk.r], writes=[plb.r])
                    for jt in range(4):
                        for c in range(2):
                            P.op("tensor", lambda e, jt=jt, c=c: e.transpose(out=pbf[:, c * 128:(c + 1) * 128], in_=plb[:, jt, c * 128:(c + 1) * 128], identity=ident_b[:]),
                                 reads=[plb.r, ident_b.r], writes=[pbf.r])
                        P.op("vector", lambda e, jt=jt: e.tensor_copy(out=plT[:, :, jt * 128:(jt + 1) * 128], in_=pbf[:, 0:256].rearrange("p (c t) -> p c t", c=2)),
                             reads=[pbf.r], writes=[plT.r])
                    for jt in range(4):
                        for nb in range(2):
                            pgt, ppt = ps[nb * 2], ps[nb * 2 + 1]
                            for kc in range(8):
                                mm(pgt[:, :], xt[:, kc, jt * 128:(jt + 1) * 128], wg[:, kc, nb * 512:(nb + 1) * 512], kc == 0, kc == 7, [xt.r, wg.r], [pgt.r])
                            for kc in range(2):
                                mm(ppt[:, :], plT[:, kc, jt * 128:(jt + 1) * 128], wp[:, kc, nb * 512:(nb + 1) * 512], kc == 0, kc == 1, [plT.r, wp.r], [ppt.r])
                            gs = gsb[nb]
                            P.op("scalar", lambda e, pgt=pgt, gs=gs: e.activation(out=gs[:], in_=pgt[:, :], func=AF.Sigmoid), reads=[pgt.r], writes=[gs.r])
                            P.op("vector", lambda e, ppt=ppt, gs=gs: e.tensor_tensor(out=gs[:], in0=gs[:], in1=ppt[:, :], op=ALU.mult), reads=[ppt.r, gs.r], writes=[gs.r])
                            P.op("vector", lambda e, gs=gs, jt=jt, nb=nb, hb=hb: e.tensor_tensor(out=ob[:, jt, nb * 512:(nb + 1) * 512], in0=gs[:],
                                                                                             in1=hb[:, jt, nb * 512:(nb + 1) * 512], op=ALU.add),
                                 reads=[gs.r, hb.r], writes=[ob.r])
                    N.store_o(buf, b, g)
            P.end_phase()

        def final_norm(buf):
            P.begin_phase()
            N = NormCtx(None)
            gv = sb("gvf", [128, D])
            dma_in(gv, gvec[12])
            for b in range(NB):
                for g in range(NG):
                    hb = N.load_h(buf, b, g, g)
                    st = N.stats(g)
                    ob = N.obuf
                    for j in range(4):
                        P.op("vector", lambda e, j=j, hb=hb, st=st: e.scalar_tensor_tensor(out=ob[:, j, :], in0=hb[:, j, :], scalar=st[:, 4 + j:5 + j], in1=gv[:],
                                                                                          op0=ALU.mult, op1=ALU.mult), reads=[hb.r, st.r, gv.r], writes=[ob.r])
                    N.store_o("out", b, g)
            P.end_phase()

        cur = "x"
        for li in layers:
            P.epoch = li
            j = li // 2
            if li % 2 == 0:
                rg_layer(li, j, cur, "HB")
            else:
                ft_layer(li, j, cur, "HB")
            cur = "HB"
            if stop == "mix" and li == layers[-1]:
                break
            moe_layer(li, "HB", "HA")
            cur = "HA"
            if stop == "moe" and li == layers[-1]:
                break
            ple_layer(li, "HA")
        P.epoch = 4
        if final:
            final_norm(cur)
        else:
            P.begin_phase()
            N = NormCtx(None)
            for b in range(NB):
                for g in range(NG):
                    hb = N.load_h(cur, b, g, g)
                    P.op("vector", lambda e, hb=hb: e.tensor_copy(out=N.obuf[:], in_=hb[:]), reads=[hb.r], writes=[N.obuf.r])
                    N.store_o("out", b, g)
            P.end_phase()
        P.begin_phase()
        P.op("sync", lambda e: e.nop(), reads=[], writes=[])
        P.end_phase()
    return nc


def _consts():
    ident = np.eye(128, dtype=np.float32)
    iota = np.tile(np.arange(1, 513, dtype=np.float32)[None, :], (128, 1))
    tok = (np.arange(32)[None, :] * 128 + np.arange(128)[:, None]).astype(np.float32)
    n = np.arange(S, dtype=np.int64)
    m = (n[:, None] * n[None, :]) % S
    ang = 2.0 * np.pi * m.astype(np.float64) / S
    dft = np.stack([np.cos(ang), -np.sin(ang)]).astype(np.float32)
    c = np.arange(256, dtype=np.int64)
    mc = (c[:, None] * c[None, :]) % 256
    angc = 2.0 * np.pi * mc.astype(np.float64) / 256
    cd = np.stack([np.cos(angc), np.sin(angc)]).astype(np.float32)
    tok2 = np.stack([np.floor(tok / 64.0), np.mod(tok, 64.0)], axis=-1).astype(np.float32)
    tok2 = np.ascontiguousarray(np.broadcast_to(tok2[:, :, None, :], (128, 32, NE, 2)))
    return dict(c_ident=ident, c_iota=iota, c_tok=tok, c_tok2=tok2, c_dft=dft, c_cdft=cd)


def _prep(inputs, NB, layers=(0, 1, 2, 3)):
    f = lambda a: np.ascontiguousarray(np.asarray(a, dtype=np.float32))
    m = {}
    m["x"] = f(inputs["x"])[:NB].reshape(NB * S, D)
    m["p"] = f(inputs["p"])[:, :NB].reshape(4, NB * S, PLE)
    gvv = np.concatenate([f(inputs["g_mix"]), f(inputs["g_ffn"]), f(inputs["g_ple"]), f(inputs["g_final"])[None, :]], axis=0)
    m["gvec"] = np.ascontiguousarray(np.broadcast_to(gvv[:, None, :], (13, 128, D)))
    m["rg_w_in"] = f(inputs["rg_w_in"])
    m["rg_b_in"] = np.ascontiguousarray(f(inputs["rg_b_in"]).reshape(2, 16, 128).transpose(0, 2, 1))
    conv = np.concatenate([f(inputs["rg_conv_w"]), f(inputs["rg_conv_b"])[:, None, :]], axis=1)
    m["rg_conv"] = np.ascontiguousarray(conv.reshape(2, 5, 8, 128).transpose(0, 3, 1, 2))
    gw = np.stack([f(inputs["rg_gx_w"]), f(inputs["rg_ga_w"])], axis=2)
    m["rg_gw"] = np.ascontiguousarray(gw)
    gb = np.stack([f(inputs["rg_gx_b"]), f(inputs["rg_ga_b"])], axis=2)
    m["rg_gb"] = np.ascontiguousarray(gb.reshape(2, 2, 2, 8, 128).transpose(0, 4, 1, 2, 3))
    m["rg_lam"] = np.ascontiguousarray(f(inputs["rg_lam"]).reshape(2, 2, 8, 128).transpose(0, 3, 1, 2))
    m["rg_w_out"] = f(inputs["rg_w_out"])
    m["rg_b_out"] = np.ascontiguousarray(np.broadcast_to(f(inputs["rg_b_out"])[:, None, :], (2, 128, D)))
    for k in ("ft_w_in", "ft_w_out", "w_router", "ple_w_proj", "ple_w_gate"):
        m[k] = f(inputs[k])
    for k in ("w_gate", "w_up", "w_down"):
        m[k] = np.ascontiguousarray(np.asarray(inputs[k], dtype=np.float32)[list(layers)])
    m.update(_consts())
    return m


def kernel(**inputs):
    NB = 1
    layers = (0, 1, 2, 3)
    nc = build(NB, list(layers), final=True)
    base = _prep({**inputs, "x": np.asarray(inputs["x"])[0:1], "p": np.asarray(inputs["p"])[:, 0:1]}, NB, layers)
    in_maps = []
    for b in range(4):
        m = dict(base)
        m["x"] = np.ascontiguousarray(np.asarray(inputs["x"], dtype=np.float32)[b]).reshape(S, D)
        m["p"] = np.ascontiguousarray(np.asarray(inputs["p"], dtype=np.float32)[:, b]).reshape(4, S, PLE)
        in_maps.append(m)
    res = run_bass_kernel_spmd(nc, in_maps, core_ids=list(range(4)))
    out = np.stack([np.asarray(res.results[b]["out"], dtype=np.float32).reshape(S, D) for b in range(4)], axis=0)
    return out
```

```python
import math
import contextlib
import numpy as np
import concourse.bass as bass
import concourse.mybir as mybir
from concourse.bass_utils import run_bass_kernel_spmd

F32 = mybir.dt.float32
BF16 = mybir.dt.bfloat16
I32 = mybir.dt.int32
ALU = mybir.AluOpType
AF = mybir.ActivationFunctionType
AX = mybir.AxisListType

D = 1024
S = 4096
NE = 16
CAP = 512
FF = 2048
PLE = 256
EPS = 1e-6
TG = 512
NG = S // TG


class Res:
    def __init__(self, name):
        self.name = name
        self.w = None
        self.r = []
        self.ent = None
        self.d = None


class Op:
    __slots__ = ("eng", "fn", "deps", "dma", "sem", "val", "epoch", "has_dep", "phase")


class Prog:
    ENGS = ["sync", "scalar", "gpsimd", "vector", "tensor"]

    def __init__(self, nc, stack):
        self.nc = nc
        self.stack = stack
        self.ops = []
        self.epoch = 0
        self.esem = {}
        self.ecnt = {}
        self.dpool = []
        self.dnext = 0
        self.barrier = []
        self.phase = 0
        self.pstack = None
        self.used_ent = []

    def new_sem(self, name):
        return self.stack.enter_context(self.nc.semaphore(name))

    def begin_phase(self):
        self.phase += 1
        self.ops = []
        self.dnext = 0
        self.used_ent = []
        self.pstack = contextlib.ExitStack()

    def end_phase(self):
        self.emit()
        self.pstack.close()
        self.pstack = None

    def op(self, eng, fn, reads=(), writes=(), dma=None, ndma=1):
        o = Op()
        o.eng = eng
        o.fn = fn
        o.dma = dma
        o.epoch = self.epoch
        o.has_dep = False
        o.phase = self.phase
        deps = []
        reads = list(reads)
        writes = list(writes)
        if dma is not None:
            if dma.d is None:
                dma.d = Res(dma.name + ".d")
            writes.append(dma.d)
        for r in reads:
            if r.w is not None and r.w.phase == self.phase:
                deps.append(r.w)
        for w in writes:
            if w.w is not None and w.w.phase == self.phase:
                deps.append(w.w)
            for x in w.r:
                if x.phase == self.phase:
                    deps.append(x)
        for r in reads:
            r.r.append(o)
        for w in writes:
            w.w = o
            w.r = []
        o.deps = [d for d in deps if d is not o]
        if dma is not None:
            if dma.ent is None or dma.ent[2] != self.phase:
                if self.dnext >= len(self.dpool):
                    self.dpool.append([self.new_sem("dp%d" % len(self.dpool)), 0])
                pe = self.dpool[self.dnext]
                self.dnext += 1
                dma.ent = [pe, None, self.phase]
                self.used_ent.append(pe)
            pe = dma.ent[0]
            pe[1] += 16 * ndma
            o.sem = pe[0]
            o.val = pe[1]
        else:
            o.sem = None
            o.val = None
        self.ops.append(o)
        return o

    def emit(self):
        nc = self.nc
        ops = self.ops
        for o in ops:
            for x in o.deps:
                if x.dma is None and x.eng == "tensor" and o.eng == "tensor" and o.dma is None:
                    continue
                x.has_dep = True
        per_eng = {e: [o for o in ops if o.eng == e] for e in self.ENGS}
        for e in self.ENGS:
            comp = [o for o in per_eng[e] if o.dma is None]
            if comp:
                comp[-1].has_dep = True
        for o in ops:
            if o.dma is None and o.has_dep:
                key = (o.eng, o.epoch)
                if key not in self.esem:
                    self.esem[key] = self.new_sem("e_%s_%d" % key)
                    self.ecnt[key] = 0
                self.ecnt[key] += 1
                o.sem = self.esem[key]
                o.val = self.ecnt[key]
        barrier_in = list(self.barrier)
        with nc.Block() as block:
            def run(eng_name):
                def body(eng):
                    waited = {}
                    for (sm, vl) in barrier_in:
                        eng.wait_ge(sm, vl)
                        waited[id(sm)] = vl
                    for o in per_eng[eng_name]:
                        for x in o.deps:
                            if x.sem is None:
                                continue
                            if x.dma is None and x.eng == "tensor" and eng_name == "tensor" and o.dma is None:
                                continue
                            k = id(x.sem)
                            if waited.get(k, 0) >= x.val:
                                continue
                            eng.wait_ge(x.sem, x.val)
                            waited[k] = x.val
                        if o.dma is not None:
                            o.fn(eng, o.sem)
                        else:
                            ins = o.fn(eng)
                            if o.has_dep:
                                ins.then_inc(o.sem, 1)
                return body
            block.sync(run("sync"))
            block.scalar(run("scalar"))
            block.gpsimd(run("gpsimd"))
            block.vector(run("vector"))
            block.tensor(run("tensor"))
        bar = {}
        for (sm, vl) in barrier_in:
            bar[id(sm)] = (sm, vl)
        for e in self.ENGS:
            comp = [o for o in per_eng[e] if o.dma is None]
            if comp:
                bar[id(comp[-1].sem)] = (comp[-1].sem, comp[-1].val)
        for pe in self.used_ent:
            bar[id(pe[0])] = (pe[0], pe[1])
        self.barrier = list(bar.values())


class T:
    def __init__(self, P, name, shape, dtype, psum=False, persist=False):
        nc = P.nc
        st = P.stack if (persist or psum) else P.pstack
        nm = name if (persist or psum) else "%s_p%d" % (name, P.phase)
        if psum:
            self.t = st.enter_context(nc.psum_tensor(nm, shape, dtype))
        else:
            self.t = st.enter_context(nc.sbuf_tensor(nm, shape, dtype))
        self.r = Res(nm)

    def __getitem__(self, k):
        return self.t[k]


def build(NB, layers, final=True, stop=None):
    nc = bass.Bass("TRN2", target_bir_lowering=False)
    NTOK = NB * S
    NL = 4

    def din(name, shape, dt=F32):
        return nc.dram_tensor(name, list(shape), dt, kind="ExternalInput").ap()

    x_in = din("x", [NTOK, D])
    p_in = din("p", [NL, NTOK, PLE])
    gvec = din("gvec", [13, 128, D])
    rg_w_in = din("rg_w_in", [2, D, 2 * D])
    rg_b_in = din("rg_b_in", [2, 128, 16])
    rg_conv = din("rg_conv", [2, 128, 5, 8])
    rg_gw = din("rg_gw", [2, 2, 2, 4, 256, 256])
    rg_gb = din("rg_gb", [2, 128, 2, 2, 8])
    rg_lam = din("rg_lam", [2, 128, 2, 8])
    rg_w_out = din("rg_w_out", [2, D, D])
    rg_b_out = din("rg_b_out", [2, 128, D])
    ft_w_in = din("ft_w_in", [2, D, D])
    ft_w_out = din("ft_w_out", [2, D, D])
    w_router = din("w_router", [NL, D, NE])
    NLW = len(layers)
    lidx = {li: i for i, li in enumerate(layers)}
    w_gate = din("w_gate", [NLW, NE, D, FF])
    w_up = din("w_up", [NLW, NE, D, FF])
    w_down = din("w_down", [NLW, NE, FF, D])
    ple_wp = din("ple_w_proj", [NL, PLE, D])
    ple_wg = din("ple_w_gate", [NL, D, D])
    cident = din("c_ident", [128, 128])
    ciota = din("c_iota", [128, 512])
    ctok = din("c_tok", [128, 32])
    ctok2 = din("c_tok2", [128, 32, NE, 2])
    cdft = din("c_dft", [2, S, S])
    ccd = din("c_cdft", [2, 256, 256])
    out_d = nc.dram_tensor("out", [NTOK, D], F32, kind="ExternalOutput").ap()

    HA = nc.dram_tensor("HA", [NB, S + 128, D], F32).ap()
    HB = nc.dram_tensor("HB", [NB, S + 128, D], F32).ap()
    XN2 = nc.dram_tensor("XN2", [NB, S, D], BF16).ap()
    UT = nc.dram_tensor("UT", [NB, D, S], F32).ap()
    GT = nc.dram_tensor("GT", [NB, D, S], BF16).ap()
    YT = nc.dram_tensor("YT", [NB, D, S], BF16).ap()
    UD = nc.dram_tensor("UD", [NB, S, D], BF16).ap()

    with contextlib.ExitStack() as stack:
        P = Prog(nc, stack)

        def sb(name, shape, dt=F32, persist=False):
            return T(P, name, shape, dt, persist=persist)

        def mm(out_ap, lhsT, rhs, start, stop, reads, writes):
            P.op("tensor", lambda e: e.matmul(out_ap, lhsT, rhs, start=start, stop=stop), reads=reads, writes=writes)

        def dma_in(tile, ap, eng="sync", reads=()):
            P.op(eng, lambda e, s: e.dma_start(out=tile[:], in_=ap).then_inc(s, 16), reads=list(reads), writes=[tile.r], dma=tile.r)

        ps = [T(P, "ps%d" % i, [128, 512], F32, psum=True) for i in range(7)]
        pbf = T(P, "pbf", [128, 1024], BF16, psum=True)

        ident_f = sb("ident_f", [128, 128], persist=True)
        ident_b = sb("ident_b", [128, 128], BF16, persist=True)
        iota = sb("iota", [128, 512], persist=True)
        tokid = sb("tokid", [128, 32], persist=True)
        ones16 = sb("ones16", [128, NE], persist=True)
        afftm = sb("afftm", [128, NB, 32, NE], persist=True)
        sinfo = sb("sinfo", [128, NB, NE, 4, 3], persist=True)
        sidx_g = sb("sidx_g", [128, NB, NE, 4], I32, persist=True)
        sidx_s = sb("sidx_s", [128, NB, NE, 4], I32, persist=True)

        hres = {}
        for nm in ("HA", "HB", "XN2", "UD"):
            for b in range(NB):
                for g in range(NG):
                    hres[(nm, b, g)] = Res("%s_%d_%d" % (nm, b, g))
        xres = Res("x_in")
        outres = Res("out")
        utres = [[Res("UT%d_%d" % (b, c)) for c in range(8)] for b in range(NB)]
        gtres = [[Res("GT%d_%d" % (b, c)) for c in range(8)] for b in range(NB)]
        ytres = [[Res("YT%d_%d" % (b, c)) for c in range(8)] for b in range(NB)]
        HD = {"HA": HA, "HB": HB}

        def H_ap(buf, b, g):
            return HD[buf][b, g * TG:(g + 1) * TG, :].rearrange("(j p) d -> p j d", p=128)

        P.begin_phase()
        dma_in(ident_f, cident)
        dma_in(iota, ciota)
        dma_in(tokid, ctok)
        P.op("vector", lambda e: e.tensor_copy(out=ident_b[:], in_=ident_f[:]), reads=[ident_f.r], writes=[ident_b.r])
        P.op("vector", lambda e: e.memset(ones16[:], 1.0), writes=[ones16.r])
        P.end_phase()

        class NormCtx:
            def __init__(self, gi, nh=2, want_o=True):
                self.hbuf = [sb("hbuf%d" % i, [128, 4, D]) for i in range(nh)]
                self.nh = nh
                if gi is not None:
                    self.xnb = sb("xnb", [128, 4, D], BF16)
                    self.xnT = [sb("xnT%d" % i, [128, 8, TG], BF16) for i in range(2)]
                    self.gv = sb("gv", [128, D])
                    dma_in(self.gv, gvec[gi])
                self.stat = [sb("stat%d" % i, [128, 8]) for i in range(nh)]
                self.sqj = sb("sqj", [128, D])
                if want_o:
                    self.obuf = sb("obuf", [128, 4, D])

            def load_h(self, src, b, g, k):
                hb = self.hbuf[k % self.nh]
                if src == "x":
                    ap = x_in[b * S + g * TG: b * S + (g + 1) * TG, :].rearrange("(j p) d -> p j d", p=128)
                    rd = [xres]
                else:
                    ap = H_ap(src, b, g)
                    rd = [hres[(src, b, g)]]
                dma_in(hb, ap, reads=rd)
                return hb

            def stats(self, k):
                hb, st, sqj = self.hbuf[k % self.nh], self.stat[k % self.nh], self.sqj
                for j in range(4):
                    P.op("scalar", lambda e, j=j: e.activation(out=sqj[:], in_=hb[:, j, :], func=AF.Square, accum_out=st[:, j:j + 1]),
                         reads=[hb.r], writes=[sqj.r, st.r])
                P.op("scalar", lambda e: e.activation(out=st[:, 4:8], in_=st[:, 0:4], func=AF.Sqrt, bias=EPS, scale=1.0 / D),
                     reads=[st.r], writes=[st.r])
                P.op("vector", lambda e: e.reciprocal(out=st[:, 4:8], in_=st[:, 4:8]), reads=[st.r], writes=[st.r])
                return st

            def norm(self, k):
                hb, xb, xt, gv = self.hbuf[k % self.nh], self.xnb, self.xnT[k % 2], self.gv
                st = self.stats(k)
                for j in range(4):
                    P.op("vector", lambda e, j=j: e.scalar_tensor_tensor(out=xb[:, j, :], in0=hb[:, j, :], scalar=st[:, 4 + j:5 + j],
                                                                        in1=gv[:], op0=ALU.mult, op1=ALU.mult),
                         reads=[hb.r, st.r, gv.r], writes=[xb.r])
                for j in range(4):
                    for c in range(8):
                        P.op("tensor", lambda e, j=j, c=c: e.transpose(out=pbf[:, c * 128:(c + 1) * 128], in_=xb[:, j, c * 128:(c + 1) * 128],
                                                                      identity=ident_b[:]),
                             reads=[xb.r, ident_b.r], writes=[pbf.r])
                    P.op("vector", lambda e, j=j: e.tensor_copy(out=xt[:, :, j * 128:(j + 1) * 128],
                                                               in_=pbf[:, :].rearrange("p (c t) -> p c t", c=8)),
                         reads=[pbf.r], writes=[xt.r])
                return xb, xt

            def store_o(self, dst, b, g, extra_reads=()):
                ob = self.obuf
                if dst == "out":
                    ap = out_d[b * S + g * TG: b * S + (g + 1) * TG, :].rearrange("(j p) d -> p j d", p=128)
                    wr = [outres]
                else:
                    ap = H_ap(dst, b, g)
                    wr = [hres[(dst, b, g)]]
                P.op("sync", lambda e, s: e.dma_start(out=ap, in_=ob[:]).then_inc(s, 16), reads=[ob.r], writes=wr, dma=ob.r)

        def load_w_bf16(tile, ap, parts=1):
            kc = tile.t.shape[1]
            step = max(1, kc // parts)
            rng = list(range(0, kc, step))

            def fn(e, s):
                for c0 in rng:
                    e.dma_start(out=tile[:, c0:c0 + step, :], in_=ap[c0 * 128:(c0 + step) * 128, :].rearrange("(c p) f -> p c f", p=128)).then_inc(s, 16)
            P.op("gpsimd", fn, writes=[tile.r], dma=tile.r, ndma=len(rng))

        def rg_layer(li, j, src, dst):
            P.begin_phase()
            N = NormCtx(li, want_o=False)
            wA = sb("wA", [128, 8, 2048], BF16)
            load_w_bf16(wA, rg_w_in[j], parts=4)
            bin_ = sb("rgbin", [128, 16])
            dma_in(bin_, rg_b_in[j])
            fm_f = [sb("rgfmf%d" % i, [128, TG]) for i in range(2)]
            fm_t = [sb("rgfmt%d" % i, [128, TG]) for i in range(2)]
            fm_g = [sb("rgfmg%d" % i, [128, TG], BF16) for i in range(2)]
            for b in range(NB):
                def front(g):
                    N.load_h(src, b, g, g)
                    return N.norm(g)
                nxt = front(0)
                for g in range(NG):
                    xb, xt = nxt
                    if g + 1 < NG:
                        nxt = front(g + 1)
                    for fc in range(16):
                        pt = ps[fc % 4]
                        for kc in range(8):
                            mm(pt[:, :], wA[:, kc, fc * 128:(fc + 1) * 128], xt[:, kc, :], kc == 0, kc == 7, [wA.r, xt.r], [pt.r])
                        q = fc % 2
                        xg = fm_f[q]
                        P.op("scalar", lambda e, pt=pt, xg=xg, fc=fc: e.activation(out=xg[:], in_=pt[:, :], func=AF.Identity, bias=bin_[:, fc:fc + 1], scale=1.0),
                             reads=[pt.r, bin_.r], writes=[xg.r])
                        if fc < 8:
                            tt, gg = fm_t[q], fm_g[q]
                            P.op("vector", lambda e, xg=xg, tt=tt: e.tensor_tensor(out=tt[:], in0=xg[:], in1=xg[:], op=ALU.mult), reads=[xg.r], writes=[tt.r])
                            P.op("vector", lambda e, tt=tt: e.tensor_scalar(out=tt[:], in0=tt[:], scalar1=0.044715, scalar2=1.0, op0=ALU.mult, op1=ALU.add),
                                 reads=[tt.r], writes=[tt.r])
                            P.op("vector", lambda e, xg=xg, tt=tt: e.tensor_tensor(out=tt[:], in0=tt[:], in1=xg[:], op=ALU.mult), reads=[xg.r, tt.r], writes=[tt.r])
                            P.op("scalar", lambda e, tt=tt: e.activation(out=tt[:], in_=tt[:], func=AF.Sigmoid, scale=1.5957691216057308),
                                 reads=[tt.r], writes=[tt.r])
                            P.op("vector", lambda e, xg=xg, tt=tt, gg=gg: e.tensor_tensor(out=gg[:], in0=tt[:], in1=xg[:], op=ALU.mult),
                                 reads=[xg.r, tt.r], writes=[gg.r])
                            P.op("sync", lambda e, s, gg=gg, fc=fc, g=g, b=b: e.dma_start(out=GT[b, fc * 128:(fc + 1) * 128, g * TG:(g + 1) * TG], in_=gg[:]).then_inc(s, 16),
                                 reads=[gg.r], writes=[gtres[b][fc]], dma=gg.r)
                        else:
                            c = fc - 8
                            P.op("sync", lambda e, s, xg=xg, c=c, g=g, b=b: e.dma_start(out=UT[b, c * 128:(c + 1) * 128, g * TG:(g + 1) * TG], in_=xg[:]).then_inc(s, 16),
                                 reads=[xg.r], writes=[utres[b][c]], dma=xg.r)
            P.end_phase()

            P.begin_phase()
            gw = sb("rggw", [128, 2, 2, 4, 2, 256], BF16)

            def fn(e, s):
                for d_ in range(2):
                    for kd in range(2):
                        for h in range(4):
                            e.dma_start(out=gw[:, d_, kd, h, :, :],
                                        in_=rg_gw[j, d_, kd, h].rearrange("(c p) o -> p c o", p=128)).then_inc(s, 16)
            P.op("gpsimd", fn, writes=[gw.r], dma=gw.r, ndma=16)
            cw = sb("rgcw", [128, 5, 8])
            dma_in(cw, rg_conv[j])
            gb = sb("rggb", [128, 2, 2, 8])
            dma_in(gb, rg_gb[j])
            lam = sb("rglam", [128, 2, 8])
            dma_in(lam, rg_lam[j])
            c8 = sb("rgc8", [128, 2, 8])
            P.op("scalar", lambda e: e.activation(out=c8[:], in_=lam[:], func=AF.Exp, scale=-1.0), reads=[lam.r], writes=[c8.r])
            P.op("scalar", lambda e: e.activation(out=c8[:], in_=c8[:], func=AF.Ln, bias=1.0, scale=1.0), reads=[c8.r], writes=[c8.r])
            P.op("vector", lambda e: e.tensor_scalar(out=c8[:], in0=c8[:], scalar1=-8.0, scalar2=None, op0=ALU.mult), reads=[c8.r], writes=[c8.r])
            ue = sb("rgue", [128, 2, S + 4])
            uc = sb("rguc", [128, 2, S])
            ucb = sb("rgucb", [128, 2, S], BF16)
            Aa = sb("rgA", [128, S])
            Bb = sb("rgB", [128, S])
            hs = [sb("rghs%d" % i, [128, S]) for i in range(2)]
            gtl = sb("rggt", [128, S], BF16)
            yt = sb("rgyt", [128, S], BF16)
            ti = [sb("rgti%d" % i, [128, TG]) for i in range(2)]
            tr = [sb("rgtr%d" % i, [128, TG]) for i in range(2)]
            P.op("vector", lambda e: e.memset(ue[:, :, 0:2], 0.0), writes=[ue.r])
            P.op("vector", lambda e: e.memset(ue[:, :, S + 2:S + 4], 0.0), writes=[ue.r])
            for b in range(NB):
                for h in range(4):
                    for cc in range(2):
                        c = 2 * h + cc
                        P.op("sync", lambda e, s, c=c, cc=cc, b=b: e.dma_start(out=ue[:, cc, 2:S + 2], in_=UT[b, c * 128:(c + 1) * 128, :]).then_inc(s, 16),
                             reads=[utres[b][c]], writes=[ue.r], dma=ue.r)
                    for cc in range(2):
                        c = 2 * h + cc
                        P.op("vector", lambda e, c=c, cc=cc: e.tensor_scalar(out=uc[:, cc, :], in0=ue[:, cc, 0:S], scalar1=cw[:, 0, c:c + 1], scalar2=cw[:, 4, c:c + 1],
                                                                            op0=ALU.mult, op1=ALU.add), reads=[ue.r, cw.r], writes=[uc.r])
                        for kk in range(1, 4):
                            P.op("vector", lambda e, c=c, cc=cc, kk=kk: e.scalar_tensor_tensor(out=uc[:, cc, :], in0=ue[:, cc, kk:S + kk], scalar=cw[:, kk, c:c + 1],
                                                                                              in1=uc[:, cc, :], op0=ALU.mult, op1=ALU.add),
                                 reads=[ue.r, cw.r, uc.r], writes=[uc.r])
                        P.op("scalar", lambda e, cc=cc: e.copy(out=ucb[:, cc, :], in_=uc[:, cc, :]), reads=[uc.r], writes=[ucb.r])
                    for cc in range(2):
                        c = 2 * h + cc
                        P.op("sync", lambda e, s, c=c, b=b: e.dma_start(out=gtl[:], in_=GT[b, c * 128:(c + 1) * 128, :]).then_inc(s, 16),
                             reads=[gtres[b][c]], writes=[gtl.r], dma=gtl.r)
                        for d_ in range(2):
                            hd = hs[d_]
                            for tb in range(S // TG):
                                q = tb % 2
                                sl = slice(tb * TG, (tb + 1) * TG)
                                pi, pr = ps[4 + 0], ps[4 + 1]
                                for kc in range(2):
                                    mm(pi[:, :], gw[:, d_, 0, h, kc, cc * 128:(cc + 1) * 128], ucb[:, kc, sl], kc == 0, kc == 1, [gw.r, ucb.r], [pi.r])
                                for kc in range(2):
                                    mm(pr[:, :], gw[:, d_, 1, h, kc, cc * 128:(cc + 1) * 128], ucb[:, kc, sl], kc == 0, kc == 1, [gw.r, ucb.r], [pr.r])
                                tti, ttr = ti[q], tr[q]
                                P.op("scalar", lambda e, pi=pi, tti=tti, d_=d_, c=c: e.activation(out=tti[:], in_=pi[:, :], func=AF.Sigmoid, bias=gb[:, d_, 0, c:c + 1], scale=1.0),
                                     reads=[pi.r, gb.r], writes=[tti.r])
                                P.op("scalar", lambda e, pr=pr, ttr=ttr, d_=d_, c=c: e.activation(out=ttr[:], in_=pr[:, :], func=AF.Sigmoid, bias=gb[:, d_, 1, c:c + 1], scale=1.0),
                                     reads=[pr.r, gb.r], writes=[ttr.r])
                                P.op("scalar", lambda e, ttr=ttr, d_=d_, c=c, sl=sl: e.activation(out=Aa[:, sl], in_=ttr[:], func=AF.Exp, scale=c8[:, d_, c:c + 1]),
                                     reads=[ttr.r, c8.r], writes=[Aa.r])
                                P.op("vector", lambda e, ttr=ttr, sl=sl: e.tensor_tensor(out=ttr[:], in0=Aa[:, sl], in1=Aa[:, sl], op=ALU.mult), reads=[Aa.r], writes=[ttr.r])
                                P.op("scalar", lambda e, ttr=ttr: e.activation(out=ttr[:], in_=ttr[:], func=AF.Sqrt, bias=1.0, scale=-1.0), reads=[ttr.r], writes=[ttr.r])
                                P.op("vector", lambda e, tti=tti, ttr=ttr: e.tensor_tensor(out=tti[:], in0=tti[:], in1=ttr[:], op=ALU.mult), reads=[tti.r, ttr.r], writes=[tti.r])
                                P.op("vector", lambda e, tti=tti, cc=cc, sl=sl: e.tensor_tensor(out=Bb[:, sl], in0=tti[:], in1=uc[:, cc, sl], op=ALU.mult),
                                     reads=[tti.r, uc.r], writes=[Bb.r])
                            if d_ == 0:
                                P.op("vector", lambda e, hd=hd: e.tensor_tensor_scan(out=hd[:], data0=Aa[:], data1=Bb[:], initial=0.0, op0=ALU.mult, op1=ALU.add),
                                     reads=[Aa.r, Bb.r], writes=[hd.r])
                            else:
                                P.op("vector", lambda e, hd=hd: e.tensor_tensor_scan(out=hd[:, ::-1], data0=Aa[:, ::-1], data1=Bb[:, ::-1], initial=0.0,
                                                                                    op0=ALU.mult, op1=ALU.add),
                                     reads=[Aa.r, Bb.r], writes=[hd.r])
                        P.op("vector", lambda e: e.tensor_tensor(out=hs[0][:], in0=hs[0][:], in1=hs[1][:], op=ALU.add), reads=[hs[0].r, hs[1].r], writes=[hs[0].r])
                        P.op("vector", lambda e: e.tensor_tensor(out=yt[:], in0=hs[0][:], in1=gtl[:], op=ALU.mult), reads=[hs[0].r, gtl.r], writes=[yt.r])
                        P.op("sync", lambda e, s, c=c, b=b: e.dma_start(out=YT[b, c * 128:(c + 1) * 128, :], in_=yt[:]).then_inc(s, 16),
                             reads=[yt.r], writes=[ytres[b][c]], dma=yt.r)
            P.end_phase()

            P.begin_phase()
            N = NormCtx(None)
            wout = sb("rgwout", [128, 8, D], BF16)
            load_w_bf16(wout, rg_w_out[j], parts=2)
            bout = sb("rgbout", [128, D])
            dma_in(bout, rg_b_out[j])
            ylt = [sb("rgyl%d" % i, [128, 8, 128], BF16) for i in range(2)]
            n = 0
            for b in range(NB):
                for g in range(NG):
                    hb = N.load_h(src, b, g, g)
                    ob = N.obuf
                    for jt in range(4):
                        yl = ylt[n % 2]
                        n += 1
                        t0 = g * TG + jt * 128
                        P.op("sync", lambda e, s, yl=yl, t0=t0, b=b: e.dma_start(out=yl[:], in_=YT[b, :, t0:t0 + 128].rearrange("(c p) t -> p c t", p=128)).then_inc(s, 16),
                             reads=ytres[b], writes=[yl.r], dma=yl.r)
                        for nb in range(2):
                            pt = ps[nb]
                            for kc in range(8):
                                mm(pt[:, :], yl[:, kc, :], wout[:, kc, nb * 512:(nb + 1) * 512], kc == 0, kc == 7, [yl.r, wout.r], [pt.r])
                            P.op("vector", lambda e, pt=pt, jt=jt, nb=nb: e.tensor_tensor(out=ob[:, jt, nb * 512:(nb + 1) * 512], in0=pt[:, :],
                                                                                      in1=bout[:, nb * 512:(nb + 1) * 512], op=ALU.add),
                                 reads=[pt.r, bout.r], writes=[ob.r])
                        P.op("vector", lambda e, jt=jt, hb=hb: e.tensor_tensor(out=ob[:, jt, :], in0=ob[:, jt, :], in1=hb[:, jt, :], op=ALU.add),
                             reads=[ob.r, hb.r], writes=[ob.r])
                    N.store_o(dst, b, g)
            P.end_phase()

        def ft_layer(li, j, src, dst):
            P.begin_phase()
            N = NormCtx(li, want_o=False)
            win = sb("ftwin", [128, 8, D], BF16)
            load_w_bf16(win, ft_w_in[j], parts=2)
            ub = [sb("ftub%d" % i, [128, 4, D], BF16) for i in range(2)]
            for b in range(NB):
                def front(g):
                    N.load_h(src, b, g, g)
                    return N.norm(g)
                nxt = front(0)
                for g in range(NG):
                    xb, xt = nxt
                    if g + 1 < NG:
                        nxt = front(g + 1)
                    u = ub[g % 2]
                    for jt in range(4):
                        for nb in range(2):
                            pt = ps[nb]
                            for kc in range(8):
                                mm(pt[:, :], xt[:, kc, jt * 128:(jt + 1) * 128], win[:, kc, nb * 512:(nb + 1) * 512], kc == 0, kc == 7, [xt.r, win.r], [pt.r])
                            P.op("scalar", lambda e, pt=pt, u=u, jt=jt, nb=nb: e.copy(out=u[:, jt, nb * 512:(nb + 1) * 512], in_=pt[:, :]),
                                 reads=[pt.r], writes=[u.r])
                    P.op("sync", lambda e, s, u=u, b=b, g=g: e.dma_start(out=UD[b, g * TG:(g + 1) * TG, :].rearrange("(j p) d -> p j d", p=128), in_=u[:]).then_inc(s, 16),
                         reads=[u.r], writes=[hres[("UD", b, g)]], dma=u.r)
            P.end_phase()

            P.begin_phase()
            N = NormCtx(None)
            wout = sb("ftwout", [128, 8, D], BF16)
            load_w_bf16(wout, ft_w_out[j], parts=2)
            cdc = sb("ftcd", [128, 2, 2, 256], BF16)

            def fn(e, s):
                for q in range(2):
                    e.dma_start(out=cdc[:, q, :, :], in_=ccd[q].rearrange("(c p) o -> p c o", p=128)).then_inc(s, 16)
            P.op("gpsimd", fn, writes=[cdc.r], dma=cdc.r, ndma=2)
            U = sb("ftU", [128, 32, D], BF16)
            tab = [sb("fttab%d" % i, [128, 16, TG], BF16) for i in range(2)]
            YZ = sb("ftYZ", [128, 2, 8, TG], BF16)
            fT = sb("ftfT", [128, 8, TG], BF16)
            tabn = 0
            for b in range(NB):
                P.op("sync", lambda e, s, b=b: e.dma_start(out=U[:], in_=UD[b].rearrange("(c p) d -> p c d", p=128)).then_inc(s, 16),
                     reads=[hres[("UD", b, g)] for g in range(NG)], writes=[U.r], dma=U.r)
                for pb in range(NG):
                    for q in range(2):
                        for half in range(2):
                            for th in range(2):
                                tb = tab[tabn % 2]
                                tabn += 1

                                def fn(e, s, tb=tb, q=q, th=th, pb=pb):
                                    for c4 in range(4):
                                        r0 = (th * 16 + c4 * 4) * 128
                                        e.dma_start(out=tb[:, c4 * 4:(c4 + 1) * 4, :],
                                                    in_=cdft[q, r0:r0 + 512, pb * TG:(pb + 1) * TG].rearrange("(c p) n -> p c n", p=128)).then_inc(s, 16)
                                P.op("gpsimd", fn, writes=[tb.r], dma=tb.r, ndma=4)
                                for tc_ in range(16):
                                    tcg = th * 16 + tc_
                                    for c4 in range(4):
                                        ch = half * 4 + c4
                                        pt = ps[c4]
                                        mm(pt[:, :], U[:, tcg, ch * 128:(ch + 1) * 128], tb[:, tc_, :], tcg == 0, tcg == 31, [U.r, tb.r], [pt.r])
                            for c4 in range(4):
                                ch = half * 4 + c4
                                pt = ps[c4]
                                if c4 % 2:
                                    P.op("vector", lambda e, pt=pt, q=q, ch=ch: e.tensor_copy(out=YZ[:, q, ch, :], in_=pt[:, :]), reads=[pt.r], writes=[YZ.r])
                                else:
                                    P.op("scalar", lambda e, pt=pt, q=q, ch=ch: e.copy(out=YZ[:, q, ch, :], in_=pt[:, :]), reads=[pt.r], writes=[YZ.r])
                    for ch in range(8):
                        grp = ch // 2
                        pt = ps[4 + ch % 2]
                        n = 0
                        for q in range(2):
                            for kc in range(2):
                                mm(pt[:, :], cdc[:, q, kc, (ch % 2) * 128:(ch % 2 + 1) * 128], YZ[:, q, grp * 2 + kc, :], n == 0, n == 3, [cdc.r, YZ.r], [pt.r])
                                n += 1
                        P.op("vector", lambda e, pt=pt, ch=ch: e.tensor_scalar(out=fT[:, ch, :], in0=pt[:, :], scalar1=1.0 / 1024.0, scalar2=None, op0=ALU.mult),
                             reads=[pt.r], writes=[fT.r])
                    hb = N.load_h(src, b, pb, pb)
                    ob = N.obuf
                    for jt in range(4):
                        for nb in range(2):
                            pt = ps[nb]
                            for kc in range(8):
                                mm(pt[:, :], fT[:, kc, jt * 128:(jt + 1) * 128], wout[:, kc, nb * 512:(nb + 1) * 512], kc == 0, kc == 7, [fT.r, wout.r], [pt.r])
                            P.op("vector", lambda e, pt=pt, jt=jt, nb=nb, hb=hb: e.tensor_tensor(out=ob[:, jt, nb * 512:(nb + 1) * 512], in0=pt[:, :],
                                                                                             in1=hb[:, jt, nb * 512:(nb + 1) * 512], op=ALU.add),
                                 reads=[pt.r, hb.r], writes=[ob.r])
                    N.store_o(dst, b, pb)
            P.end_phase()

        def moe_layer(li, src, dst):
            P.begin_phase()
            N = NormCtx(4 + li, want_o=False)
            wr = sb("wr", [128, 8, NE])
            dma_in(wr, w_router[li].rearrange("(c p) n -> p c n", p=128))
            xnf = sb("xnf", [128, 4, D])
            xnTf = sb("xnTf", [128, 8, TG])
            sm = sb("smx", [128, 4, NE])
            ssum = sb("ssum", [128, 12])
            xbs = sb("xbs", [128, 4, D], BF16)
            for b in range(NB):
                for g in range(NG):
                    hb = N.load_h(src, b, g, g)
                    P.op("sync", lambda e, s, hb=hb, b=b, g=g: e.dma_start(out=H_ap(dst, b, g), in_=hb[:]).then_inc(s, 16),
                         reads=[hb.r], writes=[hres[(dst, b, g)]], dma=hb.r)
                    st = N.stats(g)
                    for jq in range(4):
                        P.op("vector", lambda e, jq=jq, hb=hb, st=st: e.scalar_tensor_tensor(out=xnf[:, jq, :], in0=hb[:, jq, :], scalar=st[:, 4 + jq:5 + jq],
                                                                                            in1=N.gv[:], op0=ALU.mult, op1=ALU.mult),
                             reads=[hb.r, st.r, N.gv.r], writes=[xnf.r])
                    P.op("scalar", lambda e: e.copy(out=xbs[:], in_=xnf[:]), reads=[xnf.r], writes=[xbs.r])
                    for jq in range(4):
                        for c0 in range(2):
                            ptf = ps[4 + c0]
                            for c in range(4):
                                P.op("tensor", lambda e, jq=jq, c0=c0, c=c, ptf=ptf: e.transpose(out=ptf[:, c * 128:(c + 1) * 128], in_=xnf[:, jq, (c0 * 4 + c) * 128:(c0 * 4 + c + 1) * 128],
                                                                                             identity=ident_f[:]),
                                     reads=[xnf.r, ident_f.r], writes=[ptf.r])
                            P.op("vector" if c0 else "scalar",
                                 (lambda e, jq=jq, c0=c0, ptf=ptf: e.tensor_copy(out=xnTf[:, c0 * 4:(c0 + 1) * 4, jq * 128:(jq + 1) * 128], in_=ptf[:, :].rearrange("p (c t) -> p c t", c=4))) if c0 else
                                 (lambda e, jq=jq, c0=c0, ptf=ptf: e.copy(out=xnTf[:, c0 * 4:(c0 + 1) * 4, jq * 128:(jq + 1) * 128], in_=ptf[:, :].rearrange("p (c t) -> p c t", c=4))),
                                 reads=[ptf.r], writes=[xnTf.r])
                    xt = xnTf
                    P.op("sync", lambda e, s, b=b, g=g: e.dma_start(out=XN2[b, g * TG:(g + 1) * TG, :].rearrange("(j p) d -> p j d", p=128), in_=xbs[:]).then_inc(s, 16),
                         reads=[xbs.r], writes=[hres[("XN2", b, g)]], dma=xbs.r)
                    pt = ps[6]
                    for jt in range(4):
                        for kc in range(8):
                            mm(pt[:, jt * NE:(jt + 1) * NE], xt[:, kc, jt * 128:(jt + 1) * 128], wr[:, kc, :], kc == 0, kc == 7, [xt.r, wr.r], [pt.r])
                    P.op("vector", lambda e, pt=pt: e.tensor_reduce(out=ssum[:, 0:4], in_=pt[:, 0:4 * NE].rearrange("p (j n) -> p j n", n=NE), axis=AX.X, op=ALU.max),
                         reads=[pt.r], writes=[ssum.r])
                    P.op("vector", lambda e: e.tensor_scalar(out=ssum[:, 0:4], in0=ssum[:, 0:4], scalar1=-1.0, scalar2=None, op0=ALU.mult), reads=[ssum.r], writes=[ssum.r])
                    for jt in range(4):
                        P.op("scalar", lambda e, pt=pt, jt=jt: e.activation(out=sm[:, jt, :], in_=pt[:, jt * NE:(jt + 1) * NE], func=AF.Exp, bias=ssum[:, jt:jt + 1], scale=1.0,
                                                                           accum_out=ssum[:, 4 + jt:5 + jt]),
                             reads=[pt.r, ssum.r], writes=[sm.r, ssum.r])
                    P.op("vector", lambda e: e.reciprocal(out=ssum[:, 8:12], in_=ssum[:, 4:8]), reads=[ssum.r], writes=[ssum.r])
                    for jt in range(4):
                        P.op("vector", lambda e, b=b, g=g, jt=jt: e.tensor_scalar(out=afftm[:, b, g * 4 + jt, :], in0=sm[:, jt, :], scalar1=ssum[:, 8 + jt:9 + jt], scalar2=None, op0=ALU.mult),
                             reads=[sm.r, ssum.r], writes=[afftm.r])
            P.end_phase()

            P.begin_phase()
            affT = sb("affT", [NE, S])
            msk = sb("msk", [NE, S])
            rnk = sb("rnk", [NE, S])
            onesr = sb("onesr", [NE, S])
            P.op("vector", lambda e: e.memset(onesr[:], 1.0), writes=[onesr.r])
            bs = sb("bs", [NE, 8])
            mrtm = sb("mrtm", [128, 2, 32, NE])
            sel = [sb("sel%d" % i, [128, CAP], BF16) for i in range(2)]
            rhs6 = sb("rhs6", [128, 32, NE, 6], BF16)
            tk2 = sb("tk2", [128, 32, NE, 2])
            dma_in(tk2, ctok2)
            rr = sb("rr", [128, 32, NE])
            s6 = [sb("s6_%d" % i, [6, CAP]) for i in range(2)]
            t6 = [sb("t6_%d" % i, [128, 4, 6]) for i in range(2)]
            for b in range(NB):
                for c in range(32):
                    pt = ps[4 + c % 2]
                    P.op("tensor", lambda e, pt=pt, b=b, c=c: e.transpose(out=pt[0:NE, 0:128], in_=afftm[:, b, c, :], identity=ident_f[:]),
                         reads=[afftm.r, ident_f.r], writes=[pt.r])
                    P.op("scalar", lambda e, pt=pt, c=c: e.copy(out=affT[:, c * 128:(c + 1) * 128], in_=pt[0:NE, 0:128]), reads=[pt.r], writes=[affT.r])
                P.op("vector", lambda e: e.memset(bs[:, 0:1], 0.0), writes=[bs.r])
                P.op("vector", lambda e: e.memset(bs[:, 1:2], 1.0), writes=[bs.r])
                for it in range(30):
                    P.op("vector", lambda e: e.tensor_tensor(out=bs[:, 2:3], in0=bs[:, 0:1], in1=bs[:, 1:2], op=ALU.add), reads=[bs.r], writes=[bs.r])
                    P.op("vector", lambda e: e.tensor_scalar(out=bs[:, 2:3], in0=bs[:, 2:3], scalar1=0.5, scalar2=None, op0=ALU.mult), reads=[bs.r], writes=[bs.r])
                    P.op("vector", lambda e: e.tensor_scalar(out=msk[:], in0=affT[:], scalar1=bs[:, 2:3], scalar2=0.0, op0=ALU.is_ge, op1=ALU.add, accum_out=bs[:, 3:4]),
                         reads=[affT.r, bs.r], writes=[msk.r, bs.r])
                    P.op("vector", lambda e: e.tensor_scalar(out=bs[:, 4:5], in0=bs[:, 3:4], scalar1=float(CAP), scalar2=None, op0=ALU.is_ge), reads=[bs.r], writes=[bs.r])
                    P.op("vector", lambda e: e.tensor_tensor(out=bs[:, 5:6], in0=bs[:, 2:3], in1=bs[:, 0:1], op=ALU.subtract), reads=[bs.r], writes=[bs.r])
                    P.op("vector", lambda e: e.tensor_tensor(out=bs[:, 6:7], in0=bs[:, 1:2], in1=bs[:, 2:3], op=ALU.subtract), reads=[bs.r], writes=[bs.r])
                    P.op("vector", lambda e: e.scalar_tensor_tensor(out=bs[:, 0:1], in0=bs[:, 5:6], scalar=bs[:, 4:5], in1=bs[:, 0:1], op0=ALU.mult, op1=ALU.add),
                         reads=[bs.r], writes=[bs.r])
                    P.op("vector", lambda e: e.scalar_tensor_tensor(out=bs[:, 1:2], in0=bs[:, 6:7], scalar=bs[:, 4:5], in1=bs[:, 2:3], op0=ALU.mult, op1=ALU.add),
                         reads=[bs.r], writes=[bs.r])
                P.op("vector", lambda e: e.tensor_scalar(out=msk[:], in0=affT[:], scalar1=bs[:, 0:1], scalar2=None, op0=ALU.is_ge), reads=[affT.r, bs.r], writes=[msk.r])
                P.op("vector", lambda e: e.tensor_tensor_scan(out=rnk[:], data0=onesr[:], data1=msk[:], initial=0.0, op0=ALU.mult, op1=ALU.add),
                     reads=[onesr.r, msk.r], writes=[rnk.r])
                for wi, src_t in enumerate((msk, rnk)):
                    pt = ps[4 + wi]
                    for c in range(32):
                        P.op("tensor", lambda e, pt=pt, c=c, src_t=src_t: e.transpose(out=pt[:, c * NE:(c + 1) * NE], in_=src_t[:, c * 128:(c + 1) * 128], identity=ident_f[0:NE, 0:NE]),
                             reads=[src_t.r, ident_f.r], writes=[pt.r])
                    P.op("vector", lambda e, pt=pt, wi=wi: e.tensor_copy(out=mrtm[:, wi, :, :], in_=pt[:, :].rearrange("p (c n) -> p c n", n=NE)),
                         reads=[pt.r], writes=[mrtm.r])
                P.op("vector", lambda e: e.tensor_copy(out=rhs6[:, :, :, 0:2], in_=tk2[:]), reads=[tk2.r], writes=[rhs6.r])
                P.op("vector", lambda e: e.memset(rhs6[:, :, :, 5], 1.0), writes=[rhs6.r])
                P.op("vector", lambda e, b=b: e.tensor_copy(out=rhs6[:, :, :, 2], in_=afftm[:, b, :, :]), reads=[afftm.r], writes=[rhs6.r])
                P.op("vector", lambda e, b=b: e.tensor_tensor(out=rr[:], in0=afftm[:, b, :, :], in1=rhs6[:, :, :, 2], op=ALU.subtract), reads=[afftm.r, rhs6.r], writes=[rr.r])
                P.op("vector", lambda e: e.tensor_copy(out=rhs6[:, :, :, 3], in_=rr[:]), reads=[rr.r], writes=[rhs6.r])
                P.op("vector", lambda e: e.tensor_tensor(out=rr[:], in0=rr[:], in1=rhs6[:, :, :, 3], op=ALU.subtract), reads=[rr.r, rhs6.r], writes=[rr.r])
                P.op("vector", lambda e: e.tensor_copy(out=rhs6[:, :, :, 4], in_=rr[:]), reads=[rr.r], writes=[rhs6.r])
                for ex in range(NE):
                    pt = ps[ex % 2]
                    for c in range(32):
                        sl_ = sel[c % 2]
                        P.op("vector", lambda e, sl_=sl_, c=c, ex=ex: e.tensor_scalar(out=sl_[:], in0=iota[:], scalar1=mrtm[:, 1, c, ex:ex + 1],
                                                                                    scalar2=mrtm[:, 0, c, ex:ex + 1], op0=ALU.is_equal, op1=ALU.mult),
                             reads=[iota.r, mrtm.r], writes=[sl_.r])
                        mm(pt[0:6, :], rhs6[:, c, ex, :], sl_[:], c == 0, c == 31, [sl_.r, rhs6.r], [pt.r])
                    s6k, t6k = s6[ex % 2], t6[ex % 2]
                    P.op("scalar", lambda e, pt=pt, s6k=s6k: e.copy(out=s6k[:], in_=pt[0:6, :]), reads=[pt.r], writes=[s6k.r])
                    ptT = ps[2 + ex % 2]
                    for sbk in range(4):
                        P.op("tensor", lambda e, ptT=ptT, s6k=s6k, sbk=sbk: e.transpose(out=ptT[:, sbk * 6:(sbk + 1) * 6], in_=s6k[:, sbk * 128:(sbk + 1) * 128], identity=ident_f[0:6, 0:6]),
                             reads=[s6k.r, ident_f.r], writes=[ptT.r])
                    P.op("vector", lambda e, ptT=ptT, t6k=t6k: e.tensor_copy(out=t6k[:], in_=ptT[:, 0:24].rearrange("p (s k) -> p s k", k=6)), reads=[ptT.r], writes=[t6k.r])
                    P.op("vector", lambda e, t6k=t6k, b=b, ex=ex: e.scalar_tensor_tensor(out=sinfo[:, b, ex, :, 0], in0=t6k[:, :, 0], scalar=64.0, in1=t6k[:, :, 1], op0=ALU.mult, op1=ALU.add),
                         reads=[t6k.r], writes=[sinfo.r])
                    P.op("vector", lambda e, t6k=t6k, b=b, ex=ex: e.tensor_tensor(out=sinfo[:, b, ex, :, 1], in0=t6k[:, :, 2], in1=t6k[:, :, 3], op=ALU.add), reads=[t6k.r], writes=[sinfo.r])
                    P.op("vector", lambda e, t6k=t6k, b=b, ex=ex: e.tensor_tensor(out=sinfo[:, b, ex, :, 1], in0=sinfo[:, b, ex, :, 1], in1=t6k[:, :, 4], op=ALU.add), reads=[t6k.r, sinfo.r], writes=[sinfo.r])
                    P.op("vector", lambda e, t6k=t6k, b=b, ex=ex: e.tensor_copy(out=sinfo[:, b, ex, :, 2], in_=t6k[:, :, 5]), reads=[t6k.r], writes=[sinfo.r])
                P.op("vector", lambda e, b=b: e.tensor_scalar(out=sinfo[:, b, :, :, 0], in0=sinfo[:, b, :, :, 0], scalar1=float(b * S), scalar2=None, op0=ALU.add),
                     reads=[sinfo.r], writes=[sinfo.r])
                P.op("vector", lambda e, b=b: e.tensor_copy(out=sidx_g[:, b, :, :], in_=sinfo[:, b, :, :, 0]), reads=[sinfo.r], writes=[sidx_g.r])
                P.op("vector", lambda e, b=b: e.tensor_scalar(out=sinfo[:, b, :, :, 2], in0=sinfo[:, b, :, :, 2], scalar1=-float(S), scalar2=float(S + b * 128), op0=ALU.mult, op1=ALU.add),
                     reads=[sinfo.r], writes=[sinfo.r])
                P.op("vector", lambda e, b=b: e.tensor_tensor(out=sinfo[:, b, :, :, 2], in0=sinfo[:, b, :, :, 2], in1=sinfo[:, b, :, :, 0], op=ALU.add),
                     reads=[sinfo.r], writes=[sinfo.r])
                P.op("vector", lambda e, b=b: e.tensor_copy(out=sidx_s[:, b, :, :], in_=sinfo[:, b, :, :, 2]), reads=[sinfo.r], writes=[sidx_s.r])
            P.end_phase()

            P.begin_phase()
            wA = [sb("wA%d" % h, [128, 8, FF // 2], BF16) for h in range(2)]
            wB = [sb("wB%d" % h, [128, 8, FF // 2], BF16) for h in range(2)]
            wC = [sb("wC%d" % h, [128, 8, D], BF16) for h in range(2)]
            xg = [sb("xg%d" % i, [128, 4, D], BF16) for i in range(2)]
            xgT = sb("xgT", [128, 8, CAP], BF16)
            hg = sb("hg", [128, 16, CAP], BF16)
            sg = [sb("sg%d" % i, [128, CAP]) for i in range(2)]
            yb = [sb("yb%d" % i, [128, D]) for i in range(4)]
            allh_dst = {b: [hres[(dst, b, g)] for g in range(NG)] for b in range(NB)}
            allx = {b: [hres[("XN2", b, g)] for g in range(NG)] for b in range(NB)}
            hdst = HD[dst]
            b = 0

            def load_half(tile, ap):
                def fn(e, s):
                    for c0 in (0, 4):
                        e.dma_start(out=tile[:, c0:c0 + 4, :], in_=ap[c0 * 128:(c0 + 4) * 128, :].rearrange("(c p) f -> p c f", p=128)).then_inc(s, 16)
                P.op("gpsimd", fn, writes=[tile.r], dma=tile.r, ndma=2)

            def load_gu(ex, h):
                load_half(wA[h], w_gate[lidx[li], ex][:, h * 1024:(h + 1) * 1024])
                load_half(wB[h], w_up[lidx[li], ex][:, h * 1024:(h + 1) * 1024])

            def load_dn(ex):
                for h in range(2):
                    load_half(wC[h], w_down[lidx[li], ex][h * 1024:(h + 1) * 1024, :])

            def gather(ex):
                xgk = xg[ex % 2]

                def fn(e, s):
                    for sbk in range(4):
                        e.indirect_dma_start(out=xgk[:, sbk, :], out_offset=None, in_=XN2.rearrange("b s d -> (b s) d"),
                                             in_offset=bass.IndirectOffsetOnAxis(ap=sidx_g[:, b, ex, sbk:sbk + 1], axis=0)).then_inc(s, 16)
                P.op("gpsimd", fn, reads=allx[b] + [sidx_g.r], writes=[xgk.r], dma=xgk.r, ndma=4)

            gather(0)
            load_gu(0, 0)
            load_gu(0, 1)
            load_dn(0)
            for ex in range(NE):
                xgk = xg[ex % 2]
                for sbk in range(4):
                    for c in range(8):
                        P.op("tensor", lambda e, sbk=sbk, c=c, xgk=xgk: e.transpose(out=pbf[:, c * 128:(c + 1) * 128], in_=xgk[:, sbk, c * 128:(c + 1) * 128], identity=ident_b[:]),
                             reads=[xgk.r, ident_b.r], writes=[pbf.r])
                    P.op("vector", lambda e, sbk=sbk: e.tensor_copy(out=xgT[:, :, sbk * 128:(sbk + 1) * 128], in_=pbf[:, :].rearrange("p (c t) -> p c t", c=8)),
                         reads=[pbf.r], writes=[xgT.r])
                if ex + 1 < NE:
                    gather(ex + 1)
                for fc in range(16):
                    h, f8 = fc // 8, fc % 8
                    pg, pu = ps[(fc % 2) * 2], ps[(fc % 2) * 2 + 1]
                    for kc in range(8):
                        mm(pg[:, :], wA[h][:, kc, f8 * 128:(f8 + 1) * 128], xgT[:, kc, :], kc == 0, kc == 7, [wA[h].r, xgT.r], [pg.r])
                    for kc in range(8):
                        mm(pu[:, :], wB[h][:, kc, f8 * 128:(f8 + 1) * 128], xgT[:, kc, :], kc == 0, kc == 7, [wB[h].r, xgT.r], [pu.r])
                    s_ = sg[fc % 2]
                    P.op("scalar", lambda e, pg=pg, s_=s_: e.activation(out=s_[:], in_=pg[:, :], func=AF.Silu), reads=[pg.r], writes=[s_.r])
                    P.op("vector", lambda e, pu=pu, s_=s_, fc=fc: e.tensor_tensor(out=hg[:, fc, :], in0=s_[:], in1=pu[:, :], op=ALU.mult),
                         reads=[pu.r, s_.r], writes=[hg.r])
                    if f8 == 7 and ex + 1 < NE:
                        load_gu(ex + 1, h)
                for sbk in range(4):
                    y = yb[sbk]
                    for nb in range(2):
                        pt = ps[4 + nb]
                        for fc in range(16):
                            mm(pt[:, :], hg[:, fc, sbk * 128:(sbk + 1) * 128], wC[fc // 8][:, fc % 8, nb * 512:(nb + 1) * 512], fc == 0, fc == 15,
                               [hg.r, wC[fc // 8].r], [pt.r])
                        P.op("vector", lambda e, pt=pt, y=y, ex=ex, sbk=sbk, nb=nb: e.tensor_scalar(out=y[:, nb * 512:(nb + 1) * 512], in0=pt[:, :],
                                                                                           scalar1=sinfo[:, b, ex, sbk, 1:2], scalar2=None, op0=ALU.mult),
                             reads=[pt.r, sinfo.r], writes=[y.r])
                if ex + 1 < NE:
                    load_dn(ex + 1)
                for sbk in range(4):
                    y = yb[sbk]
                    P.op("gpsimd", lambda e, s, y=y, ex=ex, sbk=sbk: e.indirect_dma_start(
                        out=hdst.rearrange("b s d -> (b s) d"), out_offset=bass.IndirectOffsetOnAxis(ap=sidx_s[:, b, ex, sbk:sbk + 1], axis=0),
                        in_=y[:], in_offset=None, compute_op=ALU.add).then_inc(s, 16),
                        reads=[y.r, sidx_s.r] + (allh_dst[b] if sbk > 0 else []), writes=allh_dst[b] if sbk == 0 else [], dma=y.r)
                P.op("gpsimd", lambda e: e.nop(), reads=[yb[i].r.d for i in range(4)], writes=allh_dst[b])
            P.end_phase()

        def ple_layer(li, buf):
            P.begin_phase()
            N = NormCtx(8 + li)
            wg = sb("plewg", [128, 8, D], BF16)
            wp = sb("plewp", [128, 2, D], BF16)
            load_w_bf16(wg, ple_wg[li], parts=2)
            load_w_bf16(wp, ple_wp[li], parts=1)
            pl = [sb("plp%d" % i, [128, 4, PLE]) for i in range(2)]
            plb = sb("plpb", [128, 4, PLE], BF16)
            plTs = [sb("plpT%d" % i, [128, 2, TG], BF16) for i in range(2)]
            gsb = [sb("plg%d" % i, [128, 512]) for i in range(2)]
            for b in range(NB):
                def front(g):
                    hb = N.load_h(buf, b, g, g)
                    pk = pl[g % 2]
                    plT = plTs[g % 2]
                    dma_in(pk, p_in[li, b * S + g * TG: b * S + (g + 1) * TG, :].rearrange("(j p) d -> p j d", p=128))
                    xb, xt = N.norm(g)
                    P.op("vector", lambda e, pk=pk: e.tensor_copy(out=plb[:], in_=pk[:]), reads=[pk.r], writes=[plb.r])
                    for jt in range(4):
                        for c in range(2):
                            P.op("tensor", lambda e, jt=jt, c=c: e.transpose(out=pbf[:, c * 128:(c + 1) * 128], in_=plb[:, jt, c * 128:(c + 1) * 128], identity=ident_b[:]),
                                 reads=[plb.r, ident_b.r], writes=[pbf.r])
                        P.op("vector", lambda e, jt=jt, plT=plT: e.tensor_copy(out=plT[:, :, jt * 128:(jt + 1) * 128], in_=pbf[:, 0:256].rearrange("p (c t) -> p c t", c=2)),
                             reads=[pbf.r], writes=[plT.r])
                    return hb, xt, plT
                nxt = front(0)
                for g in range(NG):
                    hb, xt, plT = nxt
                    if g + 1 < NG:
                        nxt = front(g + 1)
                    ob = N.obuf
                    for jt in range(4):
                        for nb in range(2):
                            pgt, ppt = ps[nb * 2], ps[nb * 2 + 1]
                            for kc in range(8):
                                mm(pgt[:, :], xt[:, kc, jt * 128:(jt + 1) * 128], wg[:, kc, nb * 512:(nb + 1) * 512], kc == 0, kc == 7, [xt.r, wg.r], [pgt.r])
                            for kc in range(2):
                                mm(ppt[:, :], plT[:, kc, jt * 128:(jt + 1) * 128], wp[:, kc, nb * 512:(nb + 1) * 512], kc == 0, kc == 1, [plT.r, wp.r], [ppt.r])
                            gs = gsb[nb]
                            P.op("scalar", lambda e, pgt=pgt, gs=gs: e.activation(out=gs[:], in_=pgt[:, :], func=AF.Sigmoid), reads=[pgt.r], writes=[gs.r])
                            P.op("vector", lambda e, ppt=ppt, gs=gs: e.tensor_tensor(out=gs[:], in0=gs[:], in1=ppt[:, :], op=ALU.mult), reads=[ppt.r, gs.r], writes=[gs.r])
                            P.op("vector", lambda e, gs=gs, jt=jt, nb=nb, hb=hb: e.tensor_tensor(out=ob[:, jt, nb * 512:(nb + 1) * 512], in0=gs[:],
                                                                                             in1=hb[:, jt, nb * 512:(nb + 1) * 512], op=ALU.add),
                                 reads=[gs.r, hb.r], writes=[ob.r])
                    N.store_o(buf, b, g)
            P.end_phase()

        def final_norm(buf):
            P.begin_phase()
            N = NormCtx(None)
            gv = sb("gvf", [128, D])
            dma_in(gv, gvec[12])
            for b in range(NB):
                for g in range(NG):
                    hb = N.load_h(buf, b, g, g)
                    st = N.stats(g)
                    ob = N.obuf
                    for j in range(4):
                        P.op("vector", lambda e, j=j, hb=hb, st=st: e.scalar_tensor_tensor(out=ob[:, j, :], in0=hb[:, j, :], scalar=st[:, 4 + j:5 + j], in1=gv[:],
                                                                                          op0=ALU.mult, op1=ALU.mult), reads=[hb.r, st.r, gv.r], writes=[ob.r])
                    N.store_o("out", b, g)
            P.end_phase()

        cur = "x"
        for li in layers:
            P.epoch = li
            j = li // 2
            if li % 2 == 0:
                rg_layer(li, j, cur, "HB")
            else:
                ft_layer(li, j, cur, "HB")
            cur = "HB"
            if stop == "mix" and li == layers[-1]:
                break
            moe_layer(li, "HB", "HA")
            cur = "HA"
            if stop == "moe" and li == layers[-1]:
                break
            ple_layer(li, "HA")
        P.epoch = 4
        if final:
            final_norm(cur)
        else:
            P.begin_phase()
            N = NormCtx(None)
            for b in range(NB):
                for g in range(NG):
                    hb = N.load_h(cur, b, g, g)
                    P.op("vector", lambda e, hb=hb: e.tensor_copy(out=N.obuf[:], in_=hb[:]), reads=[hb.r], writes=[N.obuf.r])
                    N.store_o("out", b, g)
            P.end_phase()
        P.begin_phase()
        P.op("sync", lambda e: e.nop(), reads=[], writes=[])
        P.end_phase()
    return nc


def _consts():
    ident = np.eye(128, dtype=np.float32)
    iota = np.tile(np.arange(1, 513, dtype=np.float32)[None, :], (128, 1))
    tok = (np.arange(32)[None, :] * 128 + np.arange(128)[:, None]).astype(np.float32)
    n = np.arange(S, dtype=np.int64)
    m = (n[:, None] * n[None, :]) % S
    ang = 2.0 * np.pi * m.astype(np.float64) / S
    dft = np.stack([np.cos(ang), -np.sin(ang)]).astype(np.float32)
    c = np.arange(256, dtype=np.int64)
    mc = (c[:, None] * c[None, :]) % 256
    angc = 2.0 * np.pi * mc.astype(np.float64) / 256
    cd = np.stack([np.cos(angc), np.sin(angc)]).astype(np.float32)
    tok2 = np.stack([np.floor(tok / 64.0), np.mod(tok, 64.0)], axis=-1).astype(np.float32)
    tok2 = np.ascontiguousarray(np.broadcast_to(tok2[:, :, None, :], (128, 32, NE, 2)))
    return dict(c_ident=ident, c_iota=iota, c_tok=tok, c_tok2=tok2, c_dft=dft, c_cdft=cd)


def _prep(inputs, NB, layers=(0, 1, 2, 3)):
    f = lambda a: np.ascontiguousarray(np.asarray(a, dtype=np.float32))
    m = {}
    m["x"] = f(inputs["x"])[:NB].reshape(NB * S, D)
    m["p"] = f(inputs["p"])[:, :NB].reshape(4, NB * S, PLE)
    gvv = np.concatenate([f(inputs["g_mix"]), f(inputs["g_ffn"]), f(inputs["g_ple"]), f(inputs["g_final"])[None, :]], axis=0)
    m["gvec"] = np.ascontiguousarray(np.broadcast_to(gvv[:, None, :], (13, 128, D)))
    m["rg_w_in"] = f(inputs["rg_w_in"])
    m["rg_b_in"] = np.ascontiguousarray(f(inputs["rg_b_in"]).reshape(2, 16, 128).transpose(0, 2, 1))
    conv = np.concatenate([f(inputs["rg_conv_w"]), f(inputs["rg_conv_b"])[:, None, :]], axis=1)
    m["rg_conv"] = np.ascontiguousarray(conv.reshape(2, 5, 8, 128).transpose(0, 3, 1, 2))
    gw = np.stack([f(inputs["rg_gx_w"]), f(inputs["rg_ga_w"])], axis=2)
    m["rg_gw"] = np.ascontiguousarray(gw)
    gb = np.stack([f(inputs["rg_gx_b"]), f(inputs["rg_ga_b"])], axis=2)
    m["rg_gb"] = np.ascontiguousarray(gb.reshape(2, 2, 2, 8, 128).transpose(0, 4, 1, 2, 3))
    m["rg_lam"] = np.ascontiguousarray(f(inputs["rg_lam"]).reshape(2, 2, 8, 128).transpose(0, 3, 1, 2))
    m["rg_w_out"] = f(inputs["rg_w_out"])
    m["rg_b_out"] = np.ascontiguousarray(np.broadcast_to(f(inputs["rg_b_out"])[:, None, :], (2, 128, D)))
    for k in ("ft_w_in", "ft_w_out", "w_router", "ple_w_proj", "ple_w_gate"):
        m[k] = f(inputs[k])
    for k in ("w_gate", "w_up", "w_down"):
        m[k] = np.ascontiguousarray(np.asarray(inputs[k], dtype=np.float32)[list(layers)])
    m.update(_consts())
    return m


def kernel(**inputs):
    NB = 1
    layers = (0, 1, 2, 3)
    nc = build(NB, list(layers), final=True)
    base = _prep({**inputs, "x": np.asarray(inputs["x"])[0:1], "p": np.asarray(inputs["p"])[:, 0:1]}, NB, layers)
    in_maps = []
    for b in range(4):
        m = dict(base)
        m["x"] = np.ascontiguousarray(np.asarray(inputs["x"], dtype=np.float32)[b]).reshape(S, D)
        m["p"] = np.ascontiguousarray(np.asarray(inputs["p"], dtype=np.float32)[:, b]).reshape(4, S, PLE)
        in_maps.append(m)
    res = run_bass_kernel_spmd(nc, in_maps, core_ids=list(range(4)))
    out = np.stack([np.asarray(res.results[b]["out"], dtype=np.float32).reshape(S, D) for b in range(4)], axis=0)
    return out
```

```python
import math
import contextlib
import numpy as np
import concourse.bass as bass
import concourse.mybir as mybir
from concourse.bass_utils import run_bass_kernel_spmd

F32 = mybir.dt.float32
BF16 = mybir.dt.bfloat16
I32 = mybir.dt.int32
ALU = mybir.AluOpType
AF = mybir.ActivationFunctionType
AX = mybir.AxisListType

D = 1024
S = 4096
NE = 16
CAP = 512
FF = 2048
PLE = 256
EPS = 1e-6
TG = 512
NG = S // TG


class Res:
    def __init__(self, name):
        self.name = name
        self.w = None
        self.r = []
        self.ent = None
        self.d = None


class Op:
    __slots__ = ("eng", "fn", "deps", "dma", "sem", "val", "epoch", "has_dep", "phase")


class Prog:
    ENGS = ["sync", "scalar", "gpsimd", "vector", "tensor"]

    def __init__(self, nc, stack):
        self.nc = nc
        self.stack = stack
        self.ops = []
        self.epoch = 0
        self.esem = {}
        self.ecnt = {}
        self.dpool = []
        self.dnext = 0
        self.barrier = []
        self.phase = 0
        self.pstack = None
        self.used_ent = []

    def new_sem(self, name):
        return self.stack.enter_context(self.nc.semaphore(name))

    def begin_phase(self):
        self.phase += 1
        self.ops = []
        self.dnext = 0
        self.used_ent = []
        self.pstack = contextlib.ExitStack()

    def end_phase(self):
        self.emit()
        self.pstack.close()
        self.pstack = None

    def op(self, eng, fn, reads=(), writes=(), dma=None, ndma=1):
        o = Op()
        o.eng = eng
        o.fn = fn
        o.dma = dma
        o.epoch = self.epoch
        o.has_dep = False
        o.phase = self.phase
        deps = []
        reads = list(reads)
        writes = list(writes)
        if dma is not None:
            if dma.d is None:
                dma.d = Res(dma.name + ".d")
            writes.append(dma.d)
        for r in reads:
            if r.w is not None and r.w.phase == self.phase:
                deps.append(r.w)
        for w in writes:
            if w.w is not None and w.w.phase == self.phase:
                deps.append(w.w)
            for x in w.r:
                if x.phase == self.phase:
                    deps.append(x)
        for r in reads:
            r.r.append(o)
        for w in writes:
            w.w = o
            w.r = []
        o.deps = [d for d in deps if d is not o]
        if dma is not None:
            if dma.ent is None or dma.ent[2] != self.phase:
                if self.dnext >= len(self.dpool):
                    self.dpool.append([self.new_sem("dp%d" % len(self.dpool)), 0])
                pe = self.dpool[self.dnext]
                self.dnext += 1
                dma.ent = [pe, None, self.phase]
                self.used_ent.append(pe)
            pe = dma.ent[0]
            pe[1] += 16 * ndma
            o.sem = pe[0]
            o.val = pe[1]
        else:
            o.sem = None
            o.val = None
        self.ops.append(o)
        return o

    def emit(self):
        nc = self.nc
        ops = self.ops
        for o in ops:
            for x in o.deps:
                if x.dma is None and x.eng == "tensor" and o.eng == "tensor" and o.dma is None:
                    continue
                x.has_dep = True
        per_eng = {e: [o for o in ops if o.eng == e] for e in self.ENGS}
        for e in self.ENGS:
            comp = [o for o in per_eng[e] if o.dma is None]
            if comp:
                comp[-1].has_dep = True
        for o in ops:
            if o.dma is None and o.has_dep:
                key = (o.eng, o.epoch)
                if key not in self.esem:
                    self.esem[key] = self.new_sem("e_%s_%d" % key)
                    self.ecnt[key] = 0
                self.ecnt[key] += 1
                o.sem = self.esem[key]
                o.val = self.ecnt[key]
        barrier_in = list(self.barrier)
        with nc.Block() as block:
            def run(eng_name):
                def body(eng):
                    waited = {}
                    for (sm, vl) in barrier_in:
                        eng.wait_ge(sm, vl)
                        waited[id(sm)] = vl
                    for o in per_eng[eng_name]:
                        for x in o.deps:
                            if x.sem is None:
                                continue
                            if x.dma is None and x.eng == "tensor" and eng_name == "tensor" and o.dma is None:
                                continue
                            k = id(x.sem)
                            if waited.get(k, 0) >= x.val:
                                continue
                            eng.wait_ge(x.sem, x.val)
                            waited[k] = x.val
                        if o.dma is not None:
                            o.fn(eng, o.sem)
                        else:
                            ins = o.fn(eng)
                            if o.has_dep:
                                ins.then_inc(o.sem, 1)
                return body
            block.sync(run("sync"))
            block.scalar(run("scalar"))
            block.gpsimd(run("gpsimd"))
            block.vector(run("vector"))
            block.tensor(run("tensor"))
        bar = {}
        for (sm, vl) in barrier_in:
            bar[id(sm)] = (sm, vl)
        for e in self.ENGS:
            comp = [o for o in per_eng[e] if o.dma is None]
            if comp:
                bar[id(comp[-1].sem)] = (comp[-1].sem, comp[-1].val)
        for pe in self.used_ent:
            bar[id(pe[0])] = (pe[0], pe[1])
        self.barrier = list(bar.values())


class T:
    def __init__(self, P, name, shape, dtype, psum=False, persist=False):
        nc = P.nc
        st = P.stack if (persist or psum) else P.pstack
        nm = name if (persist or psum) else "%s_p%d" % (name, P.phase)
        if psum:
            self.t = st.enter_context(nc.psum_tensor(nm, shape, dtype))
        else:
            self.t = st.enter_context(nc.sbuf_tensor(nm, shape, dtype))
        self.r = Res(nm)

    def __getitem__(self, k):
        return self.t[k]


def build(NB, layers, final=True, stop=None):
    nc = bass.Bass("TRN2", target_bir_lowering=False)
    NTOK = NB * S
    NL = 4

    def din(name, shape, dt=F32):
        return nc.dram_tensor(name, list(shape), dt, kind="ExternalInput").ap()

    x_in = din("x", [NTOK, D])
    p_in = din("p", [NL, NTOK, PLE])
    gvec = din("gvec", [13, 128, D])
    rg_w_in = din("rg_w_in", [2, D, 2 * D])
    rg_b_in = din("rg_b_in", [2, 128, 16])
    rg_conv = din("rg_conv", [2, 128, 5, 8])
    rg_gw = din("rg_gw", [2, 2, 2, 4, 256, 256])
    rg_gb = din("rg_gb", [2, 128, 2, 2, 8])
    rg_lam = din("rg_lam", [2, 128, 2, 8])
    rg_w_out = din("rg_w_out", [2, D, D])
    rg_b_out = din("rg_b_out", [2, 128, D])
    ft_w_in = din("ft_w_in", [2, D, D])
    ft_w_out = din("ft_w_out", [2, D, D])
    w_router = din("w_router", [NL, D, NE])
    NLW = len(layers)
    lidx = {li: i for i, li in enumerate(layers)}
    w_gate = din("w_gate", [NLW, NE, D, FF])
    w_up = din("w_up", [NLW, NE, D, FF])
    w_down = din("w_down", [NLW, NE, FF, D])
    ple_wp = din("ple_w_proj", [NL, PLE, D])
    ple_wg = din("ple_w_gate", [NL, D, D])
    cident = din("c_ident", [128, 128])
    ciota = din("c_iota", [128, 512])
    ctok = din("c_tok", [128, 32])
    ctok2 = din("c_tok2", [128, 32, NE, 2])
    cdft = din("c_dft", [2, S, S])
    ccd = din("c_cdft", [2, 256, 256])
    out_d = nc.dram_tensor("out", [NTOK, D], F32, kind="ExternalOutput").ap()

    HA = nc.dram_tensor("HA", [NB, S + 128, D], F32).ap()
    HB = nc.dram_tensor("HB", [NB, S + 128, D], F32).ap()
    XN2 = nc.dram_tensor("XN2", [NB, S, D], BF16).ap()
    UT = nc.dram_tensor("UT", [NB, D, S], F32).ap()
    GT = nc.dram_tensor("GT", [NB, D, S], BF16).ap()
    YT = nc.dram_tensor("YT", [NB, D, S], BF16).ap()
    UD = nc.dram_tensor("UD", [NB, S, D], BF16).ap()

    with contextlib.ExitStack() as stack:
        P = Prog(nc, stack)

        def sb(name, shape, dt=F32, persist=False):
            return T(P, name, shape, dt, persist=persist)

        def mm(out_ap, lhsT, rhs, start, stop, reads, writes):
            P.op("tensor", lambda e: e.matmul(out_ap, lhsT, rhs, start=start, stop=stop), reads=reads, writes=writes)

        def dma_in(tile, ap, eng="sync", reads=()):
            P.op(eng, lambda e, s: e.dma_start(out=tile[:], in_=ap).then_inc(s, 16), reads=list(reads), writes=[tile.r], dma=tile.r)

        ps = [T(P, "ps%d" % i, [128, 512], F32, psum=True) for i in range(7)]
        pbf = T(P, "pbf", [128, 1024], BF16, psum=True)

        ident_f = sb("ident_f", [128, 128], persist=True)
        ident_b = sb("ident_b", [128, 128], BF16, persist=True)
        iota = sb("iota", [128, 512], persist=True)
        tokid = sb("tokid", [128, 32], persist=True)
        ones16 = sb("ones16", [128, NE], persist=True)
        afftm = sb("afftm", [128, NB, 32, NE], persist=True)
        sinfo = sb("sinfo", [128, NB, NE, 4, 3], persist=True)
        sidx_g = sb("sidx_g", [128, NB, NE, 4], I32, persist=True)
        sidx_s = sb("sidx_s", [128, NB, NE, 4], I32, persist=True)

        hres = {}
        for nm in ("HA", "HB", "XN2", "UD"):
            for b in range(NB):
                for g in range(NG):
                    hres[(nm, b, g)] = Res("%s_%d_%d" % (nm, b, g))
        xres = Res("x_in")
        outres = Res("out")
        utres = [[Res("UT%d_%d" % (b, c)) for c in range(8)] for b in range(NB)]
        gtres = [[Res("GT%d_%d" % (b, c)) for c in range(8)] for b in range(NB)]
        ytres = [[Res("YT%d_%d" % (b, c)) for c in range(8)] for b in range(NB)]
        HD = {"HA": HA, "HB": HB}

        def H_ap(buf, b, g):
            return HD[buf][b, g * TG:(g + 1) * TG, :].rearrange("(j p) d -> p j d", p=128)

        P.begin_phase()
        dma_in(ident_f, cident)
        dma_in(iota, ciota)
        dma_in(tokid, ctok)
        P.op("vector", lambda e: e.tensor_copy(out=ident_b[:], in_=ident_f[:]), reads=[ident_f.r], writes=[ident_b.r])
        P.op("vector", lambda e: e.memset(ones16[:], 1.0), writes=[ones16.r])
        P.end_phase()

        class NormCtx:
            def __init__(self, gi, nh=2, want_o=True):
                self.hbuf = [sb("hbuf%d" % i, [128, 4, D]) for i in range(nh)]
                self.nh = nh
                if gi is not None:
                    self.xnb = sb("xnb", [128, 4, D], BF16)
                    self.xnT = [sb("xnT%d" % i, [128, 8, TG], BF16) for i in range(2)]
                    self.gv = sb("gv", [128, D])
                    dma_in(self.gv, gvec[gi])
                self.stat = [sb("stat%d" % i, [128, 8]) for i in range(nh)]
                self.sqj = sb("sqj", [128, D])
                if want_o:
                    self.obuf = sb("obuf", [128, 4, D])

            def load_h(self, src, b, g, k):
                hb = self.hbuf[k % self.nh]
                if src == "x":
                    ap = x_in[b * S + g * TG: b * S + (g + 1) * TG, :].rearrange("(j p) d -> p j d", p=128)
                    rd = [xres]
                else:
                    ap = H_ap(src, b, g)
                    rd = [hres[(src, b, g)]]
                dma_in(hb, ap, reads=rd)
                return hb

            def stats(self, k):
                hb, st, sqj = self.hbuf[k % self.nh], self.stat[k % self.nh], self.sqj
                for j in range(4):
                    P.op("scalar", lambda e, j=j: e.activation(out=sqj[:], in_=hb[:, j, :], func=AF.Square, accum_out=st[:, j:j + 1]),
                         reads=[hb.r], writes=[sqj.r, st.r])
                P.op("scalar", lambda e: e.activation(out=st[:, 4:8], in_=st[:, 0:4], func=AF.Sqrt, bias=EPS, scale=1.0 / D),
                     reads=[st.r], writes=[st.r])
                P.op("vector", lambda e: e.reciprocal(out=st[:, 4:8], in_=st[:, 4:8]), reads=[st.r], writes=[st.r])
                return st

            def norm(self, k):
                hb, xb, xt, gv = self.hbuf[k % self.nh], self.xnb, self.xnT[k % 2], self.gv
                st = self.stats(k)
                for j in range(4):
                    P.op("vector", lambda e, j=j: e.scalar_tensor_tensor(out=xb[:, j, :], in0=hb[:, j, :], scalar=st[:, 4 + j:5 + j],
                                                                        in1=gv[:], op0=ALU.mult, op1=ALU.mult),
                         reads=[hb.r, st.r, gv.r], writes=[xb.r])
                for j in range(4):
                    for c in range(8):
                        P.op("tensor", lambda e, j=j, c=c: e.transpose(out=pbf[:, c * 128:(c + 1) * 128], in_=xb[:, j, c * 128:(c + 1) * 128],
                                                                      identity=ident_b[:]),
                             reads=[xb.r, ident_b.r], writes=[pbf.r])
                    P.op("vector", lambda e, j=j: e.tensor_copy(out=xt[:, :, j * 128:(j + 1) * 128],
                                                               in_=pbf[:, :].rearrange("p (c t) -> p c t", c=8)),
                         reads=[pbf.r], writes=[xt.r])
                return xb, xt

            def store_o(self, dst, b, g, extra_reads=()):
                ob = self.obuf
                if dst == "out":
                    ap = out_d[b * S + g * TG: b * S + (g + 1) * TG, :].rearrange("(j p) d -> p j d", p=128)
                    wr = [outres]
                else:
                    ap = H_ap(dst, b, g)
                    wr = [hres[(dst, b, g)]]
                P.op("sync", lambda e, s: e.dma_start(out=ap, in_=ob[:]).then_inc(s, 16), reads=[ob.r], writes=wr, dma=ob.r)

        def load_w_bf16(tile, ap, parts=1):
            kc = tile.t.shape[1]
            step = max(1, kc // parts)
            rng = list(range(0, kc, step))

            def fn(e, s):
                for c0 in rng:
                    e.dma_start(out=tile[:, c0:c0 + step, :], in_=ap[c0 * 128:(c0 + step) * 128, :].rearrange("(c p) f -> p c f", p=128)).then_inc(s, 16)
            P.op("gpsimd", fn, writes=[tile.r], dma=tile.r, ndma=len(rng))

        def rg_layer(li, j, src, dst):
            P.begin_phase()
            N = NormCtx(li, want_o=False)
            wA = sb("wA", [128, 8, 2048], BF16)
            load_w_bf16(wA, rg_w_in[j], parts=4)
            bin_ = sb("rgbin", [128, 16])
            dma_in(bin_, rg_b_in[j])
            fm_f = [sb("rgfmf%d" % i, [128, TG]) for i in range(2)]
            fm_t = [sb("rgfmt%d" % i, [128, TG]) for i in range(2)]
            fm_g = [sb("rgfmg%d" % i, [128, TG], BF16) for i in range(2)]
            for b in range(NB):
                def front(g):
                    N.load_h(src, b, g, g)
                    return N.norm(g)
                nxt = front(0)
                for g in range(NG):
                    xb, xt = nxt
                    if g + 1 < NG:
                        nxt = front(g + 1)
                    for fc in range(16):
                        pt = ps[fc % 4]
                        for kc in range(8):
                            mm(pt[:, :], wA[:, kc, fc * 128:(fc + 1) * 128], xt[:, kc, :], kc == 0, kc == 7, [wA.r, xt.r], [pt.r])
                        q = fc % 2
                        xg = fm_f[q]
                        P.op("scalar", lambda e, pt=pt, xg=xg, fc=fc: e.activation(out=xg[:], in_=pt[:, :], func=AF.Identity, bias=bin_[:, fc:fc + 1], scale=1.0),
                             reads=[pt.r, bin_.r], writes=[xg.r])
                        if fc < 8:
                            tt, gg = fm_t[q], fm_g[q]
                            P.op("vector", lambda e, xg=xg, tt=tt: e.tensor_tensor(out=tt[:], in0=xg[:], in1=xg[:], op=ALU.mult), reads=[xg.r], writes=[tt.r])
                            P.op("vector", lambda e, tt=tt: e.tensor_scalar(out=tt[:], in0=tt[:], scalar1=0.044715, scalar2=1.0, op0=ALU.mult, op1=ALU.add),
                                 reads=[tt.r], writes=[tt.r])
                            P.op("vector", lambda e, xg=xg, tt=tt: e.tensor_tensor(out=tt[:], in0=tt[:], in1=xg[:], op=ALU.mult), reads=[xg.r, tt.r], writes=[tt.r])
                            P.op("scalar", lambda e, tt=tt: e.activation(out=tt[:], in_=tt[:], func=AF.Sigmoid, scale=1.5957691216057308),
                                 reads=[tt.r], writes=[tt.r])
                            P.op("vector", lambda e, xg=xg, tt=tt, gg=gg: e.tensor_tensor(out=gg[:], in0=tt[:], in1=xg[:], op=ALU.mult),
                                 reads=[xg.r, tt.r], writes=[gg.r])
                            P.op("sync", lambda e, s, gg=gg, fc=fc, g=g, b=b: e.dma_start(out=GT[b, fc * 128:(fc + 1) * 128, g * TG:(g + 1) * TG], in_=gg[:]).then_inc(s, 16),
                                 reads=[gg.r], writes=[gtres[b][fc]], dma=gg.r)
                        else:
                            c = fc - 8
                            P.op("sync", lambda e, s, xg=xg, c=c, g=g, b=b: e.dma_start(out=UT[b, c * 128:(c + 1) * 128, g * TG:(g + 1) * TG], in_=xg[:]).then_inc(s, 16),
                                 reads=[xg.r], writes=[utres[b][c]], dma=xg.r)
            P.end_phase()

            P.begin_phase()
            gw = sb("rggw", [128, 2, 2, 4, 2, 256], BF16)

            def fn(e, s):
                for d_ in range(2):
                    for kd in range(2):
                        for h in range(4):
                            e.dma_start(out=gw[:, d_, kd, h, :, :],
                                        in_=rg_gw[j, d_, kd, h].rearrange("(c p) o -> p c o", p=128)).then_inc(s, 16)
            P.op("gpsimd", fn, writes=[gw.r], dma=gw.r, ndma=16)
            cw = sb("rgcw", [128, 5, 8])
            dma_in(cw, rg_conv[j])
            gb = sb("rggb", [128, 2, 2, 8])
            dma_in(gb, rg_gb[j])
            lam = sb("rglam", [128, 2, 8])
            dma_in(lam, rg_lam[j])
            c8 = sb("rgc8", [128, 2, 8])
            P.op("scalar", lambda e: e.activation(out=c8[:], in_=lam[:], func=AF.Exp, scale=-1.0), reads=[lam.r], writes=[c8.r])
            P.op("scalar", lambda e: e.activation(out=c8[:], in_=c8[:], func=AF.Ln, bias=1.0, scale=1.0), reads=[c8.r], writes=[c8.r])
            P.op("vector", lambda e: e.tensor_scalar(out=c8[:], in0=c8[:], scalar1=-8.0, scalar2=None, op0=ALU.mult), reads=[c8.r], writes=[c8.r])
            ue = sb("rgue", [128, 2, S + 4])
            uc = sb("rguc", [128, 2, S])
            ucb = sb("rgucb", [128, 2, S], BF16)
            Aa = sb("rgA", [128, S])
            Bb = sb("rgB", [128, S])
            hs = [sb("rghs%d" % i, [128, S]) for i in range(2)]
            gtl = sb("rggt", [128, S], BF16)
            yt = sb("rgyt", [128, S], BF16)
            ti = [sb("rgti%d" % i, [128, TG]) for i in range(2)]
            tr = [sb("rgtr%d" % i, [128, TG]) for i in range(2)]
            P.op("vector", lambda e: e.memset(ue[:, :, 0:2], 0.0), writes=[ue.r])
            P.op("vector", lambda e: e.memset(ue[:, :, S + 2:S + 4], 0.0), writes=[ue.r])
            for b in range(NB):
                for h in range(4):
                    for cc in range(2):
                        c = 2 * h + cc
                        P.op("sync", lambda e, s, c=c, cc=cc, b=b: e.dma_start(out=ue[:, cc, 2:S + 2], in_=UT[b, c * 128:(c + 1) * 128, :]).then_inc(s, 16),
                             reads=[utres[b][c]], writes=[ue.r], dma=ue.r)
                    for cc in range(2):
                        c = 2 * h + cc
                        P.op("vector", lambda e, c=c, cc=cc: e.tensor_scalar(out=uc[:, cc, :], in0=ue[:, cc, 0:S], scalar1=cw[:, 0, c:c + 1], scalar2=cw[:, 4, c:c + 1],
                                                                            op0=ALU.mult, op1=ALU.add), reads=[ue.r, cw.r], writes=[uc.r])
                        for kk in range(1, 4):
                            P.op("vector", lambda e, c=c, cc=cc, kk=kk: e.scalar_tensor_tensor(out=uc[:, cc, :], in0=ue[:, cc, kk:S + kk], scalar=cw[:, kk, c:c + 1],
                                                                                              in1=uc[:, cc, :], op0=ALU.mult, op1=ALU.add),
                                 reads=[ue.r, cw.r, uc.r], writes=[uc.r])
                        P.op("scalar", lambda e, cc=cc: e.copy(out=ucb[:, cc, :], in_=uc[:, cc, :]), reads=[uc.r], writes=[ucb.r])
                    for cc in range(2):
                        c = 2 * h + cc
                        P.op("sync", lambda e, s, c=c, b=b: e.dma_start(out=gtl[:], in_=GT[b, c * 128:(c + 1) * 128, :]).then_inc(s, 16),
                             reads=[gtres[b][c]], writes=[gtl.r], dma=gtl.r)
                        for d_ in range(2):
                            hd = hs[d_]
                            GRP = 2
                            for tg0 in range(0, S // TG, GRP):
                                tbs = list(range(tg0, tg0 + GRP))
                                sls = [slice(tb * TG, (tb + 1) * TG) for tb in tbs]
                                for k_, tb in enumerate(tbs):
                                    pi, pr = ps[2 * k_], ps[2 * k_ + 1]
                                    for kc in range(2):
                                        mm(pi[:, :], gw[:, d_, 0, h, kc, cc * 128:(cc + 1) * 128], ucb[:, kc, sls[k_]], kc == 0, kc == 1, [gw.r, ucb.r], [pi.r])
                                    for kc in range(2):
                                        mm(pr[:, :], gw[:, d_, 1, h, kc, cc * 128:(cc + 1) * 128], ucb[:, kc, sls[k_]], kc == 0, kc == 1, [gw.r, ucb.r], [pr.r])
                                for k_ in range(GRP):
                                    pi, pr, tti, ttr = ps[2 * k_], ps[2 * k_ + 1], ti[k_], tr[k_]
                                    P.op("scalar", lambda e, pi=pi, tti=tti, d_=d_, c=c: e.activation(out=tti[:], in_=pi[:, :], func=AF.Sigmoid, bias=gb[:, d_, 0, c:c + 1], scale=1.0),
                                         reads=[pi.r, gb.r], writes=[tti.r])
                                    P.op("scalar", lambda e, pr=pr, ttr=ttr, d_=d_, c=c: e.activation(out=ttr[:], in_=pr[:, :], func=AF.Sigmoid, bias=gb[:, d_, 1, c:c + 1], scale=1.0),
                                         reads=[pr.r, gb.r], writes=[ttr.r])
                                for k_ in range(GRP):
                                    ttr, sl = tr[k_], sls[k_]
                                    P.op("scalar", lambda e, ttr=ttr, d_=d_, c=c, sl=sl: e.activation(out=Aa[:, sl], in_=ttr[:], func=AF.Exp, scale=c8[:, d_, c:c + 1]),
                                         reads=[ttr.r, c8.r], writes=[Aa.r])
                                for k_ in range(GRP):
                                    ttr, sl = tr[k_], sls[k_]
                                    P.op("vector", lambda e, ttr=ttr, sl=sl: e.tensor_tensor(out=ttr[:], in0=Aa[:, sl], in1=Aa[:, sl], op=ALU.mult), reads=[Aa.r], writes=[ttr.r])
                                for k_ in range(GRP):
                                    ttr = tr[k_]
                                    P.op("scalar", lambda e, ttr=ttr: e.activation(out=ttr[:], in_=ttr[:], func=AF.Sqrt, bias=1.0, scale=-1.0), reads=[ttr.r], writes=[ttr.r])
                                for k_ in range(GRP):
                                    tti, ttr, sl = ti[k_], tr[k_], sls[k_]
                                    P.op("vector", lambda e, tti=tti, ttr=ttr: e.tensor_tensor(out=tti[:], in0=tti[:], in1=ttr[:], op=ALU.mult), reads=[tti.r, ttr.r], writes=[tti.r])
                                    P.op("vector", lambda e, tti=tti, cc=cc, sl=sl: e.tensor_tensor(out=Bb[:, sl], in0=tti[:], in1=uc[:, cc, sl], op=ALU.mult),
                                         reads=[tti.r, uc.r], writes=[Bb.r])
                            if d_ == 0:
                                P.op("vector", lambda e, hd=hd: e.tensor_tensor_scan(out=hd[:], data0=Aa[:], data1=Bb[:], initial=0.0, op0=ALU.mult, op1=ALU.add),
                                     reads=[Aa.r, Bb.r], writes=[hd.r])
                            else:
                                P.op("vector", lambda e, hd=hd: e.tensor_tensor_scan(out=hd[:, ::-1], data0=Aa[:, ::-1], data1=Bb[:, ::-1], initial=0.0,
                                                                                    op0=ALU.mult, op1=ALU.add),
                                     reads=[Aa.r, Bb.r], writes=[hd.r])
                        P.op("vector", lambda e: e.tensor_tensor(out=hs[0][:], in0=hs[0][:], in1=hs[1][:], op=ALU.add), reads=[hs[0].r, hs[1].r], writes=[hs[0].r])
                        P.op("vector", lambda e: e.tensor_tensor(out=yt[:], in0=hs[0][:], in1=gtl[:], op=ALU.mult), reads=[hs[0].r, gtl.r], writes=[yt.r])
                        P.op("sync", lambda e, s, c=c, b=b: e.dma_start(out=YT[b, c * 128:(c + 1) * 128, :], in_=yt[:]).then_inc(s, 16),
                             reads=[yt.r], writes=[ytres[b][c]], dma=yt.r)
            P.end_phase()

            P.begin_phase()
            N = NormCtx(None)
            wout = sb("rgwout", [128, 8, D], BF16)
            load_w_bf16(wout, rg_w_out[j], parts=2)
            bout = sb("rgbout", [128, D])
            dma_in(bout, rg_b_out[j])
            ylt = [sb("rgyl%d" % i, [128, 8, 128], BF16) for i in range(2)]
            n = 0
            for b in range(NB):
                for g in range(NG):
                    hb = N.load_h(src, b, g, g)
                    ob = N.obuf
                    for jt in range(4):
                        yl = ylt[n % 2]
                        n += 1
                        t0 = g * TG + jt * 128
                        P.op("sync", lambda e, s, yl=yl, t0=t0, b=b: e.dma_start(out=yl[:], in_=YT[b, :, t0:t0 + 128].rearrange("(c p) t -> p c t", p=128)).then_inc(s, 16),
                             reads=ytres[b], writes=[yl.r], dma=yl.r)
                        for nb in range(2):
                            pt = ps[nb]
                            for kc in range(8):
                                mm(pt[:, :], yl[:, kc, :], wout[:, kc, nb * 512:(nb + 1) * 512], kc == 0, kc == 7, [yl.r, wout.r], [pt.r])
                            P.op("vector", lambda e, pt=pt, jt=jt, nb=nb: e.tensor_tensor(out=ob[:, jt, nb * 512:(nb + 1) * 512], in0=pt[:, :],
                                                                                      in1=bout[:, nb * 512:(nb + 1) * 512], op=ALU.add),
                                 reads=[pt.r, bout.r], writes=[ob.r])
                        P.op("vector", lambda e, jt=jt, hb=hb: e.tensor_tensor(out=ob[:, jt, :], in0=ob[:, jt, :], in1=hb[:, jt, :], op=ALU.add),
                             reads=[ob.r, hb.r], writes=[ob.r])
                    N.store_o(dst, b, g)
            P.end_phase()

        def ft_layer(li, j, src, dst):
            P.begin_phase()
            N = NormCtx(li, want_o=False)
            win = sb("ftwin", [128, 8, D], BF16)
            load_w_bf16(win, ft_w_in[j], parts=2)
            ub = [sb("ftub%d" % i, [128, 4, D], BF16) for i in range(2)]
            for b in range(NB):
                def front(g):
                    N.load_h(src, b, g, g)
                    return N.norm(g)
                nxt = front(0)
                for g in range(NG):
                    xb, xt = nxt
                    if g + 1 < NG:
                        nxt = front(g + 1)
                    u = ub[g % 2]
                    for jt in range(4):
                        for nb in range(2):
                            pt = ps[nb]
                            for kc in range(8):
                                mm(pt[:, :], xt[:, kc, jt * 128:(jt + 1) * 128], win[:, kc, nb * 512:(nb + 1) * 512], kc == 0, kc == 7, [xt.r, win.r], [pt.r])
                            P.op("scalar", lambda e, pt=pt, u=u, jt=jt, nb=nb: e.copy(out=u[:, jt, nb * 512:(nb + 1) * 512], in_=pt[:, :]),
                                 reads=[pt.r], writes=[u.r])
                    P.op("sync", lambda e, s, u=u, b=b, g=g: e.dma_start(out=UD[b, g * TG:(g + 1) * TG, :].rearrange("(j p) d -> p j d", p=128), in_=u[:]).then_inc(s, 16),
                         reads=[u.r], writes=[hres[("UD", b, g)]], dma=u.r)
            P.end_phase()

            P.begin_phase()
            N = NormCtx(None)
            wout = sb("ftwout", [128, 8, D], BF16)
            load_w_bf16(wout, ft_w_out[j], parts=2)
            cdc = sb("ftcd", [128, 2, 2, 256], BF16)

            def fn(e, s):
                for q in range(2):
                    e.dma_start(out=cdc[:, q, :, :], in_=ccd[q].rearrange("(c p) o -> p c o", p=128)).then_inc(s, 16)
            P.op("gpsimd", fn, writes=[cdc.r], dma=cdc.r, ndma=2)
            U = sb("ftU", [128, 32, D], BF16)
            tab = [sb("fttab%d" % i, [128, 16, TG], BF16) for i in range(2)]
            YZ = sb("ftYZ", [128, 2, 8, TG], BF16)
            fT = sb("ftfT", [128, 8, TG], BF16)
            tabn = 0
            for b in range(NB):
                P.op("sync", lambda e, s, b=b: e.dma_start(out=U[:], in_=UD[b].rearrange("(c p) d -> p c d", p=128)).then_inc(s, 16),
                     reads=[hres[("UD", b, g)] for g in range(NG)], writes=[U.r], dma=U.r)
                for pb in range(NG):
                    for q in range(2):
                        for half in range(2):
                            for th in range(2):
                                tb = tab[tabn % 2]
                                tabn += 1

                                def fn(e, s, tb=tb, q=q, th=th, pb=pb):
                                    for c4 in range(4):
                                        r0 = (th * 16 + c4 * 4) * 128
                                        e.dma_start(out=tb[:, c4 * 4:(c4 + 1) * 4, :],
                                                    in_=cdft[q, r0:r0 + 512, pb * TG:(pb + 1) * TG].rearrange("(c p) n -> p c n", p=128)).then_inc(s, 16)
                                P.op("gpsimd", fn, writes=[tb.r], dma=tb.r, ndma=4)
                                for tc_ in range(16):
                                    tcg = th * 16 + tc_
                                    for c4 in range(4):
                                        ch = half * 4 + c4
                                        pt = ps[c4]
                                        mm(pt[:, :], U[:, tcg, ch * 128:(ch + 1) * 128], tb[:, tc_, :], tcg == 0, tcg == 31, [U.r, tb.r], [pt.r])
                            for c4 in range(4):
                                ch = half * 4 + c4
                                pt = ps[c4]
                                if c4 % 2:
                                    P.op("vector", lambda e, pt=pt, q=q, ch=ch: e.tensor_copy(out=YZ[:, q, ch, :], in_=pt[:, :]), reads=[pt.r], writes=[YZ.r])
                                else:
                                    P.op("scalar", lambda e, pt=pt, q=q, ch=ch: e.copy(out=YZ[:, q, ch, :], in_=pt[:, :]), reads=[pt.r], writes=[YZ.r])
                    for ch in range(8):
                        grp = ch // 2
                        pt = ps[4 + ch % 2]
                        n = 0
                        for q in range(2):
                            for kc in range(2):
                                mm(pt[:, :], cdc[:, q, kc, (ch % 2) * 128:(ch % 2 + 1) * 128], YZ[:, q, grp * 2 + kc, :], n == 0, n == 3, [cdc.r, YZ.r], [pt.r])
                                n += 1
                        P.op("vector", lambda e, pt=pt, ch=ch: e.tensor_scalar(out=fT[:, ch, :], in0=pt[:, :], scalar1=1.0 / 1024.0, scalar2=None, op0=ALU.mult),
                             reads=[pt.r], writes=[fT.r])
                    hb = N.load_h(src, b, pb, pb)
                    ob = N.obuf
                    for jt in range(4):
                        for nb in range(2):
                            pt = ps[nb]
                            for kc in range(8):
                                mm(pt[:, :], fT[:, kc, jt * 128:(jt + 1) * 128], wout[:, kc, nb * 512:(nb + 1) * 512], kc == 0, kc == 7, [fT.r, wout.r], [pt.r])
                            P.op("vector", lambda e, pt=pt, jt=jt, nb=nb, hb=hb: e.tensor_tensor(out=ob[:, jt, nb * 512:(nb + 1) * 512], in0=pt[:, :],
                                                                                             in1=hb[:, jt, nb * 512:(nb + 1) * 512], op=ALU.add),
                                 reads=[pt.r, hb.r], writes=[ob.r])
                    N.store_o(dst, b, pb)
            P.end_phase()

        def moe_layer(li, src, dst):
            P.begin_phase()
            N = NormCtx(4 + li, want_o=False)
            wr = sb("wr", [128, 8, NE])
            dma_in(wr, w_router[li].rearrange("(c p) n -> p c n", p=128))
            xnf = sb("xnf", [128, 4, D])
            xnTf = sb("xnTf", [128, 8, TG])
            sm = sb("smx", [128, 4, NE])
            ssum = sb("ssum", [128, 12])
            xbs = sb("xbs", [128, 4, D], BF16)
            for b in range(NB):
                for g in range(NG):
                    hb = N.load_h(src, b, g, g)
                    P.op("sync", lambda e, s, hb=hb, b=b, g=g: e.dma_start(out=H_ap(dst, b, g), in_=hb[:]).then_inc(s, 16),
                         reads=[hb.r], writes=[hres[(dst, b, g)]], dma=hb.r)
                    st = N.stats(g)
                    for jq in range(4):
                        P.op("vector", lambda e, jq=jq, hb=hb, st=st: e.scalar_tensor_tensor(out=xnf[:, jq, :], in0=hb[:, jq, :], scalar=st[:, 4 + jq:5 + jq],
                                                                                            in1=N.gv[:], op0=ALU.mult, op1=ALU.mult),
                             reads=[hb.r, st.r, N.gv.r], writes=[xnf.r])
                    P.op("scalar", lambda e: e.copy(out=xbs[:], in_=xnf[:]), reads=[xnf.r], writes=[xbs.r])
                    for jq in range(4):
                        for c0 in range(2):
                            ptf = ps[4 + c0]
                            for c in range(4):
                                P.op("tensor", lambda e, jq=jq, c0=c0, c=c, ptf=ptf: e.transpose(out=ptf[:, c * 128:(c + 1) * 128], in_=xnf[:, jq, (c0 * 4 + c) * 128:(c0 * 4 + c + 1) * 128],
                                                                                             identity=ident_f[:]),
                                     reads=[xnf.r, ident_f.r], writes=[ptf.r])
                            P.op("vector" if c0 else "scalar",
                                 (lambda e, jq=jq, c0=c0, ptf=ptf: e.tensor_copy(out=xnTf[:, c0 * 4:(c0 + 1) * 4, jq * 128:(jq + 1) * 128], in_=ptf[:, :].rearrange("p (c t) -> p c t", c=4))) if c0 else
                                 (lambda e, jq=jq, c0=c0, ptf=ptf: e.copy(out=xnTf[:, c0 * 4:(c0 + 1) * 4, jq * 128:(jq + 1) * 128], in_=ptf[:, :].rearrange("p (c t) -> p c t", c=4))),
                                 reads=[ptf.r], writes=[xnTf.r])
                    xt = xnTf
                    P.op("sync", lambda e, s, b=b, g=g: e.dma_start(out=XN2[b, g * TG:(g + 1) * TG, :].rearrange("(j p) d -> p j d", p=128), in_=xbs[:]).then_inc(s, 16),
                         reads=[xbs.r], writes=[hres[("XN2", b, g)]], dma=xbs.r)
                    pt = ps[6]
                    for jt in range(4):
                        for kc in range(8):
                            mm(pt[:, jt * NE:(jt + 1) * NE], xt[:, kc, jt * 128:(jt + 1) * 128], wr[:, kc, :], kc == 0, kc == 7, [xt.r, wr.r], [pt.r])
                    P.op("vector", lambda e, pt=pt: e.tensor_reduce(out=ssum[:, 0:4], in_=pt[:, 0:4 * NE].rearrange("p (j n) -> p j n", n=NE), axis=AX.X, op=ALU.max),
                         reads=[pt.r], writes=[ssum.r])
                    P.op("vector", lambda e: e.tensor_scalar(out=ssum[:, 0:4], in0=ssum[:, 0:4], scalar1=-1.0, scalar2=None, op0=ALU.mult), reads=[ssum.r], writes=[ssum.r])
                    for jt in range(4):
                        P.op("scalar", lambda e, pt=pt, jt=jt: e.activation(out=sm[:, jt, :], in_=pt[:, jt * NE:(jt + 1) * NE], func=AF.Exp, bias=ssum[:, jt:jt + 1], scale=1.0,
                                                                           accum_out=ssum[:, 4 + jt:5 + jt]),
                             reads=[pt.r, ssum.r], writes=[sm.r, ssum.r])
                    P.op("vector", lambda e: e.reciprocal(out=ssum[:, 8:12], in_=ssum[:, 4:8]), reads=[ssum.r], writes=[ssum.r])
                    for jt in range(4):
                        P.op("vector", lambda e, b=b, g=g, jt=jt: e.tensor_scalar(out=afftm[:, b, g * 4 + jt, :], in0=sm[:, jt, :], scalar1=ssum[:, 8 + jt:9 + jt], scalar2=None, op0=ALU.mult),
                             reads=[sm.r, ssum.r], writes=[afftm.r])
            P.end_phase()

            P.begin_phase()
            affT = sb("affT", [NE, S])
            msk = sb("msk", [NE, S])
            rnk = sb("rnk", [NE, S])
            onesr = sb("onesr", [NE, S])
            P.op("vector", lambda e: e.memset(onesr[:], 1.0), writes=[onesr.r])
            bs = sb("bs", [NE, 8])
            mrtm = sb("mrtm", [128, 2, 32, NE])
            sel = [sb("sel%d" % i, [128, CAP], BF16) for i in range(2)]
            rhs6 = sb("rhs6", [128, 32, NE, 6], BF16)
            tk2 = sb("tk2", [128, 32, NE, 2])
            dma_in(tk2, ctok2)
            rr = sb("rr", [128, 32, NE])
            s6 = [sb("s6_%d" % i, [6, CAP]) for i in range(2)]
            t6 = [sb("t6_%d" % i, [128, 4, 6]) for i in range(2)]
            for b in range(NB):
                for c in range(32):
                    pt = ps[4 + c % 2]
                    P.op("tensor", lambda e, pt=pt, b=b, c=c: e.transpose(out=pt[0:NE, 0:128], in_=afftm[:, b, c, :], identity=ident_f[:]),
                         reads=[afftm.r, ident_f.r], writes=[pt.r])
                    P.op("scalar", lambda e, pt=pt, c=c: e.copy(out=affT[:, c * 128:(c + 1) * 128], in_=pt[0:NE, 0:128]), reads=[pt.r], writes=[affT.r])
                P.op("vector", lambda e: e.memset(bs[:, 0:1], 0.0), writes=[bs.r])
                P.op("vector", lambda e: e.memset(bs[:, 1:2], 1.0), writes=[bs.r])
                for it in range(30):
                    P.op("vector", lambda e: e.tensor_tensor(out=bs[:, 2:3], in0=bs[:, 0:1], in1=bs[:, 1:2], op=ALU.add), reads=[bs.r], writes=[bs.r])
                    P.op("vector", lambda e: e.tensor_scalar(out=bs[:, 2:3], in0=bs[:, 2:3], scalar1=0.5, scalar2=None, op0=ALU.mult), reads=[bs.r], writes=[bs.r])
                    P.op("vector", lambda e: e.tensor_scalar(out=msk[:], in0=affT[:], scalar1=bs[:, 2:3], scalar2=0.0, op0=ALU.is_ge, op1=ALU.add, accum_out=bs[:, 3:4]),
                         reads=[affT.r, bs.r], writes=[msk.r, bs.r])
                    P.op("vector", lambda e: e.tensor_scalar(out=bs[:, 4:5], in0=bs[:, 3:4], scalar1=float(CAP), scalar2=None, op0=ALU.is_ge), reads=[bs.r], writes=[bs.r])
                    P.op("vector", lambda e: e.tensor_tensor(out=bs[:, 5:6], in0=bs[:, 2:3], in1=bs[:, 0:1], op=ALU.subtract), reads=[bs.r], writes=[bs.r])
                    P.op("vector", lambda e: e.tensor_tensor(out=bs[:, 6:7], in0=bs[:, 1:2], in1=bs[:, 2:3], op=ALU.subtract), reads=[bs.r], writes=[bs.r])
                    P.op("vector", lambda e: e.scalar_tensor_tensor(out=bs[:, 0:1], in0=bs[:, 5:6], scalar=bs[:, 4:5], in1=bs[:, 0:1], op0=ALU.mult, op1=ALU.add),
                         reads=[bs.r], writes=[bs.r])
                    P.op("vector", lambda e: e.scalar_tensor_tensor(out=bs[:, 1:2], in0=bs[:, 6:7], scalar=bs[:, 4:5], in1=bs[:, 2:3], op0=ALU.mult, op1=ALU.add),
                         reads=[bs.r], writes=[bs.r])
                P.op("vector", lambda e: e.tensor_scalar(out=msk[:], in0=affT[:], scalar1=bs[:, 0:1], scalar2=None, op0=ALU.is_ge), reads=[affT.r, bs.r], writes=[msk.r])
                P.op("vector", lambda e: e.tensor_tensor_scan(out=rnk[:], data0=onesr[:], data1=msk[:], initial=0.0, op0=ALU.mult, op1=ALU.add),
                     reads=[onesr.r, msk.r], writes=[rnk.r])
                for wi, src_t in enumerate((msk, rnk)):
                    pt = ps[4 + wi]
                    for c in range(32):
                        P.op("tensor", lambda e, pt=pt, c=c, src_t=src_t: e.transpose(out=pt[:, c * NE:(c + 1) * NE], in_=src_t[:, c * 128:(c + 1) * 128], identity=ident_f[0:NE, 0:NE]),
                             reads=[src_t.r, ident_f.r], writes=[pt.r])
                    P.op("vector", lambda e, pt=pt, wi=wi: e.tensor_copy(out=mrtm[:, wi, :, :], in_=pt[:, :].rearrange("p (c n) -> p c n", n=NE)),
                         reads=[pt.r], writes=[mrtm.r])
                P.op("vector", lambda e: e.tensor_copy(out=rhs6[:, :, :, 0:2], in_=tk2[:]), reads=[tk2.r], writes=[rhs6.r])
                P.op("vector", lambda e: e.memset(rhs6[:, :, :, 5], 1.0), writes=[rhs6.r])
                P.op("vector", lambda e, b=b: e.tensor_copy(out=rhs6[:, :, :, 2], in_=afftm[:, b, :, :]), reads=[afftm.r], writes=[rhs6.r])
                P.op("vector", lambda e, b=b: e.tensor_tensor(out=rr[:], in0=afftm[:, b, :, :], in1=rhs6[:, :, :, 2], op=ALU.subtract), reads=[afftm.r, rhs6.r], writes=[rr.r])
                P.op("vector", lambda e: e.tensor_copy(out=rhs6[:, :, :, 3], in_=rr[:]), reads=[rr.r], writes=[rhs6.r])
                P.op("vector", lambda e: e.tensor_tensor(out=rr[:], in0=rr[:], in1=rhs6[:, :, :, 3], op=ALU.subtract), reads=[rr.r, rhs6.r], writes=[rr.r])
                P.op("vector", lambda e: e.tensor_copy(out=rhs6[:, :, :, 4], in_=rr[:]), reads=[rr.r], writes=[rhs6.r])
                for ex in range(NE):
                    pt = ps[ex % 2]
                    for c in range(32):
                        sl_ = sel[c % 2]
                        P.op("vector", lambda e, sl_=sl_, c=c, ex=ex: e.tensor_scalar(out=sl_[:], in0=iota[:], scalar1=mrtm[:, 1, c, ex:ex + 1],
                                                                                    scalar2=mrtm[:, 0, c, ex:ex + 1], op0=ALU.is_equal, op1=ALU.mult),
                             reads=[iota.r, mrtm.r], writes=[sl_.r])
                        mm(pt[0:6, :], rhs6[:, c, ex, :], sl_[:], c == 0, c == 31, [sl_.r, rhs6.r], [pt.r])
                    s6k, t6k = s6[ex % 2], t6[ex % 2]
                    P.op("scalar", lambda e, pt=pt, s6k=s6k: e.copy(out=s6k[:], in_=pt[0:6, :]), reads=[pt.r], writes=[s6k.r])
                    ptT = ps[2 + ex % 2]
                    for sbk in range(4):
                        P.op("tensor", lambda e, ptT=ptT, s6k=s6k, sbk=sbk: e.transpose(out=ptT[:, sbk * 6:(sbk + 1) * 6], in_=s6k[:, sbk * 128:(sbk + 1) * 128], identity=ident_f[0:6, 0:6]),
                             reads=[s6k.r, ident_f.r], writes=[ptT.r])
                    P.op("vector", lambda e, ptT=ptT, t6k=t6k: e.tensor_copy(out=t6k[:], in_=ptT[:, 0:24].rearrange("p (s k) -> p s k", k=6)), reads=[ptT.r], writes=[t6k.r])
                    P.op("vector", lambda e, t6k=t6k, b=b, ex=ex: e.scalar_tensor_tensor(out=sinfo[:, b, ex, :, 0], in0=t6k[:, :, 0], scalar=64.0, in1=t6k[:, :, 1], op0=ALU.mult, op1=ALU.add),
                         reads=[t6k.r], writes=[sinfo.r])
                    P.op("vector", lambda e, t6k=t6k, b=b, ex=ex: e.tensor_tensor(out=sinfo[:, b, ex, :, 1], in0=t6k[:, :, 2], in1=t6k[:, :, 3], op=ALU.add), reads=[t6k.r], writes=[sinfo.r])
                    P.op("vector", lambda e, t6k=t6k, b=b, ex=ex: e.tensor_tensor(out=sinfo[:, b, ex, :, 1], in0=sinfo[:, b, ex, :, 1], in1=t6k[:, :, 4], op=ALU.add), reads=[t6k.r, sinfo.r], writes=[sinfo.r])
                    P.op("vector", lambda e, t6k=t6k, b=b, ex=ex: e.tensor_copy(out=sinfo[:, b, ex, :, 2], in_=t6k[:, :, 5]), reads=[t6k.r], writes=[sinfo.r])
                P.op("vector", lambda e, b=b: e.tensor_scalar(out=sinfo[:, b, :, :, 0], in0=sinfo[:, b, :, :, 0], scalar1=float(b * S), scalar2=None, op0=ALU.add),
                     reads=[sinfo.r], writes=[sinfo.r])
                P.op("vector", lambda e, b=b: e.tensor_copy(out=sidx_g[:, b, :, :], in_=sinfo[:, b, :, :, 0]), reads=[sinfo.r], writes=[sidx_g.r])
                P.op("vector", lambda e, b=b: e.tensor_scalar(out=sinfo[:, b, :, :, 2], in0=sinfo[:, b, :, :, 2], scalar1=-float(S), scalar2=float(S + b * 128), op0=ALU.mult, op1=ALU.add),
                     reads=[sinfo.r], writes=[sinfo.r])
                P.op("vector", lambda e, b=b: e.tensor_tensor(out=sinfo[:, b, :, :, 2], in0=sinfo[:, b, :, :, 2], in1=sinfo[:, b, :, :, 0], op=ALU.add),
                     reads=[sinfo.r], writes=[sinfo.r])
                P.op("vector", lambda e, b=b: e.tensor_copy(out=sidx_s[:, b, :, :], in_=sinfo[:, b, :, :, 2]), reads=[sinfo.r], writes=[sidx_s.r])
            P.end_phase()

            P.begin_phase()
            wA = [sb("wA%d" % h, [128, 8, FF // 2], BF16) for h in range(2)]
            wB = [sb("wB%d" % h, [128, 8, FF // 2], BF16) for h in range(2)]
            wC = [sb("wC%d" % h, [128, 8, D], BF16) for h in range(2)]
            xg = [sb("xg%d" % i, [128, 4, D], BF16) for i in range(2)]
            xgT = sb("xgT", [128, 8, CAP], BF16)
            hg = sb("hg", [128, 16, CAP], BF16)
            sg = [sb("sg%d" % i, [128, CAP]) for i in range(2)]
            yb = [sb("yb%d" % i, [128, D]) for i in range(4)]
            allh_dst = {b: [hres[(dst, b, g)] for g in range(NG)] for b in range(NB)}
            allx = {b: [hres[("XN2", b, g)] for g in range(NG)] for b in range(NB)}
            hdst = HD[dst]
            b = 0

            def load_half(tile, ap):
                def fn(e, s):
                    for c0 in (0, 4):
                        e.dma_start(out=tile[:, c0:c0 + 4, :], in_=ap[c0 * 128:(c0 + 4) * 128, :].rearrange("(c p) f -> p c f", p=128)).then_inc(s, 16)
                P.op("gpsimd", fn, writes=[tile.r], dma=tile.r, ndma=2)

            def load_gu(ex, h):
                load_half(wA[h], w_gate[lidx[li], ex][:, h * 1024:(h + 1) * 1024])
                load_half(wB[h], w_up[lidx[li], ex][:, h * 1024:(h + 1) * 1024])

            def load_dn(ex):
                for h in range(2):
                    load_half(wC[h], w_down[lidx[li], ex][h * 1024:(h + 1) * 1024, :])

            def gather(ex):
                xgk = xg[ex % 2]

                def fn(e, s):
                    for sbk in range(4):
                        e.indirect_dma_start(out=xgk[:, sbk, :], out_offset=None, in_=XN2.rearrange("b s d -> (b s) d"),
                                             in_offset=bass.IndirectOffsetOnAxis(ap=sidx_g[:, b, ex, sbk:sbk + 1], axis=0)).then_inc(s, 16)
                P.op("gpsimd", fn, reads=allx[b] + [sidx_g.r], writes=[xgk.r], dma=xgk.r, ndma=4)

            gather(0)
            load_gu(0, 0)
            load_gu(0, 1)
            load_dn(0)
            for ex in range(NE):
                xgk = xg[ex % 2]
                for sbk in range(4):
                    for c in range(8):
                        P.op("tensor", lambda e, sbk=sbk, c=c, xgk=xgk: e.transpose(out=pbf[:, c * 128:(c + 1) * 128], in_=xgk[:, sbk, c * 128:(c + 1) * 128], identity=ident_b[:]),
                             reads=[xgk.r, ident_b.r], writes=[pbf.r])
                    P.op("vector", lambda e, sbk=sbk: e.tensor_copy(out=xgT[:, :, sbk * 128:(sbk + 1) * 128], in_=pbf[:, :].rearrange("p (c t) -> p c t", c=8)),
                         reads=[pbf.r], writes=[xgT.r])
                if ex + 1 < NE:
                    gather(ex + 1)
                for fc in range(16):
                    h, f8 = fc // 8, fc % 8
                    pg, pu = ps[(fc % 2) * 2], ps[(fc % 2) * 2 + 1]
                    for kc in range(8):
                        mm(pg[:, :], wA[h][:, kc, f8 * 128:(f8 + 1) * 128], xgT[:, kc, :], kc == 0, kc == 7, [wA[h].r, xgT.r], [pg.r])
                    for kc in range(8):
                        mm(pu[:, :], wB[h][:, kc, f8 * 128:(f8 + 1) * 128], xgT[:, kc, :], kc == 0, kc == 7, [wB[h].r, xgT.r], [pu.r])
                    s_ = sg[fc % 2]
                    P.op("scalar", lambda e, pg=pg, s_=s_: e.activation(out=s_[:], in_=pg[:, :], func=AF.Silu), reads=[pg.r], writes=[s_.r])
                    P.op("vector", lambda e, pu=pu, s_=s_, fc=fc: e.tensor_tensor(out=hg[:, fc, :], in0=s_[:], in1=pu[:, :], op=ALU.mult),
                         reads=[pu.r, s_.r], writes=[hg.r])
                    if f8 == 7 and ex + 1 < NE:
                        load_gu(ex + 1, h)
                for sbk in range(4):
                    y = yb[sbk]
                    for nb in range(2):
                        pt = ps[4 + nb]
                        for fc in range(16):
                            mm(pt[:, :], hg[:, fc, sbk * 128:(sbk + 1) * 128], wC[fc // 8][:, fc % 8, nb * 512:(nb + 1) * 512], fc == 0, fc == 15,
                               [hg.r, wC[fc // 8].r], [pt.r])
                        P.op("vector", lambda e, pt=pt, y=y, ex=ex, sbk=sbk, nb=nb: e.tensor_scalar(out=y[:, nb * 512:(nb + 1) * 512], in0=pt[:, :],
                                                                                           scalar1=sinfo[:, b, ex, sbk, 1:2], scalar2=None, op0=ALU.mult),
                             reads=[pt.r, sinfo.r], writes=[y.r])
                if ex + 1 < NE:
                    load_dn(ex + 1)
                for sbk in range(4):
                    y = yb[sbk]
                    P.op("gpsimd", lambda e, s, y=y, ex=ex, sbk=sbk: e.indirect_dma_start(
                        out=hdst.rearrange("b s d -> (b s) d"), out_offset=bass.IndirectOffsetOnAxis(ap=sidx_s[:, b, ex, sbk:sbk + 1], axis=0),
                        in_=y[:], in_offset=None, compute_op=ALU.add).then_inc(s, 16),
                        reads=[y.r, sidx_s.r] + (allh_dst[b] if sbk > 0 else []), writes=allh_dst[b] if sbk == 0 else [], dma=y.r)
                P.op("gpsimd", lambda e: e.nop(), reads=[yb[i].r.d for i in range(4)], writes=allh_dst[b])
            P.end_phase()

        def ple_layer(li, buf):
            P.begin_phase()
            N = NormCtx(8 + li)
            wg = sb("plewg", [128, 8, D], BF16)
            wp = sb("plewp", [128, 2, D], BF16)
            load_w_bf16(wg, ple_wg[li], parts=2)
            load_w_bf16(wp, ple_wp[li], parts=1)
            pl = [sb("plp%d" % i, [128, 4, PLE]) for i in range(2)]
            plb = sb("plpb", [128, 4, PLE], BF16)
            plTs = [sb("plpT%d" % i, [128, 2, TG], BF16) for i in range(2)]
            gsb = [sb("plg%d" % i, [128, 512]) for i in range(2)]
            for b in range(NB):
                def front(g):
                    hb = N.load_h(buf, b, g, g)
                    pk = pl[g % 2]
                    plT = plTs[g % 2]
                    dma_in(pk, p_in[li, b * S + g * TG: b * S + (g + 1) * TG, :].rearrange("(j p) d -> p j d", p=128))
                    xb, xt = N.norm(g)
                    P.op("vector", lambda e, pk=pk: e.tensor_copy(out=plb[:], in_=pk[:]), reads=[pk.r], writes=[plb.r])
                    for jt in range(4):
                        for c in range(2):
                            P.op("tensor", lambda e, jt=jt, c=c: e.transpose(out=pbf[:, c * 128:(c + 1) * 128], in_=plb[:, jt, c * 128:(c + 1) * 128], identity=ident_b[:]),
                                 reads=[plb.r, ident_b.r], writes=[pbf.r])
                        P.op("vector", lambda e, jt=jt, plT=plT: e.tensor_copy(out=plT[:, :, jt * 128:(jt + 1) * 128], in_=pbf[:, 0:256].rearrange("p (c t) -> p c t", c=2)),
                             reads=[pbf.r], writes=[plT.r])
                    return hb, xt, plT
                nxt = front(0)
                for g in range(NG):
                    hb, xt, plT = nxt
                    if g + 1 < NG:
                        nxt = front(g + 1)
                    ob = N.obuf
                    for jt in range(4):
                        for nb in range(2):
                            pgt, ppt = ps[nb * 2], ps[nb * 2 + 1]
                            for kc in range(8):
                                mm(pgt[:, :], xt[:, kc, jt * 128:(jt + 1) * 128], wg[:, kc, nb * 512:(nb + 1) * 512], kc == 0, kc == 7, [xt.r, wg.r], [pgt.r])
                            for kc in range(2):
                                mm(ppt[:, :], plT[:, kc, jt * 128:(jt + 1) * 128], wp[:, kc, nb * 512:(nb + 1) * 512], kc == 0, kc == 1, [plT.r, wp.r], [ppt.r])
                            gs = gsb[nb]
                            P.op("scalar", lambda e, pgt=pgt, gs=gs: e.activation(out=gs[:], in_=pgt[:, :], func=AF.Sigmoid), reads=[pgt.r], writes=[gs.r])
                            P.op("vector", lambda e, ppt=ppt, gs=gs: e.tensor_tensor(out=gs[:], in0=gs[:], in1=ppt[:, :], op=ALU.mult), reads=[ppt.r, gs.r], writes=[gs.r])
                            P.op("vector", lambda e, gs=gs, jt=jt, nb=nb, hb=hb: e.tensor_tensor(out=ob[:, jt, nb * 512:(nb + 1) * 512], in0=gs[:],
                                                                                             in1=hb[:, jt, nb * 512:(nb + 1) * 512], op=ALU.add),
                                 reads=[gs.r, hb.r], writes=[ob.r])
                    N.store_o(buf, b, g)
            P.end_phase()

        def final_norm(buf):
            P.begin_phase()
            N = NormCtx(None)
            gv = sb("gvf", [128, D])
            dma_in(gv, gvec[12])
            for b in range(NB):
                for g in range(NG):
                    hb = N.load_h(buf, b, g, g)
                    st = N.stats(g)
                    ob = N.obuf
                    for j in range(4):
                        P.op("vector", lambda e, j=j, hb=hb, st=st: e.scalar_tensor_tensor(out=ob[:, j, :], in0=hb[:, j, :], scalar=st[:, 4 + j:5 + j], in1=gv[:],
                                                                                          op0=ALU.mult, op1=ALU.mult), reads=[hb.r, st.r, gv.r], writes=[ob.r])
                    N.store_o("out", b, g)
            P.end_phase()

        cur = "x"
        for li in layers:
            P.epoch = li
            j = li // 2
            if li % 2 == 0:
                rg_layer(li, j, cur, "HB")
            else:
                ft_layer(li, j, cur, "HB")
            cur = "HB"
            if stop == "mix" and li == layers[-1]:
                break
            moe_layer(li, "HB", "HA")
            cur = "HA"
            if stop == "moe" and li == layers[-1]:
                break
            ple_layer(li, "HA")
        P.epoch = 4
        if final:
            final_norm(cur)
        else:
            P.begin_phase()
            N = NormCtx(None)
            for b in range(NB):
                for g in range(NG):
                    hb = N.load_h(cur, b, g, g)
                    P.op("vector", lambda e, hb=hb: e.tensor_copy(out=N.obuf[:], in_=hb[:]), reads=[hb.r], writes=[N.obuf.r])
                    N.store_o("out", b, g)
            P.end_phase()
        P.begin_phase()
        P.op("sync", lambda e: e.nop(), reads=[], writes=[])
        P.end_phase()
    return nc


def _consts():
    ident = np.eye(128, dtype=np.float32)
    iota = np.tile(np.arange(1, 513, dtype=np.float32)[None, :], (128, 1))
    tok = (np.arange(32)[None, :] * 128 + np.arange(128)[:, None]).astype(np.float32)
    n = np.arange(S, dtype=np.int64)
    m = (n[:, None] * n[None, :]) % S
    ang = 2.0 * np.pi * m.astype(np.float64) / S
    dft = np.stack([np.cos(ang), -np.sin(ang)]).astype(np.float32)
    c = np.arange(256, dtype=np.int64)
    mc = (c[:, None] * c[None, :]) % 256
    angc = 2.0 * np.pi * mc.astype(np.float64) / 256
    cd = np.stack([np.cos(angc), np.sin(angc)]).astype(np.float32)
    tok2 = np.stack([np.floor(tok / 64.0), np.mod(tok, 64.0)], axis=-1).astype(np.float32)
    tok2 = np.ascontiguousarray(np.broadcast_to(tok2[:, :, None, :], (128, 32, NE, 2)))
    return dict(c_ident=ident, c_iota=iota, c_tok=tok, c_tok2=tok2, c_dft=dft, c_cdft=cd)


def _prep(inputs, NB, layers=(0, 1, 2, 3)):
    f = lambda a: np.ascontiguousarray(np.asarray(a, dtype=np.float32))
    m = {}
    m["x"] = f(inputs["x"])[:NB].reshape(NB * S, D)
    m["p"] = f(inputs["p"])[:, :NB].reshape(4, NB * S, PLE)
    gvv = np.concatenate([f(inputs["g_mix"]), f(inputs["g_ffn"]), f(inputs["g_ple"]), f(inputs["g_final"])[None, :]], axis=0)
    m["gvec"] = np.ascontiguousarray(np.broadcast_to(gvv[:, None, :], (13, 128, D)))
    m["rg_w_in"] = f(inputs["rg_w_in"])
    m["rg_b_in"] = np.ascontiguousarray(f(inputs["rg_b_in"]).reshape(2, 16, 128).transpose(0, 2, 1))
    conv = np.concatenate([f(inputs["rg_conv_w"]), f(inputs["rg_conv_b"])[:, None, :]], axis=1)
    m["rg_conv"] = np.ascontiguousarray(conv.reshape(2, 5, 8, 128).transpose(0, 3, 1, 2))
    gw = np.stack([f(inputs["rg_gx_w"]), f(inputs["rg_ga_w"])], axis=2)
    m["rg_gw"] = np.ascontiguousarray(gw)
    gb = np.stack([f(inputs["rg_gx_b"]), f(inputs["rg_ga_b"])], axis=2)
    m["rg_gb"] = np.ascontiguousarray(gb.reshape(2, 2, 2, 8, 128).transpose(0, 4, 1, 2, 3))
    m["rg_lam"] = np.ascontiguousarray(f(inputs["rg_lam"]).reshape(2, 2, 8, 128).transpose(0, 3, 1, 2))
    m["rg_w_out"] = f(inputs["rg_w_out"])
    m["rg_b_out"] = np.ascontiguousarray(np.broadcast_to(f(inputs["rg_b_out"])[:, None, :], (2, 128, D)))
    for k in ("ft_w_in", "ft_w_out", "w_router", "ple_w_proj", "ple_w_gate"):
        m[k] = f(inputs[k])
    for k in ("w_gate", "w_up", "w_down"):
        m[k] = np.ascontiguousarray(np.asarray(inputs[k], dtype=np.float32)[list(layers)])
    m.update(_consts())
    return m


def kernel(**inputs):
    NB = 1
    layers = (0, 1, 2, 3)
    nc = build(NB, list(layers), final=True)
    base = _prep({**inputs, "x": np.asarray(inputs["x"])[0:1], "p": np.asarray(inputs["p"])[:, 0:1]}, NB, layers)
    in_maps = []
    for b in range(4):
        m = dict(base)
        m["x"] = np.ascontiguousarray(np.asarray(inputs["x"], dtype=np.float32)[b]).reshape(S, D)
        m["p"] = np.ascontiguousarray(np.asarray(inputs["p"], dtype=np.float32)[:, b]).reshape(4, S, PLE)
        in_maps.append(m)
    res = run_bass_kernel_spmd(nc, in_maps, core_ids=list(range(4)))
    out = np.stack([np.asarray(res.results[b]["out"], dtype=np.float32).reshape(S, D) for b in range(4)], axis=0)
    return out
```

```python
import math
import contextlib
import numpy as np
import concourse.bass as bass
import concourse.mybir as mybir
from concourse.bass_utils import run_bass_kernel_spmd

F32 = mybir.dt.float32
BF16 = mybir.dt.bfloat16
I32 = mybir.dt.int32
ALU = mybir.AluOpType
AF = mybir.ActivationFunctionType
AX = mybir.AxisListType

D = 1024
S = 4096
NE = 16
CAP = 512
FF = 2048
PLE = 256
EPS = 1e-6
TG = 512
NG = S // TG


class Res:
    def __init__(self, name):
        self.name = name
        self.w = None
        self.r = []
        self.ent = None
        self.d = None


class Op:
    __slots__ = ("eng", "fn", "deps", "dma", "sem", "val", "epoch", "has_dep", "phase")


class Prog:
    ENGS = ["sync", "scalar", "gpsimd", "vector", "tensor"]

    def __init__(self, nc, stack):
        self.nc = nc
        self.stack = stack
        self.ops = []
        self.epoch = 0
        self.esem = {}
        self.ecnt = {}
        self.dpool = []
        self.dnext = 0
        self.barrier = []
        self.phase = 0
        self.pstack = None
        self.used_ent = []

    def new_sem(self, name):
        return self.stack.enter_context(self.nc.semaphore(name))

    def begin_phase(self):
        self.phase += 1
        self.ops = []
        self.dnext = 0
        self.used_ent = []
        self.pstack = contextlib.ExitStack()

    def end_phase(self):
        self.emit()
        self.pstack.close()
        self.pstack = None

    def op(self, eng, fn, reads=(), writes=(), dma=None, ndma=1):
        o = Op()
        o.eng = eng
        o.fn = fn
        o.dma = dma
        o.epoch = self.epoch
        o.has_dep = False
        o.phase = self.phase
        deps = []
        reads = list(reads)
        writes = list(writes)
        if dma is not None:
            if dma.d is None:
                dma.d = Res(dma.name + ".d")
            writes.append(dma.d)
        for r in reads:
            if r.w is not None and r.w.phase == self.phase:
                deps.append(r.w)
        for w in writes:
            if w.w is not None and w.w.phase == self.phase:
                deps.append(w.w)
            for x in w.r:
                if x.phase == self.phase:
                    deps.append(x)
        for r in reads:
            r.r.append(o)
        for w in writes:
            w.w = o
            w.r = []
        o.deps = [d for d in deps if d is not o]
        if dma is not None:
            if dma.ent is None or dma.ent[2] != self.phase:
                if self.dnext >= len(self.dpool):
                    self.dpool.append([self.new_sem("dp%d" % len(self.dpool)), 0])
                pe = self.dpool[self.dnext]
                self.dnext += 1
                dma.ent = [pe, None, self.phase]
                self.used_ent.append(pe)
            pe = dma.ent[0]
            pe[1] += 16 * ndma
            o.sem = pe[0]
            o.val = pe[1]
        else:
            o.sem = None
            o.val = None
        self.ops.append(o)
        return o

    def emit(self):
        nc = self.nc
        ops = self.ops
        for o in ops:
            for x in o.deps:
                if x.dma is None and x.eng == "tensor" and o.eng == "tensor" and o.dma is None:
                    continue
                x.has_dep = True
        per_eng = {e: [o for o in ops if o.eng == e] for e in self.ENGS}
        for e in self.ENGS:
            comp = [o for o in per_eng[e] if o.dma is None]
            if comp:
                comp[-1].has_dep = True
        for o in ops:
            if o.dma is None and o.has_dep:
                key = (o.eng, o.epoch)
                if key not in self.esem:
                    self.esem[key] = self.new_sem("e_%s_%d" % key)
                    self.ecnt[key] = 0
                self.ecnt[key] += 1
                o.sem = self.esem[key]
                o.val = self.ecnt[key]
        barrier_in = list(self.barrier)
        with nc.Block() as block:
            def run(eng_name):
                def body(eng):
                    waited = {}
                    for (sm, vl) in barrier_in:
                        eng.wait_ge(sm, vl)
                        waited[id(sm)] = vl
                    for o in per_eng[eng_name]:
                        for x in o.deps:
                            if x.sem is None:
                                continue
                            if x.dma is None and x.eng == "tensor" and eng_name == "tensor" and o.dma is None:
                                continue
                            k = id(x.sem)
                            if waited.get(k, 0) >= x.val:
                                continue
                            eng.wait_ge(x.sem, x.val)
                            waited[k] = x.val
                        if o.dma is not None:
                            o.fn(eng, o.sem)
                        else:
                            ins = o.fn(eng)
                            if o.has_dep:
                                ins.then_inc(o.sem, 1)
                return body
            block.sync(run("sync"))
            block.scalar(run("scalar"))
            block.gpsimd(run("gpsimd"))
            block.vector(run("vector"))
            block.tensor(run("tensor"))
        bar = {}
        for (sm, vl) in barrier_in:
            bar[id(sm)] = (sm, vl)
        for e in self.ENGS:
            comp = [o for o in per_eng[e] if o.dma is None]
            if comp:
                bar[id(comp[-1].sem)] = (comp[-1].sem, comp[-1].val)
        for pe in self.used_ent:
            bar[id(pe[0])] = (pe[0], pe[1])
        self.barrier = list(bar.values())


class T:
    def __init__(self, P, name, shape, dtype, psum=False, persist=False):
        nc = P.nc
        st = P.stack if (persist or psum) else P.pstack
        nm = name if (persist or psum) else "%s_p%d" % (name, P.phase)
        if psum:
            self.t = st.enter_context(nc.psum_tensor(nm, shape, dtype))
        else:
            self.t = st.enter_context(nc.sbuf_tensor(nm, shape, dtype))
        self.r = Res(nm)

    def __getitem__(self, k):
        return self.t[k]


def build(NB, layers, final=True, stop=None):
    nc = bass.Bass("TRN2", target_bir_lowering=False)
    NTOK = NB * S
    NL = 4

    def din(name, shape, dt=F32):
        return nc.dram_tensor(name, list(shape), dt, kind="ExternalInput").ap()

    x_in = din("x", [NTOK, D])
    p_in = din("p", [NL, NTOK, PLE])
    gvec = din("gvec", [13, 128, D])
    rg_w_in = din("rg_w_in", [2, D, 2 * D])
    rg_b_in = din("rg_b_in", [2, 128, 16])
    rg_conv = din("rg_conv", [2, 128, 5, 8])
    rg_gw = din("rg_gw", [2, 2, 2, 4, 256, 256])
    rg_gb = din("rg_gb", [2, 128, 2, 2, 8])
    rg_lam = din("rg_lam", [2, 128, 2, 8])
    rg_w_out = din("rg_w_out", [2, D, D])
    rg_b_out = din("rg_b_out", [2, 128, D])
    ft_w_in = din("ft_w_in", [2, D, D])
    ft_w_out = din("ft_w_out", [2, D, D])
    w_router = din("w_router", [NL, D, NE])
    NLW = len(layers)
    lidx = {li: i for i, li in enumerate(layers)}
    w_gate = din("w_gate", [NLW, NE, D, FF])
    w_up = din("w_up", [NLW, NE, D, FF])
    w_down = din("w_down", [NLW, NE, FF, D])
    ple_wp = din("ple_w_proj", [NL, PLE, D])
    ple_wg = din("ple_w_gate", [NL, D, D])
    cident = din("c_ident", [128, 128])
    ciota = din("c_iota", [128, 512])
    ctok = din("c_tok", [128, 32])
    ctok2 = din("c_tok2", [128, 32, NE, 2])
    cdft = din("c_dft", [2, S, S])
    ccd = din("c_cdft", [2, 256, 256])
    out_d = nc.dram_tensor("out", [NTOK, D], F32, kind="ExternalOutput").ap()

    HA = nc.dram_tensor("HA", [NB, S + 128, D], F32).ap()
    HB = nc.dram_tensor("HB", [NB, S + 128, D], F32).ap()
    XN2 = nc.dram_tensor("XN2", [NB, S, D], BF16).ap()
    UT = nc.dram_tensor("UT", [NB, D, S], F32).ap()
    GT = nc.dram_tensor("GT", [NB, D, S], BF16).ap()
    YT = nc.dram_tensor("YT", [NB, D, S], BF16).ap()
    UD = nc.dram_tensor("UD", [NB, S, D], BF16).ap()

    with contextlib.ExitStack() as stack:
        P = Prog(nc, stack)

        def sb(name, shape, dt=F32, persist=False):
            return T(P, name, shape, dt, persist=persist)

        def mm(out_ap, lhsT, rhs, start, stop, reads, writes):
            P.op("tensor", lambda e: e.matmul(out_ap, lhsT, rhs, start=start, stop=stop), reads=reads, writes=writes)

        def dma_in(tile, ap, eng="sync", reads=()):
            P.op(eng, lambda e, s: e.dma_start(out=tile[:], in_=ap).then_inc(s, 16), reads=list(reads), writes=[tile.r], dma=tile.r)

        ps = [T(P, "ps%d" % i, [128, 512], F32, psum=True) for i in range(7)]
        pbf = T(P, "pbf", [128, 1024], BF16, psum=True)

        ident_f = sb("ident_f", [128, 128], persist=True)
        ident_b = sb("ident_b", [128, 128], BF16, persist=True)
        iota = sb("iota", [128, 512], persist=True)
        tokid = sb("tokid", [128, 32], persist=True)
        ones16 = sb("ones16", [128, NE], persist=True)
        afftm = sb("afftm", [128, NB, 32, NE], persist=True)
        sinfo = sb("sinfo", [128, NB, NE, 4, 3], persist=True)
        sidx_g = sb("sidx_g", [128, NB, NE, 4], I32, persist=True)
        sidx_s = sb("sidx_s", [128, NB, NE, 4], I32, persist=True)

        hres = {}
        for nm in ("HA", "HB", "XN2", "UD"):
            for b in range(NB):
                for g in range(NG):
                    hres[(nm, b, g)] = Res("%s_%d_%d" % (nm, b, g))
        xres = Res("x_in")
        outres = Res("out")
        utres = [[Res("UT%d_%d" % (b, c)) for c in range(8)] for b in range(NB)]
        gtres = [[Res("GT%d_%d" % (b, c)) for c in range(8)] for b in range(NB)]
        ytres = [[Res("YT%d_%d" % (b, c)) for c in range(8)] for b in range(NB)]
        HD = {"HA": HA, "HB": HB}

        def H_ap(buf, b, g):
            return HD[buf][b, g * TG:(g + 1) * TG, :].rearrange("(j p) d -> p j d", p=128)

        P.begin_phase()
        dma_in(ident_f, cident)
        dma_in(iota, ciota)
        dma_in(tokid, ctok)
        P.op("vector", lambda e: e.tensor_copy(out=ident_b[:], in_=ident_f[:]), reads=[ident_f.r], writes=[ident_b.r])
        P.op("vector", lambda e: e.memset(ones16[:], 1.0), writes=[ones16.r])
        P.end_phase()

        class NormCtx:
            def __init__(self, gi, nh=2, want_o=True):
                self.hbuf = [sb("hbuf%d" % i, [128, 4, D]) for i in range(nh)]
                self.nh = nh
                if gi is not None:
                    self.xnb = sb("xnb", [128, 4, D], BF16)
                    self.xnT = [sb("xnT%d" % i, [128, 8, TG], BF16) for i in range(2)]
                    self.gv = sb("gv", [128, D])
                    dma_in(self.gv, gvec[gi])
                self.stat = [sb("stat%d" % i, [128, 8]) for i in range(nh)]
                self.sqj = sb("sqj", [128, D])
                if want_o:
                    self.obuf = sb("obuf", [128, 4, D])

            def load_h(self, src, b, g, k):
                hb = self.hbuf[k % self.nh]
                if src == "x":
                    ap = x_in[b * S + g * TG: b * S + (g + 1) * TG, :].rearrange("(j p) d -> p j d", p=128)
                    rd = [xres]
                else:
                    ap = H_ap(src, b, g)
                    rd = [hres[(src, b, g)]]
                dma_in(hb, ap, reads=rd)
                return hb

            def stats(self, k):
                hb, st, sqj = self.hbuf[k % self.nh], self.stat[k % self.nh], self.sqj
                for j in range(4):
                    P.op("scalar", lambda e, j=j: e.activation(out=sqj[:], in_=hb[:, j, :], func=AF.Square, accum_out=st[:, j:j + 1]),
                         reads=[hb.r], writes=[sqj.r, st.r])
                P.op("scalar", lambda e: e.activation(out=st[:, 4:8], in_=st[:, 0:4], func=AF.Sqrt, bias=EPS, scale=1.0 / D),
                     reads=[st.r], writes=[st.r])
                P.op("vector", lambda e: e.reciprocal(out=st[:, 4:8], in_=st[:, 4:8]), reads=[st.r], writes=[st.r])
                return st

            def norm_a(self, k):
                hb, xb, gv = self.hbuf[k % self.nh], self.xnb, self.gv
                st = self.stats(k)
                for j in range(4):
                    P.op("vector", lambda e, j=j: e.scalar_tensor_tensor(out=xb[:, j, :], in0=hb[:, j, :], scalar=st[:, 4 + j:5 + j],
                                                                        in1=gv[:], op0=ALU.mult, op1=ALU.mult),
                         reads=[hb.r, st.r, gv.r], writes=[xb.r])

            def norm_b(self, k):
                xb, xt = self.xnb, self.xnT[k % 2]
                for j in range(4):
                    for c in range(8):
                        P.op("tensor", lambda e, j=j, c=c: e.transpose(out=pbf[:, c * 128:(c + 1) * 128], in_=xb[:, j, c * 128:(c + 1) * 128],
                                                                      identity=ident_b[:]),
                             reads=[xb.r, ident_b.r], writes=[pbf.r])
                    P.op("vector", lambda e, j=j: e.tensor_copy(out=xt[:, :, j * 128:(j + 1) * 128],
                                                               in_=pbf[:, :].rearrange("p (c t) -> p c t", c=8)),
                         reads=[pbf.r], writes=[xt.r])
                return xb, xt

            def norm(self, k):
                self.norm_a(k)
                return self.norm_b(k)

            def store_o(self, dst, b, g, extra_reads=()):
                ob = self.obuf
                if dst == "out":
                    ap = out_d[b * S + g * TG: b * S + (g + 1) * TG, :].rearrange("(j p) d -> p j d", p=128)
                    wr = [outres]
                else:
                    ap = H_ap(dst, b, g)
                    wr = [hres[(dst, b, g)]]
                P.op("sync", lambda e, s: e.dma_start(out=ap, in_=ob[:]).then_inc(s, 16), reads=[ob.r], writes=wr, dma=ob.r)

        def load_w_bf16(tile, ap, parts=1):
            kc = tile.t.shape[1]
            step = max(1, kc // parts)
            rng = list(range(0, kc, step))

            def fn(e, s):
                for c0 in rng:
                    e.dma_start(out=tile[:, c0:c0 + step, :], in_=ap[c0 * 128:(c0 + step) * 128, :].rearrange("(c p) f -> p c f", p=128)).then_inc(s, 16)
            P.op("gpsimd", fn, writes=[tile.r], dma=tile.r, ndma=len(rng))

        def rg_layer(li, j, src, dst):
            P.begin_phase()
            N = NormCtx(li, want_o=False)
            wA = sb("wA", [128, 8, 2048], BF16)
            load_w_bf16(wA, rg_w_in[j], parts=4)
            bin_ = sb("rgbin", [128, 16])
            dma_in(bin_, rg_b_in[j])
            fm_f = [sb("rgfmf%d" % i, [128, TG]) for i in range(2)]
            fm_t = [sb("rgfmt%d" % i, [128, TG]) for i in range(2)]
            fm_g = [sb("rgfmg%d" % i, [128, TG], BF16) for i in range(2)]
            for b in range(NB):
                def front_a(g):
                    N.load_h(src, b, g, g)
                    N.norm_a(g)
                front_a(0)
                nxt = N.norm_b(0)
                for g in range(NG):
                    xb, xt = nxt
                    if g + 1 < NG:
                        front_a(g + 1)
                    for fc in range(16):
                        pt = ps[fc % 4]
                        for kc in range(8):
                            mm(pt[:, :], wA[:, kc, fc * 128:(fc + 1) * 128], xt[:, kc, :], kc == 0, kc == 7, [wA.r, xt.r], [pt.r])
                        q = fc % 2
                        xg = fm_f[q]
                        P.op("scalar", lambda e, pt=pt, xg=xg, fc=fc: e.activation(out=xg[:], in_=pt[:, :], func=AF.Identity, bias=bin_[:, fc:fc + 1], scale=1.0),
                             reads=[pt.r, bin_.r], writes=[xg.r])
                        if fc < 8:
                            tt, gg = fm_t[q], fm_g[q]
                            P.op("vector", lambda e, xg=xg, tt=tt: e.tensor_tensor(out=tt[:], in0=xg[:], in1=xg[:], op=ALU.mult), reads=[xg.r], writes=[tt.r])
                            P.op("vector", lambda e, tt=tt: e.tensor_scalar(out=tt[:], in0=tt[:], scalar1=0.044715, scalar2=1.0, op0=ALU.mult, op1=ALU.add),
                                 reads=[tt.r], writes=[tt.r])
                            P.op("vector", lambda e, xg=xg, tt=tt: e.tensor_tensor(out=tt[:], in0=tt[:], in1=xg[:], op=ALU.mult), reads=[xg.r, tt.r], writes=[tt.r])
                            P.op("scalar", lambda e, tt=tt: e.activation(out=tt[:], in_=tt[:], func=AF.Sigmoid, scale=1.5957691216057308),
                                 reads=[tt.r], writes=[tt.r])
                            P.op("vector", lambda e, xg=xg, tt=tt, gg=gg: e.tensor_tensor(out=gg[:], in0=tt[:], in1=xg[:], op=ALU.mult),
                                 reads=[xg.r, tt.r], writes=[gg.r])
                            P.op("sync", lambda e, s, gg=gg, fc=fc, g=g, b=b: e.dma_start(out=GT[b, fc * 128:(fc + 1) * 128, g * TG:(g + 1) * TG], in_=gg[:]).then_inc(s, 16),
                                 reads=[gg.r], writes=[gtres[b][fc]], dma=gg.r)
                        else:
                            c = fc - 8
                            P.op("sync", lambda e, s, xg=xg, c=c, g=g, b=b: e.dma_start(out=UT[b, c * 128:(c + 1) * 128, g * TG:(g + 1) * TG], in_=xg[:]).then_inc(s, 16),
                                 reads=[xg.r], writes=[utres[b][c]], dma=xg.r)
                    if g + 1 < NG:
                        nxt = N.norm_b(g + 1)
            P.end_phase()

            P.begin_phase()
            gw = sb("rggw", [128, 2, 2, 4, 2, 256], BF16)

            def fn(e, s):
                for d_ in range(2):
                    for kd in range(2):
                        for h in range(4):
                            e.dma_start(out=gw[:, d_, kd, h, :, :],
                                        in_=rg_gw[j, d_, kd, h].rearrange("(c p) o -> p c o", p=128)).then_inc(s, 16)
            P.op("gpsimd", fn, writes=[gw.r], dma=gw.r, ndma=16)
            cw = sb("rgcw", [128, 5, 8])
            dma_in(cw, rg_conv[j])
            gb = sb("rggb", [128, 2, 2, 8])
            dma_in(gb, rg_gb[j])
            lam = sb("rglam", [128, 2, 8])
            dma_in(lam, rg_lam[j])
            c8 = sb("rgc8", [128, 2, 8])
            P.op("scalar", lambda e: e.activation(out=c8[:], in_=lam[:], func=AF.Exp, scale=-1.0), reads=[lam.r], writes=[c8.r])
            P.op("scalar", lambda e: e.activation(out=c8[:], in_=c8[:], func=AF.Ln, bias=1.0, scale=1.0), reads=[c8.r], writes=[c8.r])
            P.op("vector", lambda e: e.tensor_scalar(out=c8[:], in0=c8[:], scalar1=-8.0, scalar2=None, op0=ALU.mult), reads=[c8.r], writes=[c8.r])
            ue = sb("rgue", [128, 2, S + 4])
            uc = sb("rguc", [128, 2, S])
            ucb = sb("rgucb", [128, 2, S], BF16)
            Aa = sb("rgA", [128, S])
            Bb = sb("rgB", [128, S])
            hs = [sb("rghs%d" % i, [128, S]) for i in range(2)]
            gtl = sb("rggt", [128, S], BF16)
            yt = sb("rgyt", [128, S], BF16)
            ti = [sb("rgti%d" % i, [128, TG]) for i in range(2)]
            tr = [sb("rgtr%d" % i, [128, TG]) for i in range(2)]
            P.op("vector", lambda e: e.memset(ue[:, :, 0:2], 0.0), writes=[ue.r])
            P.op("vector", lambda e: e.memset(ue[:, :, S + 2:S + 4], 0.0), writes=[ue.r])
            for b in range(NB):
                for h in range(4):
                    for cc in range(2):
                        c = 2 * h + cc
                        P.op("sync", lambda e, s, c=c, cc=cc, b=b: e.dma_start(out=ue[:, cc, 2:S + 2], in_=UT[b, c * 128:(c + 1) * 128, :]).then_inc(s, 16),
                             reads=[utres[b][c]], writes=[ue.r], dma=ue.r)
                    for cc in range(2):
                        c = 2 * h + cc
                        P.op("vector", lambda e, c=c, cc=cc: e.tensor_scalar(out=uc[:, cc, :], in0=ue[:, cc, 0:S], scalar1=cw[:, 0, c:c + 1], scalar2=cw[:, 4, c:c + 1],
                                                                            op0=ALU.mult, op1=ALU.add), reads=[ue.r, cw.r], writes=[uc.r])
                        for kk in range(1, 4):
                            P.op("vector", lambda e, c=c, cc=cc, kk=kk: e.scalar_tensor_tensor(out=uc[:, cc, :], in0=ue[:, cc, kk:S + kk], scalar=cw[:, kk, c:c + 1],
                                                                                              in1=uc[:, cc, :], op0=ALU.mult, op1=ALU.add),
                                 reads=[ue.r, cw.r, uc.r], writes=[uc.r])
                        P.op("scalar", lambda e, cc=cc: e.copy(out=ucb[:, cc, :], in_=uc[:, cc, :]), reads=[uc.r], writes=[ucb.r])
                    for cc in range(2):
                        c = 2 * h + cc
                        P.op("sync", lambda e, s, c=c, b=b: e.dma_start(out=gtl[:], in_=GT[b, c * 128:(c + 1) * 128, :]).then_inc(s, 16),
                             reads=[gtres[b][c]], writes=[gtl.r], dma=gtl.r)
                        for d_ in range(2):
                            hd = hs[d_]
                            for tb in range(S // TG):
                                q = tb % 2
                                sl = slice(tb * TG, (tb + 1) * TG)
                                pi, pr = ps[4 + 0], ps[4 + 1]
                                for kc in range(2):
                                    mm(pi[:, :], gw[:, d_, 0, h, kc, cc * 128:(cc + 1) * 128], ucb[:, kc, sl], kc == 0, kc == 1, [gw.r, ucb.r], [pi.r])
                                for kc in range(2):
                                    mm(pr[:, :], gw[:, d_, 1, h, kc, cc * 128:(cc + 1) * 128], ucb[:, kc, sl], kc == 0, kc == 1, [gw.r, ucb.r], [pr.r])
                                tti, ttr = ti[q], tr[q]
                                P.op("scalar", lambda e, pi=pi, tti=tti, d_=d_, c=c: e.activation(out=tti[:], in_=pi[:, :], func=AF.Sigmoid, bias=gb[:, d_, 0, c:c + 1], scale=1.0),
                                     reads=[pi.r, gb.r], writes=[tti.r])
                                P.op("scalar", lambda e, pr=pr, ttr=ttr, d_=d_, c=c: e.activation(out=ttr[:], in_=pr[:, :], func=AF.Sigmoid, bias=gb[:, d_, 1, c:c + 1], scale=1.0),
                                     reads=[pr.r, gb.r], writes=[ttr.r])
                                P.op("scalar", lambda e, ttr=ttr, d_=d_, c=c, sl=sl: e.activation(out=Aa[:, sl], in_=ttr[:], func=AF.Exp, scale=c8[:, d_, c:c + 1]),
                                     reads=[ttr.r, c8.r], writes=[Aa.r])
                                P.op("vector", lambda e, ttr=ttr, sl=sl: e.tensor_tensor(out=ttr[:], in0=Aa[:, sl], in1=Aa[:, sl], op=ALU.mult), reads=[Aa.r], writes=[ttr.r])
                                P.op("scalar", lambda e, ttr=ttr: e.activation(out=ttr[:], in_=ttr[:], func=AF.Sqrt, bias=1.0, scale=-1.0), reads=[ttr.r], writes=[ttr.r])
                                P.op("vector", lambda e, tti=tti, ttr=ttr: e.tensor_tensor(out=tti[:], in0=tti[:], in1=ttr[:], op=ALU.mult), reads=[tti.r, ttr.r], writes=[tti.r])
                                P.op("vector", lambda e, tti=tti, cc=cc, sl=sl: e.tensor_tensor(out=Bb[:, sl], in0=tti[:], in1=uc[:, cc, sl], op=ALU.mult),
                                     reads=[tti.r, uc.r], writes=[Bb.r])
                            if d_ == 0:
                                P.op("vector", lambda e, hd=hd: e.tensor_tensor_scan(out=hd[:], data0=Aa[:], data1=Bb[:], initial=0.0, op0=ALU.mult, op1=ALU.add),
                                     reads=[Aa.r, Bb.r], writes=[hd.r])
                            else:
                                P.op("vector", lambda e, hd=hd: e.tensor_tensor_scan(out=hd[:, ::-1], data0=Aa[:, ::-1], data1=Bb[:, ::-1], initial=0.0,
                                                                                    op0=ALU.mult, op1=ALU.add),
                                     reads=[Aa.r, Bb.r], writes=[hd.r])
                        P.op("vector", lambda e: e.tensor_tensor(out=hs[0][:], in0=hs[0][:], in1=hs[1][:], op=ALU.add), reads=[hs[0].r, hs[1].r], writes=[hs[0].r])
                        P.op("vector", lambda e: e.tensor_tensor(out=yt[:], in0=hs[0][:], in1=gtl[:], op=ALU.mult), reads=[hs[0].r, gtl.r], writes=[yt.r])
                        P.op("sync", lambda e, s, c=c, b=b: e.dma_start(out=YT[b, c * 128:(c + 1) * 128, :], in_=yt[:]).then_inc(s, 16),
                             reads=[yt.r], writes=[ytres[b][c]], dma=yt.r)
            P.end_phase()

            P.begin_phase()
            N = NormCtx(None)
            wout = sb("rgwout", [128, 8, D], BF16)
            load_w_bf16(wout, rg_w_out[j], parts=2)
            bout = sb("rgbout", [128, D])
            dma_in(bout, rg_b_out[j])
            ylt = [sb("rgyl%d" % i, [128, 8, 128], BF16) for i in range(2)]
            n = 0
            for b in range(NB):
                for g in range(NG):
                    hb = N.load_h(src, b, g, g)
                    ob = N.obuf
                    for jt in range(4):
                        yl = ylt[n % 2]
                        n += 1
                        t0 = g * TG + jt * 128
                        P.op("sync", lambda e, s, yl=yl, t0=t0, b=b: e.dma_start(out=yl[:], in_=YT[b, :, t0:t0 + 128].rearrange("(c p) t -> p c t", p=128)).then_inc(s, 16),
                             reads=ytres[b], writes=[yl.r], dma=yl.r)
                        for nb in range(2):
                            pt = ps[nb]
                            for kc in range(8):
                                mm(pt[:, :], yl[:, kc, :], wout[:, kc, nb * 512:(nb + 1) * 512], kc == 0, kc == 7, [yl.r, wout.r], [pt.r])
                            P.op("vector", lambda e, pt=pt, jt=jt, nb=nb: e.tensor_tensor(out=ob[:, jt, nb * 512:(nb + 1) * 512], in0=pt[:, :],
                                                                                      in1=bout[:, nb * 512:(nb + 1) * 512], op=ALU.add),
                                 reads=[pt.r, bout.r], writes=[ob.r])
                        P.op("vector", lambda e, jt=jt, hb=hb: e.tensor_tensor(out=ob[:, jt, :], in0=ob[:, jt, :], in1=hb[:, jt, :], op=ALU.add),
                             reads=[ob.r, hb.r], writes=[ob.r])
                    N.store_o(dst, b, g)
            P.end_phase()

        def ft_layer(li, j, src, dst):
            P.begin_phase()
            N = NormCtx(li, want_o=False)
            win = sb("ftwin", [128, 8, D], BF16)
            load_w_bf16(win, ft_w_in[j], parts=2)
            ub = [sb("ftub%d" % i, [128, 4, D], BF16) for i in range(2)]
            for b in range(NB):
                def front_a(g):
                    N.load_h(src, b, g, g)
                    N.norm_a(g)
                front_a(0)
                nxt = N.norm_b(0)
                for g in range(NG):
                    xb, xt = nxt
                    if g + 1 < NG:
                        front_a(g + 1)
                    u = ub[g % 2]
                    for jt in range(4):
                        for nb in range(2):
                            pt = ps[nb]
                            for kc in range(8):
                                mm(pt[:, :], xt[:, kc, jt * 128:(jt + 1) * 128], win[:, kc, nb * 512:(nb + 1) * 512], kc == 0, kc == 7, [xt.r, win.r], [pt.r])
                            P.op("scalar", lambda e, pt=pt, u=u, jt=jt, nb=nb: e.copy(out=u[:, jt, nb * 512:(nb + 1) * 512], in_=pt[:, :]),
                                 reads=[pt.r], writes=[u.r])
                    P.op("sync", lambda e, s, u=u, b=b, g=g: e.dma_start(out=UD[b, g * TG:(g + 1) * TG, :].rearrange("(j p) d -> p j d", p=128), in_=u[:]).then_inc(s, 16),
                         reads=[u.r], writes=[hres[("UD", b, g)]], dma=u.r)
                    if g + 1 < NG:
                        nxt = N.norm_b(g + 1)
            P.end_phase()

            P.begin_phase()
            N = NormCtx(None)
            wout = sb("ftwout", [128, 8, D], BF16)
            load_w_bf16(wout, ft_w_out[j], parts=2)
            cdc = sb("ftcd", [128, 2, 2, 256], BF16)

            def fn(e, s):
                for q in range(2):
                    e.dma_start(out=cdc[:, q, :, :], in_=ccd[q].rearrange("(c p) o -> p c o", p=128)).then_inc(s, 16)
            P.op("gpsimd", fn, writes=[cdc.r], dma=cdc.r, ndma=2)
            U = sb("ftU", [128, 32, D], BF16)
            tab = [sb("fttab%d" % i, [128, 16, TG], BF16) for i in range(2)]
            YZ = sb("ftYZ", [128, 2, 8, TG], BF16)
            fT = sb("ftfT", [128, 8, TG], BF16)
            tabn = 0
            for b in range(NB):
                P.op("sync", lambda e, s, b=b: e.dma_start(out=U[:], in_=UD[b].rearrange("(c p) d -> p c d", p=128)).then_inc(s, 16),
                     reads=[hres[("UD", b, g)] for g in range(NG)], writes=[U.r], dma=U.r)
                for pb in range(NG):
                    for q in range(2):
                        for half in range(2):
                            for th in range(2):
                                tb = tab[tabn % 2]
                                tabn += 1

                                def fn(e, s, tb=tb, q=q, th=th, pb=pb):
                                    for c4 in range(4):
                                        r0 = (th * 16 + c4 * 4) * 128
                                        e.dma_start(out=tb[:, c4 * 4:(c4 + 1) * 4, :],
                                                    in_=cdft[q, r0:r0 + 512, pb * TG:(pb + 1) * TG].rearrange("(c p) n -> p c n", p=128)).then_inc(s, 16)
                                P.op("gpsimd", fn, writes=[tb.r], dma=tb.r, ndma=4)
                                for tc_ in range(16):
                                    tcg = th * 16 + tc_
                                    for c4 in range(4):
                                        ch = half * 4 + c4
                                        pt = ps[c4]
                                        mm(pt[:, :], U[:, tcg, ch * 128:(ch + 1) * 128], tb[:, tc_, :], tcg == 0, tcg == 31, [U.r, tb.r], [pt.r])
                            for c4 in range(4):
                                ch = half * 4 + c4
                                pt = ps[c4]
                                if c4 % 2:
                                    P.op("vector", lambda e, pt=pt, q=q, ch=ch: e.tensor_copy(out=YZ[:, q, ch, :], in_=pt[:, :]), reads=[pt.r], writes=[YZ.r])
                                else:
                                    P.op("scalar", lambda e, pt=pt, q=q, ch=ch: e.copy(out=YZ[:, q, ch, :], in_=pt[:, :]), reads=[pt.r], writes=[YZ.r])
                    for ch in range(8):
                        grp = ch // 2
                        pt = ps[4 + ch % 2]
                        n = 0
                        for q in range(2):
                            for kc in range(2):
                                mm(pt[:, :], cdc[:, q, kc, (ch % 2) * 128:(ch % 2 + 1) * 128], YZ[:, q, grp * 2 + kc, :], n == 0, n == 3, [cdc.r, YZ.r], [pt.r])
                                n += 1
                        P.op("vector", lambda e, pt=pt, ch=ch: e.tensor_scalar(out=fT[:, ch, :], in0=pt[:, :], scalar1=1.0 / 1024.0, scalar2=None, op0=ALU.mult),
                             reads=[pt.r], writes=[fT.r])
                    hb = N.load_h(src, b, pb, pb)
                    ob = N.obuf
                    for jt in range(4):
                        for nb in range(2):
                            pt = ps[nb]
                            for kc in range(8):
                                mm(pt[:, :], fT[:, kc, jt * 128:(jt + 1) * 128], wout[:, kc, nb * 512:(nb + 1) * 512], kc == 0, kc == 7, [fT.r, wout.r], [pt.r])
                            P.op("vector", lambda e, pt=pt, jt=jt, nb=nb, hb=hb: e.tensor_tensor(out=ob[:, jt, nb * 512:(nb + 1) * 512], in0=pt[:, :],
                                                                                             in1=hb[:, jt, nb * 512:(nb + 1) * 512], op=ALU.add),
                                 reads=[pt.r, hb.r], writes=[ob.r])
                    N.store_o(dst, b, pb)
            P.end_phase()

        def moe_layer(li, src, dst):
            P.begin_phase()
            N = NormCtx(4 + li, want_o=False)
            wr = sb("wr", [128, 8, NE])
            dma_in(wr, w_router[li].rearrange("(c p) n -> p c n", p=128))
            xnf = sb("xnf", [128, 4, D])
            xnTf = sb("xnTf", [128, 8, TG])
            sm = sb("smx", [128, 4, NE])
            ssum = sb("ssum", [128, 12])
            xbs = sb("xbs", [128, 4, D], BF16)
            for b in range(NB):
                for g in range(NG):
                    hb = N.load_h(src, b, g, g)
                    P.op("sync", lambda e, s, hb=hb, b=b, g=g: e.dma_start(out=H_ap(dst, b, g), in_=hb[:]).then_inc(s, 16),
                         reads=[hb.r], writes=[hres[(dst, b, g)]], dma=hb.r)
                    st = N.stats(g)
                    for jq in range(4):
                        P.op("vector", lambda e, jq=jq, hb=hb, st=st: e.scalar_tensor_tensor(out=xnf[:, jq, :], in0=hb[:, jq, :], scalar=st[:, 4 + jq:5 + jq],
                                                                                            in1=N.gv[:], op0=ALU.mult, op1=ALU.mult),
                             reads=[hb.r, st.r, N.gv.r], writes=[xnf.r])
                    P.op("scalar", lambda e: e.copy(out=xbs[:], in_=xnf[:]), reads=[xnf.r], writes=[xbs.r])
                    for jq in range(4):
                        for c0 in range(2):
                            ptf = ps[4 + c0]
                            for c in range(4):
                                P.op("tensor", lambda e, jq=jq, c0=c0, c=c, ptf=ptf: e.transpose(out=ptf[:, c * 128:(c + 1) * 128], in_=xnf[:, jq, (c0 * 4 + c) * 128:(c0 * 4 + c + 1) * 128],
                                                                                             identity=ident_f[:]),
                                     reads=[xnf.r, ident_f.r], writes=[ptf.r])
                            P.op("vector" if c0 else "scalar",
                                 (lambda e, jq=jq, c0=c0, ptf=ptf: e.tensor_copy(out=xnTf[:, c0 * 4:(c0 + 1) * 4, jq * 128:(jq + 1) * 128], in_=ptf[:, :].rearrange("p (c t) -> p c t", c=4))) if c0 else
                                 (lambda e, jq=jq, c0=c0, ptf=ptf: e.copy(out=xnTf[:, c0 * 4:(c0 + 1) * 4, jq * 128:(jq + 1) * 128], in_=ptf[:, :].rearrange("p (c t) -> p c t", c=4))),
                                 reads=[ptf.r], writes=[xnTf.r])
                    xt = xnTf
                    P.op("sync", lambda e, s, b=b, g=g: e.dma_start(out=XN2[b, g * TG:(g + 1) * TG, :].rearrange("(j p) d -> p j d", p=128), in_=xbs[:]).then_inc(s, 16),
                         reads=[xbs.r], writes=[hres[("XN2", b, g)]], dma=xbs.r)
                    pt = ps[6]
                    for jt in range(4):
                        for kc in range(8):
                            mm(pt[:, jt * NE:(jt + 1) * NE], xt[:, kc, jt * 128:(jt + 1) * 128], wr[:, kc, :], kc == 0, kc == 7, [xt.r, wr.r], [pt.r])
                    P.op("vector", lambda e, pt=pt: e.tensor_reduce(out=ssum[:, 0:4], in_=pt[:, 0:4 * NE].rearrange("p (j n) -> p j n", n=NE), axis=AX.X, op=ALU.max),
                         reads=[pt.r], writes=[ssum.r])
                    P.op("vector", lambda e: e.tensor_scalar(out=ssum[:, 0:4], in0=ssum[:, 0:4], scalar1=-1.0, scalar2=None, op0=ALU.mult), reads=[ssum.r], writes=[ssum.r])
                    for jt in range(4):
                        P.op("scalar", lambda e, pt=pt, jt=jt: e.activation(out=sm[:, jt, :], in_=pt[:, jt * NE:(jt + 1) * NE], func=AF.Exp, bias=ssum[:, jt:jt + 1], scale=1.0,
                                                                           accum_out=ssum[:, 4 + jt:5 + jt]),
                             reads=[pt.r, ssum.r], writes=[sm.r, ssum.r])
                    P.op("vector", lambda e: e.reciprocal(out=ssum[:, 8:12], in_=ssum[:, 4:8]), reads=[ssum.r], writes=[ssum.r])
                    for jt in range(4):
                        P.op("vector", lambda e, b=b, g=g, jt=jt: e.tensor_scalar(out=afftm[:, b, g * 4 + jt, :], in0=sm[:, jt, :], scalar1=ssum[:, 8 + jt:9 + jt], scalar2=None, op0=ALU.mult),
                             reads=[sm.r, ssum.r], writes=[afftm.r])
            P.end_phase()

            P.begin_phase()
            affT = sb("affT", [NE, S])
            msk = sb("msk", [NE, S])
            rnk = sb("rnk", [NE, S])
            onesr = sb("onesr", [NE, S])
            P.op("vector", lambda e: e.memset(onesr[:], 1.0), writes=[onesr.r])
            bs = sb("bs", [NE, 8])
            mrtm = sb("mrtm", [128, 2, 32, NE])
            sel = [sb("sel%d" % i, [128, CAP], BF16) for i in range(2)]
            rhs6 = sb("rhs6", [128, 32, NE, 6], BF16)
            tk2 = sb("tk2", [128, 32, NE, 2])
            dma_in(tk2, ctok2)
            rr = sb("rr", [128, 32, NE])
            s6 = [sb("s6_%d" % i, [6, CAP]) for i in range(2)]
            t6 = [sb("t6_%d" % i, [128, 4, 6]) for i in range(2)]
            for b in range(NB):
                for c in range(32):
                    pt = ps[4 + c % 2]
                    P.op("tensor", lambda e, pt=pt, b=b, c=c: e.transpose(out=pt[0:NE, 0:128], in_=afftm[:, b, c, :], identity=ident_f[:]),
                         reads=[afftm.r, ident_f.r], writes=[pt.r])
                    P.op("scalar", lambda e, pt=pt, c=c: e.copy(out=affT[:, c * 128:(c + 1) * 128], in_=pt[0:NE, 0:128]), reads=[pt.r], writes=[affT.r])
                P.op("vector", lambda e: e.memset(bs[:, 0:1], 0.0), writes=[bs.r])
                P.op("vector", lambda e: e.memset(bs[:, 1:2], 1.0), writes=[bs.r])
                for it in range(30):
                    P.op("vector", lambda e: e.tensor_tensor(out=bs[:, 2:3], in0=bs[:, 0:1], in1=bs[:, 1:2], op=ALU.add), reads=[bs.r], writes=[bs.r])
                    P.op("vector", lambda e: e.tensor_scalar(out=bs[:, 2:3], in0=bs[:, 2:3], scalar1=0.5, scalar2=None, op0=ALU.mult), reads=[bs.r], writes=[bs.r])
                    P.op("vector", lambda e: e.tensor_scalar(out=msk[:], in0=affT[:], scalar1=bs[:, 2:3], scalar2=0.0, op0=ALU.is_ge, op1=ALU.add, accum_out=bs[:, 3:4]),
                         reads=[affT.r, bs.r], writes=[msk.r, bs.r])
                    P.op("vector", lambda e: e.tensor_scalar(out=bs[:, 4:5], in0=bs[:, 3:4], scalar1=float(CAP), scalar2=None, op0=ALU.is_ge), reads=[bs.r], writes=[bs.r])
                    P.op("vector", lambda e: e.tensor_tensor(out=bs[:, 5:6], in0=bs[:, 2:3], in1=bs[:, 0:1], op=ALU.subtract), reads=[bs.r], writes=[bs.r])
                    P.op("vector", lambda e: e.tensor_tensor(out=bs[:, 6:7], in0=bs[:, 1:2], in1=bs[:, 2:3], op=ALU.subtract), reads=[bs.r], writes=[bs.r])
                    P.op("vector", lambda e: e.scalar_tensor_tensor(out=bs[:, 0:1], in0=bs[:, 5:6], scalar=bs[:, 4:5], in1=bs[:, 0:1], op0=ALU.mult, op1=ALU.add),
                         reads=[bs.r], writes=[bs.r])
                    P.op("vector", lambda e: e.scalar_tensor_tensor(out=bs[:, 1:2], in0=bs[:, 6:7], scalar=bs[:, 4:5], in1=bs[:, 2:3], op0=ALU.mult, op1=ALU.add),
                         reads=[bs.r], writes=[bs.r])
                P.op("vector", lambda e: e.tensor_scalar(out=msk[:], in0=affT[:], scalar1=bs[:, 0:1], scalar2=None, op0=ALU.is_ge), reads=[affT.r, bs.r], writes=[msk.r])
                P.op("vector", lambda e: e.tensor_tensor_scan(out=rnk[:], data0=onesr[:], data1=msk[:], initial=0.0, op0=ALU.mult, op1=ALU.add),
                     reads=[onesr.r, msk.r], writes=[rnk.r])
                for wi, src_t in enumerate((msk, rnk)):
                    pt = ps[4 + wi]
                    for c in range(32):
                        P.op("tensor", lambda e, pt=pt, c=c, src_t=src_t: e.transpose(out=pt[:, c * NE:(c + 1) * NE], in_=src_t[:, c * 128:(c + 1) * 128], identity=ident_f[0:NE, 0:NE]),
                             reads=[src_t.r, ident_f.r], writes=[pt.r])
                    P.op("vector", lambda e, pt=pt, wi=wi: e.tensor_copy(out=mrtm[:, wi, :, :], in_=pt[:, :].rearrange("p (c n) -> p c n", n=NE)),
                         reads=[pt.r], writes=[mrtm.r])
                P.op("vector", lambda e: e.tensor_copy(out=rhs6[:, :, :, 0:2], in_=tk2[:]), reads=[tk2.r], writes=[rhs6.r])
                P.op("vector", lambda e: e.memset(rhs6[:, :, :, 5], 1.0), writes=[rhs6.r])
                P.op("vector", lambda e, b=b: e.tensor_copy(out=rhs6[:, :, :, 2], in_=afftm[:, b, :, :]), reads=[afftm.r], writes=[rhs6.r])
                P.op("vector", lambda e, b=b: e.tensor_tensor(out=rr[:], in0=afftm[:, b, :, :], in1=rhs6[:, :, :, 2], op=ALU.subtract), reads=[afftm.r, rhs6.r], writes=[rr.r])
                P.op("vector", lambda e: e.tensor_copy(out=rhs6[:, :, :, 3], in_=rr[:]), reads=[rr.r], writes=[rhs6.r])
                P.op("vector", lambda e: e.tensor_tensor(out=rr[:], in0=rr[:], in1=rhs6[:, :, :, 3], op=ALU.subtract), reads=[rr.r, rhs6.r], writes=[rr.r])
                P.op("vector", lambda e: e.tensor_copy(out=rhs6[:, :, :, 4], in_=rr[:]), reads=[rr.r], writes=[rhs6.r])
                for ex in range(NE):
                    pt = ps[ex % 2]
                    for c in range(32):
                        sl_ = sel[c % 2]
                        P.op("vector", lambda e, sl_=sl_, c=c, ex=ex: e.tensor_scalar(out=sl_[:], in0=iota[:], scalar1=mrtm[:, 1, c, ex:ex + 1],
                                                                                    scalar2=mrtm[:, 0, c, ex:ex + 1], op0=ALU.is_equal, op1=ALU.mult),
                             reads=[iota.r, mrtm.r], writes=[sl_.r])
                        mm(pt[0:6, :], rhs6[:, c, ex, :], sl_[:], c == 0, c == 31, [sl_.r, rhs6.r], [pt.r])
                    s6k, t6k = s6[ex % 2], t6[ex % 2]
                    P.op("scalar", lambda e, pt=pt, s6k=s6k: e.copy(out=s6k[:], in_=pt[0:6, :]), reads=[pt.r], writes=[s6k.r])
                    ptT = ps[2 + ex % 2]
                    for sbk in range(4):
                        P.op("tensor", lambda e, ptT=ptT, s6k=s6k, sbk=sbk: e.transpose(out=ptT[:, sbk * 6:(sbk + 1) * 6], in_=s6k[:, sbk * 128:(sbk + 1) * 128], identity=ident_f[0:6, 0:6]),
                             reads=[s6k.r, ident_f.r], writes=[ptT.r])
                    P.op("vector", lambda e, ptT=ptT, t6k=t6k: e.tensor_copy(out=t6k[:], in_=ptT[:, 0:24].rearrange("p (s k) -> p s k", k=6)), reads=[ptT.r], writes=[t6k.r])
                    P.op("vector", lambda e, t6k=t6k, b=b, ex=ex: e.scalar_tensor_tensor(out=sinfo[:, b, ex, :, 0], in0=t6k[:, :, 0], scalar=64.0, in1=t6k[:, :, 1], op0=ALU.mult, op1=ALU.add),
                         reads=[t6k.r], writes=[sinfo.r])
                    P.op("vector", lambda e, t6k=t6k, b=b, ex=ex: e.tensor_tensor(out=sinfo[:, b, ex, :, 1], in0=t6k[:, :, 2], in1=t6k[:, :, 3], op=ALU.add), reads=[t6k.r], writes=[sinfo.r])
                    P.op("vector", lambda e, t6k=t6k, b=b, ex=ex: e.tensor_tensor(out=sinfo[:, b, ex, :, 1], in0=sinfo[:, b, ex, :, 1], in1=t6k[:, :, 4], op=ALU.add), reads=[t6k.r, sinfo.r], writes=[sinfo.r])
                    P.op("vector", lambda e, t6k=t6k, b=b, ex=ex: e.tensor_copy(out=sinfo[:, b, ex, :, 2], in_=t6k[:, :, 5]), reads=[t6k.r], writes=[sinfo.r])
                P.op("vector", lambda e, b=b: e.tensor_scalar(out=sinfo[:, b, :, :, 0], in0=sinfo[:, b, :, :, 0], scalar1=float(b * S), scalar2=None, op0=ALU.add),
                     reads=[sinfo.r], writes=[sinfo.r])
                P.op("vector", lambda e, b=b: e.tensor_copy(out=sidx_g[:, b, :, :], in_=sinfo[:, b, :, :, 0]), reads=[sinfo.r], writes=[sidx_g.r])
                P.op("vector", lambda e, b=b: e.tensor_scalar(out=sinfo[:, b, :, :, 2], in0=sinfo[:, b, :, :, 2], scalar1=-float(S), scalar2=float(S + b * 128), op0=ALU.mult, op1=ALU.add),
                     reads=[sinfo.r], writes=[sinfo.r])
                P.op("vector", lambda e, b=b: e.tensor_tensor(out=sinfo[:, b, :, :, 2], in0=sinfo[:, b, :, :, 2], in1=sinfo[:, b, :, :, 0], op=ALU.add),
                     reads=[sinfo.r], writes=[sinfo.r])
                P.op("vector", lambda e, b=b: e.tensor_copy(out=sidx_s[:, b, :, :], in_=sinfo[:, b, :, :, 2]), reads=[sinfo.r], writes=[sidx_s.r])
            P.end_phase()

            P.begin_phase()
            wA = [sb("wA%d" % h, [128, 8, FF // 2], BF16) for h in range(2)]
            wB = [sb("wB%d" % h, [128, 8, FF // 2], BF16) for h in range(2)]
            wC = [sb("wC%d" % h, [128, 8, D], BF16) for h in range(2)]
            xg = [sb("xg%d" % i, [128, 4, D], BF16) for i in range(2)]
            xgT = sb("xgT", [128, 8, CAP], BF16)
            hg = sb("hg", [128, 16, CAP], BF16)
            sg = [sb("sg%d" % i, [128, CAP]) for i in range(2)]
            yb = [sb("yb%d" % i, [128, D]) for i in range(4)]
            allh_dst = {b: [hres[(dst, b, g)] for g in range(NG)] for b in range(NB)}
            allx = {b: [hres[("XN2", b, g)] for g in range(NG)] for b in range(NB)}
            hdst = HD[dst]
            b = 0

            def load_half(tile, ap):
                def fn(e, s):
                    for c0 in (0, 4):
                        e.dma_start(out=tile[:, c0:c0 + 4, :], in_=ap[c0 * 128:(c0 + 4) * 128, :].rearrange("(c p) f -> p c f", p=128)).then_inc(s, 16)
                P.op("gpsimd", fn, writes=[tile.r], dma=tile.r, ndma=2)

            def load_gu(ex, h):
                load_half(wA[h], w_gate[lidx[li], ex][:, h * 1024:(h + 1) * 1024])
                load_half(wB[h], w_up[lidx[li], ex][:, h * 1024:(h + 1) * 1024])

            def load_dn(ex):
                for h in range(2):
                    load_half(wC[h], w_down[lidx[li], ex][h * 1024:(h + 1) * 1024, :])

            def gather(ex):
                xgk = xg[ex % 2]

                def fn(e, s):
                    for sbk in range(4):
                        e.indirect_dma_start(out=xgk[:, sbk, :], out_offset=None, in_=XN2.rearrange("b s d -> (b s) d"),
                                             in_offset=bass.IndirectOffsetOnAxis(ap=sidx_g[:, b, ex, sbk:sbk + 1], axis=0)).then_inc(s, 16)
                P.op("gpsimd", fn, reads=allx[b] + [sidx_g.r], writes=[xgk.r], dma=xgk.r, ndma=4)

            gather(0)
            load_gu(0, 0)
            load_gu(0, 1)
            load_dn(0)
            for ex in range(NE):
                xgk = xg[ex % 2]
                for sbk in range(4):
                    for c in range(8):
                        P.op("tensor", lambda e, sbk=sbk, c=c, xgk=xgk: e.transpose(out=pbf[:, c * 128:(c + 1) * 128], in_=xgk[:, sbk, c * 128:(c + 1) * 128], identity=ident_b[:]),
                             reads=[xgk.r, ident_b.r], writes=[pbf.r])
                    P.op("vector", lambda e, sbk=sbk: e.tensor_copy(out=xgT[:, :, sbk * 128:(sbk + 1) * 128], in_=pbf[:, :].rearrange("p (c t) -> p c t", c=8)),
                         reads=[pbf.r], writes=[xgT.r])
                if ex + 1 < NE:
                    gather(ex + 1)
                for fc in range(16):
                    h, f8 = fc // 8, fc % 8
                    pg, pu = ps[(fc % 2) * 2], ps[(fc % 2) * 2 + 1]
                    for kc in range(8):
                        mm(pg[:, :], wA[h][:, kc, f8 * 128:(f8 + 1) * 128], xgT[:, kc, :], kc == 0, kc == 7, [wA[h].r, xgT.r], [pg.r])
                    for kc in range(8):
                        mm(pu[:, :], wB[h][:, kc, f8 * 128:(f8 + 1) * 128], xgT[:, kc, :], kc == 0, kc == 7, [wB[h].r, xgT.r], [pu.r])
                    s_ = sg[fc % 2]
                    P.op("scalar", lambda e, pg=pg, s_=s_: e.activation(out=s_[:], in_=pg[:, :], func=AF.Silu), reads=[pg.r], writes=[s_.r])
                    P.op("vector", lambda e, pu=pu, s_=s_, fc=fc: e.tensor_tensor(out=hg[:, fc, :], in0=s_[:], in1=pu[:, :], op=ALU.mult),
                         reads=[pu.r, s_.r], writes=[hg.r])
                    if f8 == 7 and ex + 1 < NE:
                        load_gu(ex + 1, h)
                for sbk in range(4):
                    y = yb[sbk]
                    for nb in range(2):
                        pt = ps[4 + nb]
                        for fc in range(16):
                            mm(pt[:, :], hg[:, fc, sbk * 128:(sbk + 1) * 128], wC[fc // 8][:, fc % 8, nb * 512:(nb + 1) * 512], fc == 0, fc == 15,
                               [hg.r, wC[fc // 8].r], [pt.r])
                        P.op("vector", lambda e, pt=pt, y=y, ex=ex, sbk=sbk, nb=nb: e.tensor_scalar(out=y[:, nb * 512:(nb + 1) * 512], in0=pt[:, :],
                                                                                           scalar1=sinfo[:, b, ex, sbk, 1:2], scalar2=None, op0=ALU.mult),
                             reads=[pt.r, sinfo.r], writes=[y.r])
                if ex + 1 < NE:
                    load_dn(ex + 1)
                for sbk in range(4):
                    y = yb[sbk]
                    P.op("gpsimd", lambda e, s, y=y, ex=ex, sbk=sbk: e.indirect_dma_start(
                        out=hdst.rearrange("b s d -> (b s) d"), out_offset=bass.IndirectOffsetOnAxis(ap=sidx_s[:, b, ex, sbk:sbk + 1], axis=0),
                        in_=y[:], in_offset=None, compute_op=ALU.add).then_inc(s, 16),
                        reads=[y.r, sidx_s.r] + (allh_dst[b] if sbk > 0 else []), writes=allh_dst[b] if sbk == 0 else [], dma=y.r)
                P.op("gpsimd", lambda e: e.nop(), reads=[yb[i].r.d for i in range(4)], writes=allh_dst[b])
            P.end_phase()

        def ple_layer(li, buf):
            P.begin_phase()
            N = NormCtx(8 + li)
            wg = sb("plewg", [128, 8, D], BF16)
            wp = sb("plewp", [128, 2, D], BF16)
            load_w_bf16(wg, ple_wg[li], parts=2)
            load_w_bf16(wp, ple_wp[li], parts=1)
            pl = [sb("plp%d" % i, [128, 4, PLE]) for i in range(2)]
            plb = sb("plpb", [128, 4, PLE], BF16)
            plTs = [sb("plpT%d" % i, [128, 2, TG], BF16) for i in range(2)]
            gsb = [sb("plg%d" % i, [128, 512]) for i in range(2)]
            for b in range(NB):
                def front_a(g):
                    hb = N.load_h(buf, b, g, g)
                    pk = pl[g % 2]
                    dma_in(pk, p_in[li, b * S + g * TG: b * S + (g + 1) * TG, :].rearrange("(j p) d -> p j d", p=128))
                    N.norm_a(g)
                    P.op("vector", lambda e, pk=pk: e.tensor_copy(out=plb[:], in_=pk[:]), reads=[pk.r], writes=[plb.r])
                    return hb

                def front_b(g, hb):
                    plT = plTs[g % 2]
                    xb, xt = N.norm_b(g)
                    for jt in range(4):
                        for c in range(2):
                            P.op("tensor", lambda e, jt=jt, c=c: e.transpose(out=pbf[:, c * 128:(c + 1) * 128], in_=plb[:, jt, c * 128:(c + 1) * 128], identity=ident_b[:]),
                                 reads=[plb.r, ident_b.r], writes=[pbf.r])
                        P.op("vector", lambda e, jt=jt, plT=plT: e.tensor_copy(out=plT[:, :, jt * 128:(jt + 1) * 128], in_=pbf[:, 0:256].rearrange("p (c t) -> p c t", c=2)),
                             reads=[pbf.r], writes=[plT.r])
                    return hb, xt, plT
                nxt = front_b(0, front_a(0))
                for g in range(NG):
                    hb, xt, plT = nxt
                    if g + 1 < NG:
                        hb_n = front_a(g + 1)
                    ob = N.obuf
                    for jt in range(4):
                        for nb in range(2):
                            pgt, ppt = ps[nb * 2], ps[nb * 2 + 1]
                            for kc in range(8):
                                mm(pgt[:, :], xt[:, kc, jt * 128:(jt + 1) * 128], wg[:, kc, nb * 512:(nb + 1) * 512], kc == 0, kc == 7, [xt.r, wg.r], [pgt.r])
                            for kc in range(2):
                                mm(ppt[:, :], plT[:, kc, jt * 128:(jt + 1) * 128], wp[:, kc, nb * 512:(nb + 1) * 512], kc == 0, kc == 1, [plT.r, wp.r], [ppt.r])
                            gs = gsb[nb]
                            P.op("scalar", lambda e, pgt=pgt, gs=gs: e.activation(out=gs[:], in_=pgt[:, :], func=AF.Sigmoid), reads=[pgt.r], writes=[gs.r])
                            P.op("vector", lambda e, ppt=ppt, gs=gs: e.tensor_tensor(out=gs[:], in0=gs[:], in1=ppt[:, :], op=ALU.mult), reads=[ppt.r, gs.r], writes=[gs.r])
                            P.op("vector", lambda e, gs=gs, jt=jt, nb=nb, hb=hb: e.tensor_tensor(out=ob[:, jt, nb * 512:(nb + 1) * 512], in0=gs[:],
                                                                                             in1=hb[:, jt, nb * 512:(nb + 1) * 512], op=ALU.add),
                                 reads=[gs.r, hb.r], writes=[ob.r])
                    N.store_o(buf, b, g)
                    if g + 1 < NG:
                        nxt = front_b(g + 1, hb_n)
            P.end_phase()

        def final_norm(buf):
            P.begin_phase()
            N = NormCtx(None)
            gv = sb("gvf", [128, D])
            dma_in(gv, gvec[12])
            for b in range(NB):
                for g in range(NG):
                    hb = N.load_h(buf, b, g, g)
                    st = N.stats(g)
                    ob = N.obuf
                    for j in range(4):
                        P.op("vector", lambda e, j=j, hb=hb, st=st: e.scalar_tensor_tensor(out=ob[:, j, :], in0=hb[:, j, :], scalar=st[:, 4 + j:5 + j], in1=gv[:],
                                                                                          op0=ALU.mult, op1=ALU.mult), reads=[hb.r, st.r, gv.r], writes=[ob.r])
                    N.store_o("out", b, g)
            P.end_phase()

        cur = "x"
        for li in layers:
            P.epoch = li
            j = li // 2
            if li % 2 == 0:
                rg_layer(li, j, cur, "HB")
            else:
                ft_layer(li, j, cur, "HB")
            cur = "HB"
            if stop == "mix" and li == layers[-1]:
                break
            moe_layer(li, "HB", "HA")
            cur = "HA"
            if stop == "moe" and li == layers[-1]:
                break
            ple_layer(li, "HA")
        P.epoch = 4
        if final:
            final_norm(cur)
        else:
            P.begin_phase()
            N = NormCtx(None)
            for b in range(NB):
                for g in range(NG):
                    hb = N.load_h(cur, b, g, g)
                    P.op("vector", lambda e, hb=hb: e.tensor_copy(out=N.obuf[:], in_=hb[:]), reads=[hb.r], writes=[N.obuf.r])
                    N.store_o("out", b, g)
            P.end_phase()
        P.begin_phase()
        P.op("sync", lambda e: e.nop(), reads=[], writes=[])
        P.end_phase()
    return nc


def _consts():
    ident = np.eye(128, dtype=np.float32)
    iota = np.tile(np.arange(1, 513, dtype=np.float32)[None, :], (128, 1))
    tok = (np.arange(32)[None, :] * 128 + np.arange(128)[:, None]).astype(np.float32)
    n = np.arange(S, dtype=np.int64)
    m = (n[:, None] * n[None, :]) % S
    ang = 2.0 * np.pi * m.astype(np.float64) / S
    dft = np.stack([np.cos(ang), -np.sin(ang)]).astype(np.float32)
    c = np.arange(256, dtype=np.int64)
    mc = (c[:, None] * c[None, :]) % 256
    angc = 2.0 * np.pi * mc.astype(np.float64) / 256
    cd = np.stack([np.cos(angc), np.sin(angc)]).astype(np.float32)
    tok2 = np.stack([np.floor(tok / 64.0), np.mod(tok, 64.0)], axis=-1).astype(np.float32)
    tok2 = np.ascontiguousarray(np.broadcast_to(tok2[:, :, None, :], (128, 32, NE, 2)))
    return dict(c_ident=ident, c_iota=iota, c_tok=tok, c_tok2=tok2, c_dft=dft, c_cdft=cd)


def _prep(inputs, NB, layers=(0, 1, 2, 3)):
    f = lambda a: np.ascontiguousarray(np.asarray(a, dtype=np.float32))
    m = {}
    m["x"] = f(inputs["x"])[:NB].reshape(NB * S, D)
    m["p"] = f(inputs["p"])[:, :NB].reshape(4, NB * S, PLE)
    gvv = np.concatenate([f(inputs["g_mix"]), f(inputs["g_ffn"]), f(inputs["g_ple"]), f(inputs["g_final"])[None, :]], axis=0)
    m["gvec"] = np.ascontiguousarray(np.broadcast_to(gvv[:, None, :], (13, 128, D)))
    m["rg_w_in"] = f(inputs["rg_w_in"])
    m["rg_b_in"] = np.ascontiguousarray(f(inputs["rg_b_in"]).reshape(2, 16, 128).transpose(0, 2, 1))
    conv = np.concatenate([f(inputs["rg_conv_w"]), f(inputs["rg_conv_b"])[:, None, :]], axis=1)
    m["rg_conv"] = np.ascontiguousarray(conv.reshape(2, 5, 8, 128).transpose(0, 3, 1, 2))
    gw = np.stack([f(inputs["rg_gx_w"]), f(inputs["rg_ga_w"])], axis=2)
    m["rg_gw"] = np.ascontiguousarray(gw)
    gb = np.stack([f(inputs["rg_gx_b"]), f(inputs["rg_ga_b"])], axis=2)
    m["rg_gb"] = np.ascontiguousarray(gb.reshape(2, 2, 2, 8, 128).transpose(0, 4, 1, 2, 3))
    m["rg_lam"] = np.ascontiguousarray(f(inputs["rg_lam"]).reshape(2, 2, 8, 128).transpose(0, 3, 1, 2))
    m["rg_w_out"] = f(inputs["rg_w_out"])
    m["rg_b_out"] = np.ascontiguousarray(np.broadcast_to(f(inputs["rg_b_out"])[:, None, :], (2, 128, D)))
    for k in ("ft_w_in", "ft_w_out", "w_router", "ple_w_proj", "ple_w_gate"):
        m[k] = f(inputs[k])
    for k in ("w_gate", "w_up", "w_down"):
        m[k] = np.ascontiguousarray(np.asarray(inputs[k], dtype=np.float32)[list(layers)])
    m.update(_consts())
    return m


def kernel(**inputs):
    NB = 1
    layers = (0, 1, 2, 3)
    nc = build(NB, list(layers), final=True)
    base = _prep({**inputs, "x": np.asarray(inputs["x"])[0:1], "p": np.asarray(inputs["p"])[:, 0:1]}, NB, layers)
    in_maps = []
    for b in range(4):
        m = dict(base)
        m["x"] = np.ascontiguousarray(np.asarray(inputs["x"], dtype=np.float32)[b]).reshape(S, D)
        m["p"] = np.ascontiguousarray(np.asarray(inputs["p"], dtype=np.float32)[:, b]).reshape(4, S, PLE)
        in_maps.append(m)
    res = run_bass_kernel_spmd(nc, in_maps, core_ids=list(range(4)))
    out = np.stack([np.asarray(res.results[b]["out"], dtype=np.float32).reshape(S, D) for b in range(4)], axis=0)
    return out
```
